# Optimizing a Trainium2 kernel written in Bass

```python
import math
import jax, jax.numpy as jnp
from jax import lax
import numpy as np

D_MODEL = 1024
BATCH = 8
SEQ = 4096
DEPTH = 2

GRID_W = 64
CTX_LEN = 256
RET_HEADS = 4
RET_QK_DIM = 64
RET_V_DIM = 128
RET_CHUNK = 128
ATT_HEADS = 4
ATT_KV_HEADS = 2
ATT_HEAD_DIM = 128
ATT_BLOCK = 128
ROPE_THETA = 10000.0
POOL_WINDOWS = (2, 4, 8, 16)
POOL_GROUP_DIM = 128
N_BRANCH = 3
BRANCH_WIDTH = 512
FFN_DIM = 3584
N_EXPERTS = 8
TOP_K = 2
NORM_EPS = 1e-6
GN_EPS = 1e-5
RET_QK_WIDTH = RET_HEADS * RET_QK_DIM
RET_WIDTH = RET_HEADS * RET_V_DIM
ATT_Q_WIDTH = ATT_HEADS * ATT_HEAD_DIM
ATT_KV_WIDTH = ATT_KV_HEADS * ATT_HEAD_DIM
POOL_WIDTH = len(POOL_WINDOWS) * POOL_GROUP_DIM
IN_SPLITS = (RET_QK_WIDTH, RET_QK_WIDTH, RET_WIDTH, RET_WIDTH, ATT_Q_WIDTH, ATT_KV_WIDTH, ATT_KV_WIDTH, POOL_WIDTH, N_BRANCH * D_MODEL)
IN_WIDTH = sum(IN_SPLITS)

kernel_name = 'hybrid_retention_gqa_pool_moe_dit'


def rms_norm(x, gain):
    xf = x.astype(jnp.float32)
    y = xf * lax.rsqrt(jnp.mean(xf * xf, axis=-1, keepdims=True) + NORM_EPS)
    return (y * gain.astype(jnp.float32)).astype(x.dtype)


def modulate(x, shift, scale):
    return x * (1 + scale) + shift


def flip(a):
    return a[:, ::-1]


def rope_angles(pos, dim):
    freqs = 1.0 / (ROPE_THETA ** (jnp.arange(0, dim, 2, dtype=jnp.float32) / dim))
    return pos[:, None] * freqs[None, :]


def apply_rope(x, ang):
    xf = x.astype(jnp.float32).reshape(x.shape[:-1] + (x.shape[-1] // 2, 2))
    cos = jnp.cos(ang)[None, :, None, :]
    sin = jnp.sin(ang)[None, :, None, :]
    x0, x1 = xf[..., 0], xf[..., 1]
    out = jnp.stack([x0 * cos - x1 * sin, x0 * sin + x1 * cos], axis=-1)
    return out.reshape(x.shape).astype(x.dtype)


def retention_scan(q, k, v, log_g, state0):
    b, n, h, _ = q.shape
    dv = v.shape[-1]
    nc = n // RET_CHUNK

    def chunks(a):
        return a.reshape(b, nc, RET_CHUNK, h, a.shape[-1]).transpose(1, 0, 3, 2, 4)

    pos = jnp.arange(RET_CHUNK, dtype=jnp.float32)
    diff = pos[:, None] - pos[None, :]
    decay_in = jnp.where(diff >= 0, jnp.exp(jnp.maximum(diff, 0.0) * log_g[:, None, None]), 0.0)
    xi = jnp.exp((pos + 1.0) * log_g[:, None])[None, :, :, None]
    zeta = jnp.exp((RET_CHUNK - 1.0 - pos) * log_g[:, None])[None, :, :, None]
    g_chunk = jnp.exp(RET_CHUNK * log_g)[None, :, None, None]

    def step(state, inp):
        qc, kc, vc = inp
        scores = jnp.einsum('bhld,bhmd->bhlm', qc, kc) * decay_in
        out = jnp.einsum('bhlm,bhme->bhle', scores, vc) + jnp.einsum('bhld,bhde->bhle', qc, state) * xi
        state = g_chunk * state + jnp.einsum('bhld,bhle->bhde', kc * zeta, vc)
        return state, out

    state, out = lax.scan(step, state0, (chunks(q), chunks(k), chunks(v)))
    out = out.transpose(1, 0, 3, 2, 4).reshape(b, n, h, dv)
    return out, state


def retention_branch(q, k, v, g, qc, kc, vc, gc, ret_decay, ret_gn, ang_ret, need_ctx):
    f32 = jnp.float32
    b = q.shape[0]

    def heads(a, d):
        return a.astype(f32).reshape(a.shape[0], a.shape[1], RET_HEADS, d)

    scale = RET_QK_DIM ** -0.5
    q_l = apply_rope(heads(q, RET_QK_DIM), ang_ret)
    k_l = apply_rope(heads(k, RET_QK_DIM), ang_ret) * scale
    v_l = heads(v, RET_V_DIM)
    q_c = heads(qc, RET_QK_DIM)
    k_c = heads(kc, RET_QK_DIM) * scale
    v_c = heads(vc, RET_V_DIM)
    log_g = -jnp.exp(ret_decay.astype(f32))
    zeros = jnp.zeros((b, RET_HEADS, RET_QK_DIM, RET_V_DIM), f32)
    oc_f, st_f = retention_scan(q_c, k_c, v_c, log_g[0], zeros)
    oc_b, st_b = retention_scan(flip(q_c), flip(k_c), flip(v_c), log_g[1], zeros)
    o_f, _ = retention_scan(q_l, k_l, v_l, log_g[0], st_f)
    o_b, _ = retention_scan(flip(q_l), flip(k_l), flip(v_l), log_g[1], st_b)
    gn_w = ret_gn.astype(f32).reshape(RET_HEADS, RET_V_DIM)

    def finish(o, gate):
        mu = jnp.mean(o, axis=-1, keepdims=True)
        var = jnp.mean(jnp.square(o - mu), axis=-1, keepdims=True)
        o = (o - mu) * lax.rsqrt(var + GN_EPS) * gn_w
        o = o.reshape(o.shape[0], o.shape[1], RET_WIDTH)
        return (jax.nn.silu(gate.astype(f32)) * o).astype(gate.dtype)

    y = finish(o_f + flip(o_b), g)
    yc = finish(oc_f + flip(oc_b), gc) if need_ctx else None
    return y, yc


def attention_branch(q, k, v, qc, kc, vc, attn_qn, attn_kn, ang_att, need_ctx):
    b, s, _ = q.shape
    grp = ATT_HEADS // ATT_KV_HEADS

    def prep_q(a):
        return rms_norm(a.reshape(a.shape[0], a.shape[1], ATT_HEADS, ATT_HEAD_DIM), attn_qn)

    def prep_k(a):
        return rms_norm(a.reshape(a.shape[0], a.shape[1], ATT_KV_HEADS, ATT_HEAD_DIM), attn_kn)

    q_l = apply_rope(prep_q(q), ang_att)
    k_l = apply_rope(prep_k(k), ang_att)
    v_l = v.reshape(b, s, ATT_KV_HEADS, ATT_HEAD_DIM)
    k_c = prep_k(kc)
    v_c = vc.reshape(b, vc.shape[1], ATT_KV_HEADS, ATT_HEAD_DIM)
    keys = jnp.concatenate([k_l, k_c], axis=1)
    vals = jnp.concatenate([v_l, v_c], axis=1)
    scale = ATT_HEAD_DIM ** -0.5

    def attend(qb, kk, vv):
        sc = jnp.einsum('bqkgd,bskd->bkgqs', qb, kk).astype(jnp.float32) * scale
        p = jax.nn.softmax(sc, axis=-1).astype(vv.dtype)
        return jnp.einsum('bkgqs,bskd->bqkgd', p, vv)

    nb = s // ATT_BLOCK
    qblocks = q_l.reshape(b, nb, ATT_BLOCK, ATT_KV_HEADS, grp, ATT_HEAD_DIM).transpose(1, 0, 2, 3, 4, 5)
    o = lax.map(lambda qb: attend(qb, keys, vals), qblocks)
    y = o.transpose(1, 0, 2, 3, 4, 5).reshape(b, s, ATT_Q_WIDTH)
    yc = None
    if need_ctx:
        q_c = prep_q(qc).reshape(b, qc.shape[1], ATT_KV_HEADS, grp, ATT_HEAD_DIM)
        yc = attend(q_c, k_c, v_c).reshape(b, qc.shape[1], ATT_Q_WIDTH)
    return y, yc


def pool_mixer(u, pool_w, pool_scale):
    b, n, _ = u.shape
    uf = u.astype(jnp.float32)
    cs = jnp.concatenate([jnp.zeros((b, 1, POOL_WIDTH), jnp.float32), jnp.cumsum(uf, axis=1)], axis=1)
    t = jnp.arange(n)
    diffs = []
    for gi, w in enumerate(POOL_WINDOWS):
        lo = jnp.clip(t - w // 2, 0, n)
        hi = jnp.clip(t + w // 2, 0, n)
        sl = slice(gi * POOL_GROUP_DIM, (gi + 1) * POOL_GROUP_DIM)
        csg = cs[..., sl]
        mean = (jnp.take(csg, hi, axis=1) - jnp.take(csg, lo, axis=1)) / (hi - lo).astype(jnp.float32)[None, :, None]
        diffs.append(mean - uf[..., sl])
    d = jnp.stack(diffs, axis=2).astype(u.dtype)
    y = jnp.einsum('bngc,gcd->bngd', d, pool_w)
    return y.reshape(b, n, POOL_WIDTH) * pool_scale


def token_mixer(h, hc, w_in, ret_decay, ret_gn, attn_qn, attn_kn, pool_w, pool_scale, w_branch, w_out, ang_att, ang_ret, need_ctx):
    offs = np.cumsum(IN_SPLITS)[:-1].tolist()
    p = jnp.split(h @ w_in, offs, axis=-1)
    pc = jnp.split(hc @ w_in, offs, axis=-1)
    y_ret, yc_ret = retention_branch(p[0], p[1], p[2], p[3], pc[0], pc[1], pc[2], pc[3], ret_decay, ret_gn, ang_ret, need_ctx)
    y_att, yc_att = attention_branch(p[4], p[5], p[6], pc[4], pc[5], pc[6], attn_qn, attn_kn, ang_att, need_ctx)

    def merge(y_r, y_a, y_p, gate_logits):
        gates = jax.nn.sigmoid(gate_logits.astype(jnp.float32)).astype(y_r.dtype)
        gates = gates.reshape(gates.shape[:-1] + (N_BRANCH, D_MODEL))
        mixed = (gates[..., 0, :] * (y_r @ w_branch[0])
                 + gates[..., 1, :] * (y_a @ w_branch[1])
                 + gates[..., 2, :] * (y_p @ w_branch[2]))
        return mixed @ w_out

    y = merge(y_ret, y_att, pool_mixer(p[7], pool_w, pool_scale), p[8])
    yc = merge(yc_ret, yc_att, pool_mixer(pc[7], pool_w, pool_scale), pc[8]) if need_ctx else None
    return y, yc


def swiglu(h, w1, w3, w2):
    return (jax.nn.silu(h @ w1) * (h @ w3)) @ w2


def moe_swiglu(h, router_w, router_b, w1, w3, w2):
    shape = h.shape
    t = h.reshape(-1, shape[-1])
    logits = (t @ router_w + router_b).astype(jnp.float32)
    top_logit, top_idx = lax.top_k(logits, TOP_K)
    weights = jax.nn.softmax(top_logit, axis=-1)
    flat_e = top_idx.reshape(-1)
    order = jnp.argsort(flat_e)
    tok = order // TOP_K
    sizes = jnp.bincount(flat_e, length=N_EXPERTS).astype(jnp.int32)
    xs = t[tok]
    a = lax.ragged_dot(xs, w1, sizes)
    bb = lax.ragged_dot(xs, w3, sizes)
    ys = lax.ragged_dot(jax.nn.silu(a) * bb, w2, sizes)
    ys = ys * weights.reshape(-1)[order][:, None].astype(ys.dtype)
    out = jnp.zeros_like(t).at[tok].add(ys)
    return out.reshape(shape)


def channel_mixer(a, layer, ffn_w1, ffn_w3, ffn_w2, moe_router, moe_router_b, moe_w1, moe_w3, moe_w2):
    j = layer // 2
    if layer % 2 == 0:
        return swiglu(a, ffn_w1[j], ffn_w3[j], ffn_w2[j])
    return moe_swiglu(a, moe_router[j], moe_router_b[j], moe_w1[j], moe_w3[j], moe_w2[j])


def setup_inputs(seed: int = 0) -> dict:
    key = jax.random.key(seed)
    ks = jax.random.split(key, 26)
    f32 = jnp.float32
    n_dense = (DEPTH + 1) // 2
    n_moe = DEPTH // 2

    def normal(k, shape, scale):
        return jax.random.normal(k, shape, f32) * scale

    def gain(k, shape):
        return 1.0 + 0.02 * jax.random.normal(k, shape, f32)

    heads = jnp.arange(RET_HEADS, dtype=f32)
    base_decay = jnp.log(-jnp.log(1.0 - 2.0 ** (-5.0 - heads)))
    return {
        'x': normal(ks[0], (BATCH, SEQ, D_MODEL), 1.0),
        'c': normal(ks[1], (BATCH, D_MODEL), 1.0),
        'ctx': normal(ks[2], (BATCH, CTX_LEN, D_MODEL), 1.0),
        'c_ctx': normal(ks[3], (D_MODEL,), 1.0),
        'w_ada': normal(ks[4], (DEPTH, D_MODEL, 6 * D_MODEL), 0.5 * D_MODEL ** -0.5),
        'b_ada': normal(ks[5], (DEPTH, 6 * D_MODEL), 0.02),
        'norm_mix': gain(ks[6], (DEPTH, D_MODEL)),
        'norm_ffn': gain(ks[7], (DEPTH, D_MODEL)),
        'w_in': normal(ks[8], (DEPTH, D_MODEL, IN_WIDTH), D_MODEL ** -0.5),
        'ret_decay': base_decay + normal(ks[9], (DEPTH, 2, RET_HEADS), 0.1),
        'ret_gn': gain(ks[10], (DEPTH, RET_WIDTH)),
        'attn_qn': gain(ks[11], (DEPTH, ATT_HEAD_DIM)),
        'attn_kn': gain(ks[12], (DEPTH, ATT_HEAD_DIM)),
        'pool_w': normal(ks[13], (DEPTH, len(POOL_WINDOWS), POOL_GROUP_DIM, POOL_GROUP_DIM), POOL_GROUP_DIM ** -0.5),
        'pool_scale': gain(ks[14], (DEPTH, POOL_WIDTH)),
        'w_branch': normal(ks[15], (DEPTH, N_BRANCH, BRANCH_WIDTH, D_MODEL), BRANCH_WIDTH ** -0.5),
        'w_out': normal(ks[16], (DEPTH, D_MODEL, D_MODEL), D_MODEL ** -0.5),
        'ffn_w1': normal(ks[17], (n_dense, D_MODEL, FFN_DIM), D_MODEL ** -0.5),
        'ffn_w3': normal(ks[18], (n_dense, D_MODEL, FFN_DIM), D_MODEL ** -0.5),
        'ffn_w2': normal(ks[19], (n_dense, FFN_DIM, D_MODEL), FFN_DIM ** -0.5),
        'moe_router': normal(ks[20], (n_moe, D_MODEL, N_EXPERTS), D_MODEL ** -0.5),
        'moe_router_b': normal(ks[21], (n_moe, N_EXPERTS), 0.01),
        'moe_w1': normal(ks[22], (n_moe, N_EXPERTS, D_MODEL, FFN_DIM), D_MODEL ** -0.5),
        'moe_w3': normal(ks[23], (n_moe, N_EXPERTS, D_MODEL, FFN_DIM), D_MODEL ** -0.5),
        'moe_w2': normal(ks[24], (n_moe, N_EXPERTS, FFN_DIM, D_MODEL), FFN_DIM ** -0.5),
        'final_norm': gain(ks[25], (D_MODEL,)),
    }


def reference(x, c, ctx, c_ctx, w_ada, b_ada, norm_mix, norm_ffn, w_in, ret_decay, ret_gn, attn_qn, attn_kn, pool_w, pool_scale, w_branch, w_out, ffn_w1, ffn_w3, ffn_w2, moe_router, moe_router_b, moe_w1, moe_w3, moe_w2, final_norm):
    s = x.shape[1]
    rows = s // GRID_W
    row = jnp.repeat(jnp.arange(rows, dtype=jnp.float32), GRID_W)
    col = jnp.tile(jnp.arange(GRID_W, dtype=jnp.float32), rows)
    half = ATT_HEAD_DIM // 2
    ang_att = jnp.concatenate([rope_angles(row, half), rope_angles(col, half)], axis=-1)
    ang_ret = rope_angles(jnp.arange(s, dtype=jnp.float32), RET_QK_DIM)
    s_c = jax.nn.silu(c)
    s_cc = jax.nn.silu(c_ctx)
    xc = ctx
    for i in range(DEPTH):
        need_ctx = i < DEPTH - 1
        sh1, sc1, g1, sh2, sc2, g2 = jnp.split((s_c @ w_ada[i] + b_ada[i])[:, None, :], 6, axis=-1)
        csh1, csc1, cg1, csh2, csc2, cg2 = jnp.split(s_cc @ w_ada[i] + b_ada[i], 6, axis=-1)
        h = modulate(rms_norm(x, norm_mix[i]), sh1, sc1)
        hc = modulate(rms_norm(xc, norm_mix[i]), csh1, csc1)
        y, yc = token_mixer(h, hc, w_in[i], ret_decay[i], ret_gn[i], attn_qn[i], attn_kn[i], pool_w[i], pool_scale[i], w_branch[i], w_out[i], ang_att, ang_ret, need_ctx)
        x = x + g1 * y
        x = x + g2 * channel_mixer(modulate(rms_norm(x, norm_ffn[i]), sh2, sc2), i, ffn_w1, ffn_w3, ffn_w2, moe_router, moe_router_b, moe_w1, moe_w3, moe_w2)
        if need_ctx:
            xc = xc + cg1 * yc
            xc = xc + cg2 * channel_mixer(modulate(rms_norm(xc, norm_ffn[i]), csh2, csc2), i, ffn_w1, ffn_w3, ffn_w2, moe_router, moe_router_b, moe_w1, moe_w3, moe_w2)
    return rms_norm(x, final_norm)
```

```python
import os
import numpy as np
import ml_dtypes
CUT = int(os.environ.get('MK_CUT', '99'))
from contextlib import ExitStack
import concourse.bass as bass
import concourse.mybir as mybir
from concourse.bass_utils import run_bass_kernel_spmd

F32 = mybir.dt.float32
BF16 = mybir.dt.bfloat16
AF = mybir.ActivationFunctionType
ALU = mybir.AluOpType
AX = mybir.AxisListType

ALL_Q = ("tensor", "vector", "scalar", "gpsimd", "sync")
N_DMA_SEMS = 16

D = 1024
SEQ = 4096
CTX = 256
NT = SEQ + CTX
NTILE = NT // 128
FFN = 3584
NE = 8
INW = 6144
SB_BASE = 16640
SB_END = 229376
NORM_EPS = 1e-6
GN_EPS = 1e-5


class Op:
    __slots__ = ("q", "fn", "is_dma", "deps", "signal", "sem", "target", "prev_on_sem")

    def __init__(self, q, fn, is_dma):
        self.q = q
        self.fn = fn
        self.is_dma = is_dma
        self.deps = []
        self.signal = False
        self.sem = None
        self.target = 0
        self.prev_on_sem = None


class Prog:
    def __init__(self, nc, same_engine_sync=True):
        self.nc = nc
        self.ops = {q: [] for q in ALL_Q}
        self.last_writer = {}
        self.readers = {}
        self.same_engine_sync = same_engine_sync
        self.dma_rr = {q: 0 for q in ALL_Q}
        self.dma_last = {}
        self.mute = False

    def op(self, q, fn, reads=(), writes=(), dma=False, extra_deps=()):
        if self.mute:
            return None
        o = Op(q, fn, dma)
        deps = list(extra_deps)
        for r in reads:
            w = self.last_writer.get(r)
            if w is not None:
                deps.append(w)
        for w_ in writes:
            w = self.last_writer.get(w_)
            if w is not None:
                deps.append(w)
            deps.extend(self.readers.get(w_, ()))
        seen = set()
        for d in deps:
            if id(d) in seen or d is o:
                continue
            seen.add(id(d))
            if d.q == q and not d.is_dma:
                if q == "tensor" or not self.same_engine_sync:
                    continue
            o.deps.append(d)
            d.signal = True
        for r in reads:
            self.readers.setdefault(r, []).append(o)
        for w_ in writes:
            self.last_writer[w_] = o
            self.readers[w_] = []
        if dma:
            k = self.dma_rr[q]
            self.dma_rr[q] = k + 1
            slot = (q, k % N_DMA_SEMS)
            o.sem = slot
            o.prev_on_sem = self.dma_last.get(slot)
            o.target = (o.prev_on_sem.target if o.prev_on_sem is not None else 0) + 16
            self.dma_last[slot] = o
        self.ops[q].append(o)
        return o

    def dma(self, q, out, in_, reads=(), writes=(), **kw):
        return self.op(q, lambda e: e.dma_start(out=out, in_=in_, **kw), reads, writes, dma=True)

    def barrier(self):
        if self.mute:
            return
        deps = []
        for q in ALL_Q:
            for o in reversed(self.ops[q]):
                if not o.is_dma:
                    deps.append(o)
                    break
        deps.extend(self.dma_last.values())
        b = self.op("sync", lambda e: e.nop(), extra_deps=deps)
        for q in ALL_Q:
            if q != "sync":
                self.op(q, lambda e: e.nop(), extra_deps=[b])
        self.last_writer = {}
        self.readers = {}

    def emit(self):
        nc = self.nc
        for q in ALL_Q:
            c = 0
            for o in self.ops[q]:
                if o.is_dma:
                    continue
                if o.signal:
                    c += 1
                    o.target = c
        with ExitStack() as st:
            qsem = {q: st.enter_context(nc.semaphore("s_" + q)) for q in ALL_Q}
            dsem = {}
            for q in ALL_Q:
                for k in range(min(N_DMA_SEMS, self.dma_rr[q])):
                    dsem[(q, k)] = st.enter_context(nc.semaphore("d_%s_%d" % (q, k)))
            block = st.enter_context(nc.Block())

            def run_queue(q, eng):
                known = {}

                def wait_for(d):
                    if d.is_dma:
                        key = d.sem
                        sem = dsem[key]
                    else:
                        key = d.q
                        sem = qsem[d.q]
                    if known.get(key, 0) >= d.target:
                        return
                    eng.wait_ge(sem, d.target)
                    known[key] = d.target

                for o in self.ops[q]:
                    for d in o.deps:
                        wait_for(d)
                    if o.is_dma and o.prev_on_sem is not None:
                        wait_for(o.prev_on_sem)
                    ins = o.fn(eng)
                    if o.is_dma:
                        ins.then_inc(dsem[o.sem], 16)
                    elif o.signal:
                        ins.then_inc(qsem[q], 1)

            @block.tensor
            def _(e):
                run_queue("tensor", e)

            @block.vector
            def _(e):
                run_queue("vector", e)

            @block.scalar
            def _(e):
                run_queue("scalar", e)

            @block.gpsimd
            def _(e):
                run_queue("gpsimd", e)

            @block.sync
            def _(e):
                run_queue("sync", e)


class Arena:
    def __init__(self, nc, base=SB_BASE, end=SB_END):
        self.nc = nc
        self.base = base
        self.off = base
        self.end = end
        self.n = 0

    def reset(self, to=None):
        self.off = self.base if to is None else to

    def mark(self):
        return self.off

    def alloc(self, name, shape, dt):
        esz = 2 if dt == BF16 else 4
        nbytes = int(np.prod(shape[1:])) * esz
        nbytes = (nbytes + 63) // 64 * 64
        assert self.off + nbytes <= self.end, ("SBUF overflow", name, self.off, nbytes)
        self.n += 1
        t = self.nc.alloc_sbuf_tensor_at("%s_%d" % (name, self.n), list(shape), dt, offset=self.off)
        self.off += nbytes
        return t


def _host_consts():
    c = {}
    c["ident"] = np.eye(128, dtype=np.float32)
    half = 64
    fr = (1.0 / (10000.0 ** (np.arange(0, half, 2, dtype=np.float32) / np.float32(half)))).astype(np.float32)
    t = np.arange(SEQ)
    row = (t // 64).astype(np.float32)
    col = (t % 64).astype(np.float32)
    ang = np.concatenate([row[:, None] * fr[None, :], col[:, None] * fr[None, :]], axis=-1).astype(np.float32)
    cos = np.concatenate([np.cos(ang), np.ones((CTX, 64), np.float32)], 0).astype(np.float32)
    sin = np.concatenate([np.sin(ang), np.zeros((CTX, 64), np.float32)], 0).astype(np.float32)

    def tl(a):
        return np.ascontiguousarray(a.reshape(NTILE, 128, -1).transpose(1, 0, 2))

    c["acos"] = tl(cos)
    c["asin"] = tl(sin)
    fr2 = (1.0 / (10000.0 ** (np.arange(0, 64, 2, dtype=np.float32) / np.float32(64)))).astype(np.float32)
    ang2 = (np.arange(SEQ, dtype=np.float32)[:, None] * fr2[None, :]).astype(np.float32)
    rc = np.concatenate([np.cos(ang2), np.ones((CTX, 32), np.float32)], 0).astype(np.float32)
    rs = np.concatenate([np.sin(ang2), np.zeros((CTX, 32), np.float32)], 0).astype(np.float32)
    c["rcos"] = tl(rc)
    c["rsin"] = tl(rs)
    c["rcosk"] = tl(rc * np.float32(0.125))
    c["rsink"] = tl(rs * np.float32(0.125))
    m = np.arange(128, dtype=np.float32)[:, None]
    l = np.arange(128, dtype=np.float32)[None, :]
    rt = np.zeros((128, 6, 128), np.float32)
    rt[:, 0, :] = np.maximum(l - m, 0)
    rt[:, 1, :] = (l >= m)
    rt[:, 2, :] = np.maximum(m - l, 0)
    rt[:, 3, :] = (m >= l)
    rt[:, 4, :] = l + 1.0
    rt[:, 5, :] = 128.0 - l
    c["rtab"] = rt
    pc = np.zeros((128, 2), np.float32)
    pc[:, 0] = 127.0 - np.arange(128)
    pc[:, 1] = np.arange(128)
    c["pcol"] = pc
    pe = np.ones((128, 4, 2, 8), np.float32)
    for gi, w in enumerate((2, 4, 8, 16)):
        hw = w // 2
        for j in range(hw):
            pe[:, gi, 0, j] = w / float(j + hw)
            cnt = min(j + 1 + hw, w)
            pe[:, gi, 1, hw - 1 - j] = w / float(cnt)
    c["pedge"] = pe
    c["tri"] = np.triu(np.ones((128, 128), np.float32), 1)
    io = np.zeros((128, 25), np.float32)
    io[:, 0:24] = np.arange(24, dtype=np.float32)[None, :]
    io[:, 24] = np.arange(128, dtype=np.float32)
    c["iot"] = io
    c["prow"] = np.ascontiguousarray(np.broadcast_to((NE * 1280.0 + np.arange(128, dtype=np.float32))[:, None], (128, NE)))
    c["ec"] = np.ascontiguousarray(np.broadcast_to((np.arange(NE, dtype=np.float32) * 1280.0)[None, :], (128, NE)))
    return c


def build(dbg=False, stop_after=None, skip=(), ext=None):
    nc = bass.Bass("TRN2", target_bir_lowering=False)
    P = Prog(nc, same_engine_sync=(os.environ.get('MK_SES', '1') == '1'))
    A = Arena(nc)

    def din(name, shape, dt=F32):
        kind = "ExternalInput" if (ext is None or name in ext) else "Internal"
        return nc.dram_tensor(name, list(shape), dt, kind=kind).ap()

    skind = "ExternalOutput" if dbg else "Internal"

    def dscr(name, shape, dt=F32):
        return nc.dram_tensor(name, list(shape), dt, kind=skind).ap()

    xin = din("xin", [NT, D])
    cT = din("cT", [128, 16])
    w_ada = din("w_ada", [2, D, INW])
    b_ada = din("b_ada", [2, INW])
    norm_mix = din("norm_mix", [2, D])
    norm_ffn = din("norm_ffn", [2, D])
    w_in = din("w_in", [2, D, INW])
    ret_decay = din("ret_decay", [2, 8])
    ret_gn = din("ret_gn", [2, 512])
    attn_qn = din("attn_qn", [2, 128])
    attn_kn = din("attn_kn", [2, 128])
    pool_w = din("pool_w", [2, 4, 128, 128])
    pool_scale = din("pool_scale", [2, 512])
    w_branch = din("w_branch", [2, 3, 512, D])
    w_out = din("w_out", [2, D, D])
    ffn_w1 = din("ffn_w1", [1, D, FFN])
    ffn_w3 = din("ffn_w3", [1, D, FFN])
    ffn_w2 = din("ffn_w2", [1, FFN, D])
    moe_router_t = din("moe_router_t", [NE, D])
    moe_router_p = din("moe_router_p", [128, 8, NE])
    moe_router_b = din("moe_router_b", [1, NE])
    w1q = [[din("w1q_%d_%d" % (fb, h), [NE * 128, 2048]) for h in range(2)] for fb in range(7)]
    w3q = [[din("w3q_%d_%d" % (fb, h), [NE * 128, 2048]) for h in range(2)] for fb in range(7)]
    w2q = [[din("w2q_%d_%d" % (fb, h), [NE * 128, 2048]) for h in range(2)] for fb in range(7)]
    final_norm = din("final_norm", [D])
    k_ident = din("k_ident", [128, 128])
    k_acos = din("k_acos", [128, NTILE, 64])
    k_asin = din("k_asin", [128, NTILE, 64])
    k_rt4 = din("k_rt4", [128, NTILE, 4, 32])
    k_rtab = din("k_rtab", [128, 6, 128])
    k_pcol = din("k_pcol", [128, 2])
    k_pedge = din("k_pedge", [128, 4, 2, 8])
    k_tri = din("k_tri", [128, 128])
    k_iot = din("k_iot", [128, 25])

    out = nc.dram_tensor("out", [SEQ, D], F32, kind="ExternalOutput").ap()

    modd = dscr("modd", [2, 2, 6, 128, D])
    hT_d = dscr("hT_d", [128, 8, NT], BF16)
    yT_d = dscr("yT_d", [3, 128, 4, NT], BF16)
    xa_d = dscr("xa_d", [NT, D])
    xb_d = dscr("xb_d", [NT, D])
    xc_d = dscr("xc_d", [NT, D])
    xd_d = dscr("xd_d", [NT, D])
    G_d = dscr("G_d", [24 * 512, D], BF16)
    Y_d = dscr("Y_d", [24 * 512, D])

    ps = [nc.alloc_psum_tensor("ps%d" % i, [128, 512], F32) for i in range(8)]
    PS = ["ps%d" % i for i in range(8)]

    GROUPS = [(g * 512, 512) for g in range(8)] + [(SEQ, CTX)]

    ident = A.alloc("ident", [128, 128], BF16)
    identf = A.alloc("identf", [128, 128], F32)
    ones_bf = A.alloc("ones", [128, 128], BF16)
    P.dma("sync", identf[:], k_ident, writes=["identf"])
    P.op("vector", lambda e: e.tensor_copy(ident[:], identf[:]), reads=["identf"], writes=["ident"])
    P.op("vector", lambda e: e.memset(ones_bf[:], 1.0), writes=["ones"])
    P.barrier()
    A_PERSIST = A.mark()

    def done(tag):
        return stop_after is not None and stop_after == tag

    def finish():
        P.mute = False
        P.barrier()
        P.emit()
        return nc

    def make_normT(pfx, Amod, Bmod, psbank):
        xt = [A.alloc(pfx + "xt%d" % i, [128, D], F32) for i in range(2)]
        junks = [A.alloc(pfx + "junk%d" % i, [128, D], BF16) for i in range(2)]
        tmps = [A.alloc(pfx + "tmp%d" % i, [128, D], F32) for i in range(2)]
        hb = [A.alloc(pfx + "hb%d" % i, [128, D], BF16) for i in range(4)]
        sts = [A.alloc(pfx + "st%d" % i, [128, 4], F32) for i in range(2)]
        cnt = [0]

        def runA(src_d, t0, ntl, which):
            hs = []
            for j in range(ntl):
                i = cnt[0]
                cnt[0] += 1
                s = i % 2
                x_t, h_b = xt[s], hb[i % 4]
                junk, tmp, st = junks[s], tmps[s], sts[s]
                xtok, htok = pfx + "xt%d" % s, pfx + "hb%d" % (i % 4)
                sfx = "_%d" % s
                r0 = t0 + j * 128
                P.dma("sync", x_t[:], src_d[r0:r0 + 128, :], writes=[xtok])
                P.op("scalar", lambda e, x_t=x_t, junk=junk, st=st: e.activation(junk[:], x_t[:], AF.Square, accum_out=st[:, 0:1]),
                     reads=[xtok], writes=[pfx + "junk" + sfx, pfx + "st" + sfx])
                P.op("scalar", lambda e, st=st: e.activation(st[:, 1:2], st[:, 0:1], AF.Sqrt, bias=NORM_EPS, scale=1.0 / D),
                     reads=[pfx + "st" + sfx], writes=[pfx + "st1" + sfx])
                P.op("vector", lambda e, st=st: e.reciprocal(st[:, 2:3], st[:, 1:2]),
                     reads=[pfx + "st1" + sfx], writes=[pfx + "st2" + sfx])
                am, bm = Amod[which], Bmod[which]
                P.op("vector", lambda e, x_t=x_t, am=am, tmp=tmp, st=st: e.scalar_tensor_tensor(tmp[:], x_t[:], st[:, 2:3], am[0][:], ALU.mult, ALU.mult),
                     reads=[xtok, pfx + "st2" + sfx, am[1]], writes=[pfx + "tmp" + sfx])
                P.op("gpsimd", lambda e, h_b=h_b, bm=bm, tmp=tmp: e.tensor_tensor(h_b[:], tmp[:], bm[0][:], ALU.add),
                     reads=[pfx + "tmp" + sfx, bm[1]], writes=[htok])
                hs.append((h_b, htok))
            return hs

        def runB(hs, dst, dst_tok):
            pb = ps[psbank][:].bitcast(BF16)
            for j, (h_b, htok) in enumerate(hs):
                for k in range(8):
                    P.op("tensor", lambda e, k=k, h_b=h_b: e.transpose(pb[:, k * 128:(k + 1) * 128], h_b[:, k * 128:(k + 1) * 128], ident[:]),
                         reads=[htok, "ident"], writes=[PS[psbank]])
                P.op("scalar", lambda e, j=j: e.copy(dst[:, :, j * 128:(j + 1) * 128], pb[:, 0:1024].rearrange("p (k t) -> p k t", k=8)),
                     reads=[PS[psbank]], writes=[dst_tok])

        def run(src_d, t0, ntl, dst, dst_tok, which):
            prev = None
            for j in range(ntl):
                cur = (runA(src_d, t0 + j * 128, 1, which), j)
                if prev is not None:
                    runB(prev[0], dst[:, :, prev[1] * 128:(prev[1] + 1) * 128], dst_tok)
                prev = cur
            runB(prev[0], dst[:, :, prev[1] * 128:(prev[1] + 1) * 128], dst_tok)

        return runA, runB, run

    def load_mod(L, idxs):
        res = {}
        for idx in idxs:
            pair = []
            for w in range(2):
                t = A.alloc("mod%d_%d" % (idx, w), [128, D], F32)
                tok = "mod%d_%d" % (idx, w)
                P.dma("sync", t[:], modd[L, w, idx], reads=[("modd", L, w, idx)], writes=[tok])
                pair.append((t, tok))
            res[idx] = pair
        return res

    P.mute = "mod" in skip
    A.reset(A_PERSIST)
    cs = A.alloc("cs", [128, 16], F32)
    ss = A.alloc("ss", [128, 16], F32)
    sbl = A.alloc("sbl", [128, 16, 128], BF16)
    P.dma("sync", cs[:], cT, writes=["cs"])
    P.op("scalar", lambda e: e.activation(ss[:], cs[:], AF.Silu), reads=["cs"], writes=["ss"])
    P.op("vector", lambda e: e.tensor_copy(sbl[:], ss[:].unsqueeze(2).to_broadcast([128, 16, 128])), reads=["ss"], writes=["sbl"])
    wad = [A.alloc("wad%d" % i, [128, 8, 512], BF16) for i in range(2)]
    bt = [A.alloc("bt%d" % i, [128, 512], F32) for i in range(2)]
    gn = [A.alloc("gnm%d" % i, [128, D], F32) for i in range(2)]
    mo = [A.alloc("mo%d" % i, [128, 512], F32) for i in range(4)]
    it = 0
    for L in range(2):
        P.dma("sync", gn[0][:], norm_mix[L].partition_broadcast(128), writes=["gn0"])
        P.dma("sync", gn[1][:], norm_ffn[L].partition_broadcast(128), writes=["gn1"])
        for cb in range(12):
            s = it % 2
            it += 1
            idx = cb // 2
            c0 = (cb % 2) * 512
            P.dma("gpsimd", wad[s][:], w_ada[L, :, cb * 512:(cb + 1) * 512].rearrange("(k p) f -> p k f", p=128), writes=["wad%d" % s])
            P.dma("sync", bt[s][:], b_ada[L, cb * 512:(cb + 1) * 512].partition_broadcast(128), writes=["bt%d" % s])
            for w in range(2):
                bank = 2 * s + w
                for k in range(8):
                    P.op("tensor", lambda e, k=k, w=w, s=s, bank=bank: e.matmul(ps[bank][:], sbl[:, w * 8 + k, :], wad[s][:, k, :], start=(k == 0), stop=(k == 7)),
                         reads=["sbl", "wad%d" % s], writes=[PS[bank]])
                m = mo[bank]
                mtok = "mo%d" % bank
                P.op("vector", lambda e, m=m, bank=bank, s=s: e.tensor_tensor(m[:], ps[bank][:], bt[s][:], ALU.add),
                     reads=[PS[bank], "bt%d" % s], writes=[mtok])
                if idx in (1, 4):
                    g = gn[0] if idx == 1 else gn[1]
                    gtok = "gn0" if idx == 1 else "gn1"
                    P.op("vector", lambda e, m=m, g=g, c0=c0: e.scalar_tensor_tensor(m[:], m[:], 1.0, g[:, c0:c0 + 512], ALU.add, ALU.mult),
                         reads=[mtok, gtok], writes=[mtok])
                P.dma("sync", modd[L, w, idx, :, c0:c0 + 512], m[:], reads=[mtok], writes=[("modd", L, w, idx, c0)])
    P.barrier()
    if done("mod"):
        return finish()

    def layer(L, x_src, x_mix, x_out, need_ctx, moe):
        ngrp = 9
        P.mute = "p1" in skip
        A.reset(A_PERSIST)
        md = load_mod(L, [0, 1])
        _, _, normT = make_normT("p1", md[1], md[0], 0)
        hTt = [A.alloc("hTt%d" % i, [128, 8, 512], BF16) for i in range(2)]
        for g, (t0, n) in enumerate(GROUPS):
            s = g % 2
            normT(x_src, t0, n // 128, hTt[s], "hTt%d" % s, 0 if g < 8 else 1)
            P.dma("sync", hT_d[:, :, t0:t0 + n], hTt[s][:, :, 0:n], reads=["hTt%d" % s], writes=[("hT", g)])
        P.barrier()
        if done("p1_%d" % L):
            return True

        P.mute = "p2" in skip
        A.reset(A_PERSIST)
        Wa = A.alloc("Wa", [128, 8, 1024], BF16)
        for cbk in range(2):
            P.dma("gpsimd", Wa[:, :, cbk * 512:(cbk + 1) * 512], w_in[L, :, 1536 + cbk * 512:1536 + (cbk + 1) * 512].rearrange("(k p) f -> p k f", p=128), writes=[("Wa", cbk)])
        QT = A.alloc("QT", [128, 4, NT], BF16)
        KT = A.alloc("KT", [128, 2, NT], BF16)
        Vres = A.alloc("Vres", [128, NTILE, 256], BF16)
        acos = A.alloc("acos", [128, NTILE, 64], F32)
        asin = A.alloc("asin", [128, NTILE, 64], F32)
        P.dma("sync", acos[:], k_acos, writes=["acos"])
        P.dma("sync", asin[:], k_asin, writes=["asin"])
        gq = A.alloc("gq", [128, 128], F32)
        gk = A.alloc("gk", [128, 128], F32)
        P.dma("sync", gq[:], attn_qn[L].partition_broadcast(128), writes=["gq"])
        P.dma("sync", gk[:], attn_kn[L].partition_broadcast(128), writes=["gk"])
        m_prep = A.mark()
        hg = [A.alloc("hg%d" % i, [128, 8, 512], BF16) for i in range(2)]
        sqs = [A.alloc("sq%d" % i, [128, 768], F32) for i in range(2)]
        st6s = [A.alloc("st6_%d" % i, [128, 6], F32) for i in range(2)]
        st6bs = [A.alloc("st6b%d" % i, [128, 6], F32) for i in range(2)]
        rs6s = [A.alloc("rs6_%d" % i, [128, 6], F32) for i in range(2)]
        qns = [A.alloc("qn%d" % i, [128, 6, 128], F32) for i in range(2)]
        tas = [A.alloc("ta%d" % i, [128, 6, 64], F32) for i in range(2)]
        tbs = [A.alloc("tb%d" % i, [128, 6, 64], F32) for i in range(2)]
        tcs = [A.alloc("tc%d" % i, [128, 6, 64], F32) for i in range(2)]
        tds = [A.alloc("td%d" % i, [128, 6, 64], F32) for i in range(2)]
        qrs = [A.alloc("qr%d" % i, [128, 6, 128], BF16) for i in range(2)]
        prev_gen = [None]
        for g, (t0, n) in enumerate(GROUPS):
            s = g % 2
            P.dma("sync", hg[s][:, :, 0:n], hT_d[:, :, t0:t0 + n], reads=[("hT", g)], writes=["hg%d" % s])
            for j in range(n // 128):
                def tile_body(g=g, s=s, j=j, ti=(t0 // 128) + j):
                    u = ti % 2
                    sfx = "_%d" % u
                    sq, st6, st6b, rs6, qn = sqs[u], st6s[u], st6bs[u], rs6s[u], qns[u]
                    ta, tb, tc_, td, qr = tas[u], tbs[u], tcs[u], tds[u], qrs[u]
                    bk0, bk1, bk2 = (0, 1, 2) if u == 0 else (3, 4, 5)
                    for k in range(8):
                        P.op("tensor", lambda e, k=k, s=s, j=j: e.matmul(ps[bk0][:], hg[s][:, k, j * 128:(j + 1) * 128], Wa[:, k, 0:512], start=(k == 0), stop=(k == 7)),
                             reads=["hg%d" % s, ("Wa", 0)], writes=[PS[bk0]])
                    for k in range(8):
                        P.op("tensor", lambda e, k=k, s=s, j=j: e.matmul(ps[bk1][:], hg[s][:, k, j * 128:(j + 1) * 128], Wa[:, k, 512:1024], start=(k == 0), stop=(k == 7)),
                             reads=["hg%d" % s, ("Wa", 1)], writes=[PS[bk1]])
                    P.op("scalar", lambda e: e.activation(sq[:, 0:512], ps[bk0][:], AF.Square), reads=[PS[bk0]], writes=["sq" + sfx])
                    P.op("scalar", lambda e: e.activation(sq[:, 512:768], ps[bk1][:, 0:256], AF.Square), reads=[PS[bk1]], writes=["sqb" + sfx])
                    P.op("vector", lambda e: e.reduce_sum(st6[:], sq[:].rearrange("p (h d) -> p h d", d=128), axis=AX.X),
                         reads=["sq" + sfx, "sqb" + sfx], writes=["st6" + sfx])
                    P.op("scalar", lambda e: e.activation(st6b[:], st6[:], AF.Sqrt, bias=NORM_EPS, scale=1.0 / 128), reads=["st6" + sfx], writes=["st6b" + sfx])
                    P.op("vector", lambda e: e.reciprocal(rs6[:], st6b[:]), reads=["st6b" + sfx], writes=["rs6" + sfx])
                    P.op("vector", lambda e: e.tensor_tensor(qn[:, 0:4, :], ps[bk0][:].rearrange("p (h d) -> p h d", d=128),
                                                             rs6[:, 0:4].unsqueeze(2).to_broadcast([128, 4, 128]), ALU.mult),
                         reads=[PS[bk0], "rs6" + sfx], writes=["qn_q" + sfx])
                    P.op("vector", lambda e: e.tensor_tensor(qn[:, 4:6, :], ps[bk1][:, 0:256].rearrange("p (h d) -> p h d", d=128),
                                                             rs6[:, 4:6].unsqueeze(2).to_broadcast([128, 2, 128]), ALU.mult),
                         reads=[PS[bk1], "rs6" + sfx], writes=["qn_k" + sfx])
                    P.op("scalar", lambda e, ti=ti: e.copy(Vres[:, ti, :], ps[bk1][:, 256:512]), reads=[PS[bk1]], writes=[("Vres", ti)])
                    P.op("gpsimd", lambda e: e.tensor_tensor(qn[:, 0:4, :], qn[:, 0:4, :], gq[:].unsqueeze(1).to_broadcast([128, 4, 128]), ALU.mult),
                         reads=["qn_q" + sfx, "gq"], writes=["qn_q" + sfx])
                    P.op("gpsimd", lambda e: e.tensor_tensor(qn[:, 4:6, :], qn[:, 4:6, :], gk[:].unsqueeze(1).to_broadcast([128, 2, 128]), ALU.mult),
                         reads=["qn_k" + sfx, "gk"], writes=["qn_k" + sfx])
                    yield
                    x0 = qn[:, :, 0::2]
                    x1 = qn[:, :, 1::2]
                    cb_ = acos[:, ti, :].unsqueeze(1).to_broadcast([128, 6, 64])
                    sb_ = asin[:, ti, :].unsqueeze(1).to_broadcast([128, 6, 64])
                    P.op("vector", lambda e, x0=x0, cb_=cb_: e.tensor_tensor(ta[:], x0, cb_, ALU.mult), reads=["qn_q" + sfx, "qn_k" + sfx, "acos"], writes=["ta" + sfx])
                    P.op("gpsimd", lambda e, x1=x1, sb_=sb_: e.tensor_tensor(tb[:], x1, sb_, ALU.mult), reads=["qn_q" + sfx, "qn_k" + sfx, "asin"], writes=["tb" + sfx])
                    P.op("vector", lambda e, x0=x0, sb_=sb_: e.tensor_tensor(tc_[:], x0, sb_, ALU.mult), reads=["qn_q" + sfx, "qn_k" + sfx, "asin"], writes=["tc" + sfx])
                    P.op("gpsimd", lambda e, x1=x1, cb_=cb_: e.tensor_tensor(td[:], x1, cb_, ALU.mult), reads=["qn_q" + sfx, "qn_k" + sfx, "acos"], writes=["td" + sfx])
                    P.op("vector", lambda e: e.tensor_tensor(qr[:, :, 0::2], ta[:], tb[:], ALU.subtract), reads=["ta" + sfx, "tb" + sfx], writes=["qr0" + sfx])
                    P.op("gpsimd", lambda e: e.tensor_tensor(qr[:, :, 1::2], tc_[:], td[:], ALU.add), reads=["tc" + sfx, "td" + sfx], writes=["qr1" + sfx])
                    pbA = ps[bk2][:].bitcast(BF16)
                    for h in range(6):
                        P.op("tensor", lambda e, h=h: e.transpose(pbA[:, h * 128:(h + 1) * 128], qr[:, h, :], ident[:]),
                             reads=["qr0" + sfx, "qr1" + sfx, "ident"], writes=[PS[bk2]])
                    P.op("scalar", lambda e, ti=ti: e.copy(QT[:, :, ti * 128:(ti + 1) * 128], pbA[:, 0:512].rearrange("p (h t) -> p h t", h=4)),
                         reads=[PS[bk2]], writes=[("QT", ti)])
                    P.op("scalar", lambda e, ti=ti: e.copy(KT[:, :, ti * 128:(ti + 1) * 128], pbA[:, 512:768].rearrange("p (h t) -> p h t", h=2)),
                         reads=[PS[bk2]], writes=[("KT", ti)])
                gen_ = tile_body()
                next(gen_)
                if prev_gen[0] is not None:
                    for _ in prev_gen[0]:
                        pass
                prev_gen[0] = gen_
        for _ in prev_gen[0]:
            pass
        P.barrier()
        if done("p2a_%d" % L):
            return True
        A.reset(m_prep)
        pT = [A.alloc("pT%d" % i, [128, 512], BF16) for i in range(3)]
        rden = A.alloc("rden", [128, 512], F32)
        yo = [A.alloc("yo%d" % i, [128, 512], BF16) for i in range(2)]
        scale = float(128 ** -0.5)
        qgroups = list(range(8)) + ([8] if need_ctx else [])
        blocks = []
        oc = 0
        for g in qgroups:
            t0, n = GROUPS[g]
            keys = list(range(NTILE)) if g < 8 else [32, 33]
            for h in range(4):
                for ji, j in enumerate(keys):
                    blocks.append((g, t0, n, h, ji, j, len(keys), oc))
                oc += 1
        LOOK = 2

        def emit_score(bi):
            g, t0, n, h, ji, j, nk, oc_ = blocks[bi]
            sbk = bi % 3
            kvh = h // 2
            P.op("tensor", lambda e: e.matmul(ps[sbk][:, 0:n], KT[:, kvh, j * 128:(j + 1) * 128], QT[:, h, t0:t0 + n], start=True, stop=True),
                 reads=[], writes=[PS[sbk]])
            P.op("scalar", lambda e: e.activation(pT[sbk][:, 0:n], ps[sbk][:, 0:n], AF.Exp, scale=scale),
                 reads=[PS[sbk]], writes=["pT%d" % sbk])

        def emit_pv(bi):
            g, t0, n, h, ji, j, nk, oc_ = blocks[bi]
            sbk = bi % 3
            kvh = h // 2
            ob = 4 + (oc_ % 2) * 2
            db = ob + 1
            pt = pT[sbk]
            P.op("tensor", lambda e: e.matmul(ps[ob][:, 0:n], Vres[:, j, kvh * 128:(kvh + 1) * 128], pt[:, 0:n], start=(ji == 0), stop=(ji == nk - 1)),
                 reads=["pT%d" % sbk], writes=[PS[ob]])
            P.op("tensor", lambda e: e.matmul(ps[db][:, 0:n], ones_bf[:], pt[:, 0:n], start=(ji == 0), stop=(ji == nk - 1)),
                 reads=["pT%d" % sbk], writes=[PS[db]])
            if ji == nk - 1:
                y_o = yo[oc_ % 2]
                ytok = "yo%d" % (oc_ % 2)
                P.op("vector", lambda e: e.reciprocal(rden[:, 0:n], ps[db][:, 0:n]), reads=[PS[db]], writes=["rden"])
                P.op("vector", lambda e: e.tensor_tensor(y_o[:, 0:n], ps[ob][:, 0:n], rden[:, 0:n], ALU.mult),
                     reads=[PS[ob], "rden"], writes=[ytok])
                P.dma("sync", yT_d[1, :, h, t0:t0 + n], y_o[:, 0:n], reads=[ytok], writes=[("yT1", g, h)])

        for bi in range(len(blocks) + LOOK):
            if bi < len(blocks):
                emit_score(bi)
            if bi - LOOK >= 0:
                emit_pv(bi - LOOK)
        P.barrier()
        if done("p2_%d" % L):
            return True
        P.mute = "p3" in skip
        A.reset(A_PERSIST)
        Wr = A.alloc("Wr", [128, 8, 1536], BF16)
        m_wr = A.mark()
        for cbk in range(3):
            P.dma("gpsimd", Wr[:, :, cbk * 512:(cbk + 1) * 512], w_in[L, :, cbk * 512:(cbk + 1) * 512].rearrange("(k p) f -> p k f", p=128), writes=[("Wr", cbk)])
        QTr = A.alloc("QTr", [128, 2, NT], BF16)
        KTr = A.alloc("KTr", [128, 2, NT], BF16)
        Kres = A.alloc("Kres", [128, NTILE, 256], BF16)
        Vr = A.alloc("Vr", [128, NTILE, 512], BF16)
        SG = A.alloc("SG", [128, NTILE, 512], BF16)
        rtab = A.alloc("rtab", [128, 6, 128], F32)
        pcol = A.alloc("pcol", [128, 2], F32)
        rd8 = A.alloc("rd8", [128, 8], F32)
        lg8 = A.alloc("lg8", [128, 8], F32)
        lgp = A.alloc("lgp", [128, 4], F32)
        gch = A.alloc("gch", [128, 4], F32)
        Dcomb = A.alloc("Dcomb", [128, 4, 128], F32)
        XI = A.alloc("XI", [128, 4, 128], F32)
        ZZ = A.alloc("ZZ", [128, 2, 256], F32)
        XIm = A.alloc("XIm", [128, 6, 2, 128], F32)
        mk = A.alloc("mk", [128, 2], F32)
        P.op("vector", lambda e: e.memset(mk[:], 0.0), writes=["mk"])
        P.op("vector", lambda e: e.memset(mk[0:64, 0:1], 1.0), reads=["mk"], writes=["mk"])
        P.op("vector", lambda e: e.memset(mk[64:128, 1:2], 1.0), reads=["mk"], writes=["mk"])
        gnw = A.alloc("gnw", [128, 512], F32)
        t1 = A.alloc("t1", [128, 128], F32)
        t2 = A.alloc("t2", [128, 128], F32)
        P.dma("sync", rtab[:], k_rtab, writes=["rtab"])
        P.dma("sync", pcol[:], k_pcol, writes=["pcol"])
        P.dma("sync", rd8[:], ret_decay[L].partition_broadcast(128), writes=["rd8"])
        P.dma("sync", gnw[:], ret_gn[L].partition_broadcast(128), writes=["gnw"])
        P.op("scalar", lambda e: e.activation(lg8[:], rd8[:], AF.Exp), reads=["rd8"], writes=["lg8a"])
        P.op("vector", lambda e: e.tensor_scalar(lg8[:], lg8[:], -1.0, None, ALU.mult), reads=["lg8a"], writes=["lg8"])
        for j in range(2):
            for dr in range(2):
                P.op("vector", lambda e, j=j, dr=dr: e.tensor_copy(lgp[0:64, dr * 2 + j:dr * 2 + j + 1], lg8[0:64, dr * 4 + 2 * j:dr * 4 + 2 * j + 1]), reads=["lg8"], writes=[("lgp", j, dr, 0)])
                P.op("vector", lambda e, j=j, dr=dr: e.tensor_copy(lgp[64:128, dr * 2 + j:dr * 2 + j + 1], lg8[64:128, dr * 4 + 2 * j + 1:dr * 4 + 2 * j + 2]), reads=["lg8"], writes=[("lgp", j, dr, 1)])
        LGP = [("lgp", j, dr, hh) for j in range(2) for dr in range(2) for hh in range(2)]
        P.op("scalar", lambda e: e.activation(gch[:], lgp[:], AF.Exp, scale=128.0), reads=LGP, writes=["gch"])
        for h in range(4):
            P.op("scalar", lambda e, h=h: e.activation(t1[:], rtab[:, 0, :], AF.Exp, scale=lg8[:, h:h + 1]), reads=["rtab", "lg8"], writes=["t1"])
            P.op("vector", lambda e: e.tensor_tensor(t1[:], t1[:], rtab[:, 1, :], ALU.mult), reads=["t1", "rtab"], writes=["t1"])
            P.op("scalar", lambda e, h=h: e.activation(t2[:], rtab[:, 2, :], AF.Exp, scale=lg8[:, 4 + h:5 + h]), reads=["rtab", "lg8"], writes=["t2"])
            P.op("vector", lambda e: e.tensor_tensor(t2[:], t2[:], rtab[:, 3, :], ALU.mult), reads=["t2", "rtab"], writes=["t2"])
            P.op("vector", lambda e, h=h: e.tensor_tensor(Dcomb[:, h, :], t1[:], t2[:], ALU.add), reads=["t1", "t2"], writes=[("Dcomb", h)])
            for dr in range(2):
                P.op("scalar", lambda e, h=h, dr=dr: e.activation(ZZ[:, dr, h * 64:(h + 1) * 64], lg8[:, dr * 4 + h:dr * 4 + h + 1].to_broadcast([128, 64]), AF.Exp, scale=pcol[:, dr:dr + 1]),
                     reads=["lg8", "pcol"], writes=[("ZZ", dr, h)])
        for j in range(2):
            for dr in range(2):
                P.op("scalar", lambda e, j=j, dr=dr: e.activation(XI[:, dr * 2 + j, :], rtab[:, 4 + dr, :], AF.Exp, scale=lgp[:, dr * 2 + j:dr * 2 + j + 1]),
                     reads=["rtab"] + LGP, writes=[("XI", dr, j)])
        XIT = [("XI", dr, j) for dr in range(2) for j in range(2)]
        for hh in range(2):
            for dr in range(2):
                P.op("vector", lambda e, hh=hh, dr=dr: e.tensor_scalar(XIm[:, 2 * dr + hh], XI[:, dr * 2:dr * 2 + 2, :], mk[:, hh:hh + 1], None, ALU.mult), reads=XIT + ["mk"], writes=[("XIm", 2 * dr + hh)])
            P.op("vector", lambda e, hh=hh: e.tensor_copy(XIm[:, 4 + hh].rearrange("p j l -> p (j l)"), mk[:, hh:hh + 1].to_broadcast([128, 256])), reads=["mk"], writes=[("XIm", 4 + hh)])
        if done("p3a_%d" % L):
            return True
        m_rprep = A.mark()
        hgrs = [A.alloc("hgr%d" % i, [128, 8, 512], BF16) for i in range(2)]
        rt4 = [A.alloc("rt4_%d" % i, [128, 4, 32], F32) for i in range(2)]
        ras = [A.alloc("ra%d" % i, [128, 8, 32], F32) for i in range(2)]
        rbs = [A.alloc("rb%d" % i, [128, 8, 32], F32) for i in range(2)]
        rcs = [A.alloc("rc%d" % i, [128, 8, 32], F32) for i in range(2)]
        rdds = [A.alloc("rdd%d" % i, [128, 8, 32], F32) for i in range(2)]
        qrrs = [A.alloc("qrr%d" % i, [128, 256], BF16) for i in range(2)]
        prev_r = [None]

        def r_load(g_):
            t0_, n_ = GROUPS[g_]
            P.dma("sync", hgrs[g_ % 2][:, :, 0:n_], hT_d[:, :, t0_:t0_ + n_], reads=[("hT", g_)], writes=["hgr%d" % (g_ % 2)])

        r_load(0)
        for g, (t0, n) in enumerate(GROUPS):
            if g + 1 < len(GROUPS):
                r_load(g + 1)
            for j in range(n // 128):
                def r_tile(g=g, j=j, ti=(t0 // 128) + j):
                    hgr = hgrs[g % 2]
                    hgtok = "hgr%d" % (g % 2)
                    u = ti % 2
                    s = u
                    sfx = "_%d" % u
                    ra, rb, rc_, rdd, qrr = ras[u], rbs[u], rcs[u], rdds[u], qrrs[u]
                    b0 = 0 if u == 0 else 4
                    P.dma("sync", rt4[s][:], k_rt4[:, ti], writes=["rt4_%d" % s])
                    for blk in range(3):
                        for k in range(8):
                            P.op("tensor", lambda e, k=k, j=j, blk=blk: e.matmul(ps[b0 + blk][:], hgr[:, k, j * 128:(j + 1) * 128], Wr[:, k, blk * 512:(blk + 1) * 512], start=(k == 0), stop=(k == 7)),
                                 reads=[hgtok, ("Wr", blk)], writes=[PS[b0 + blk]])
                    P.op("scalar", lambda e, ti=ti: e.copy(Vr[:, ti, :], ps[b0 + 1][:]), reads=[PS[b0 + 1]], writes=[("Vr", ti)])
                    P.op("scalar", lambda e, ti=ti: e.activation(SG[:, ti, :], ps[b0 + 2][:], AF.Silu), reads=[PS[b0 + 2]], writes=[("SG", ti)])
                    pv = ps[b0][:].rearrange("p (h d) -> p h d", d=64)
                    for hs, (ci, si) in ((slice(0, 4), (0, 1)), (slice(4, 8), (2, 3))):
                        x0 = pv[:, hs, 0::2]
                        x1 = pv[:, hs, 1::2]
                        cb_ = rt4[s][:, ci, :].unsqueeze(1).to_broadcast([128, 4, 32])
                        sb_ = rt4[s][:, si, :].unsqueeze(1).to_broadcast([128, 4, 32])
                        tk = "q" if ci == 0 else "k"
                        P.op("vector", lambda e, x0=x0, cb_=cb_, hs=hs: e.tensor_tensor(ra[:, hs, :], x0, cb_, ALU.mult), reads=[PS[b0], "rt4_%d" % s], writes=["ra" + tk + sfx])
                        P.op("vector", lambda e, x1=x1, sb_=sb_, hs=hs: e.tensor_tensor(rb[:, hs, :], x1, sb_, ALU.mult), reads=[PS[b0], "rt4_%d" % s], writes=["rb" + tk + sfx])
                        P.op("vector", lambda e, x0=x0, sb_=sb_, hs=hs: e.tensor_tensor(rc_[:, hs, :], x0, sb_, ALU.mult), reads=[PS[b0], "rt4_%d" % s], writes=["rc" + tk + sfx])
                        P.op("vector", lambda e, x1=x1, cb_=cb_, hs=hs: e.tensor_tensor(rdd[:, hs, :], x1, cb_, ALU.mult), reads=[PS[b0], "rt4_%d" % s], writes=["rd" + tk + sfx])
                    yield
                    qv = qrr[:].rearrange("p (h d) -> p h d", d=64)
                    kv_ = Kres[:, ti, :].rearrange("p (h d) -> p h d", d=64)
                    P.op("gpsimd", lambda e, qv=qv: e.tensor_tensor(qv[:, :, 0::2], ra[:, 0:4, :], rb[:, 0:4, :], ALU.subtract), reads=["raq" + sfx, "rbq" + sfx], writes=["qrr0" + sfx])
                    P.op("gpsimd", lambda e, qv=qv: e.tensor_tensor(qv[:, :, 1::2], rc_[:, 0:4, :], rdd[:, 0:4, :], ALU.add), reads=["rcq" + sfx, "rdq" + sfx], writes=["qrr1" + sfx])
                    P.op("gpsimd", lambda e, kv_=kv_: e.tensor_tensor(kv_[:, :, 0::2], ra[:, 4:8, :], rb[:, 4:8, :], ALU.subtract), reads=["rak" + sfx, "rbk" + sfx], writes=[("Kres0", ti)])
                    P.op("gpsimd", lambda e, kv_=kv_: e.tensor_tensor(kv_[:, :, 1::2], rc_[:, 4:8, :], rdd[:, 4:8, :], ALU.add), reads=["rck" + sfx, "rdk" + sfx], writes=[("Kres1", ti)])
                    pbB = ps[b0 + 3][:].bitcast(BF16)
                    for jj in range(2):
                        P.op("tensor", lambda e, jj=jj: e.transpose(pbB[:, jj * 128:(jj + 1) * 128], qrr[:, jj * 128:(jj + 1) * 128], ident[:]), reads=["qrr0" + sfx, "qrr1" + sfx, "ident"], writes=[PS[b0 + 3]])
                    for jj in range(2):
                        P.op("tensor", lambda e, jj=jj, ti=ti: e.transpose(pbB[:, 256 + jj * 128:256 + (jj + 1) * 128], Kres[:, ti, jj * 128:(jj + 1) * 128], ident[:]), reads=[("Kres0", ti), ("Kres1", ti), "ident"], writes=[PS[b0 + 3]])
                    P.op("scalar", lambda e, ti=ti: e.copy(QTr[:, :, ti * 128:(ti + 1) * 128], pbB[:, 0:256].rearrange("p (h t) -> p h t", h=2)), reads=[PS[b0 + 3]], writes=[("QTr", ti)])
                    P.op("scalar", lambda e, ti=ti: e.copy(KTr[:, :, ti * 128:(ti + 1) * 128], pbB[:, 256:512].rearrange("p (h t) -> p h t", h=2)), reads=[PS[b0 + 3]], writes=[("KTr", ti)])
                gen_ = r_tile()
                next(gen_)
                if prev_r[0] is not None:
                    for _ in prev_r[0]:
                        pass
                prev_r[0] = gen_
        for _ in prev_r[0]:
            pass
        P.barrier()
        if done("p3b_%d" % L):
            return True
        A.reset(m_rprep)
        Sb_all = nc.alloc_sbuf_tensor_at("Sb_all_%d" % L, [128, NTILE, 2, 128], BF16, offset=A_PERSIST)
        Sf = A.alloc("Sf", [128, 2, 128], F32)
        Sb = A.alloc("Sb", [128, 2, 128], F32)
        Sfb = [A.alloc("Sfb%d" % i, [128, 2, 128], BF16) for i in range(2)]
        kz = [A.alloc("kz%d" % i, [128, 256], BF16) for i in range(2)]
        PTs = [A.alloc("PT%d" % i, [128, 512], BF16) for i in range(2)]
        qms = [[A.alloc("qm%d_%d" % (i, q), [128, 2, 128], BF16) for i in range(6)] for q in range(2)]
        sqrs = [A.alloc("sqr%d" % i, [128, 512], F32) for i in range(2)]
        onrs = [A.alloc("onr%d" % i, [128, 512], F32) for i in range(2)]
        osbs = [A.alloc("osb%d" % i, [128, 512], F32) for i in range(2)]
        gss = [A.alloc("gs%d" % i, [128, 24], F32) for i in range(2)]
        yrbs = [A.alloc("yrb%d" % i, [128, 512], BF16) for i in range(2)]
        yrT = [A.alloc("yrT%d" % i, [128, 4, 512], BF16) for i in range(2)]
        P.op("vector", lambda e: e.memset(Sf[:], 0.0), writes=["Sf"])
        P.op("vector", lambda e: e.memset(Sb[:], 0.0), writes=["Sb"])

        def state_update(S, Stok, dr, c, bank):
            kzt = kz[dr]
            P.op("gpsimd", lambda e: e.tensor_tensor(kzt[:], Kres[:, c, :], ZZ[:, dr, :], ALU.mult), reads=[], writes=["kz%d" % dr])
            for h in range(4):
                P.op("tensor", lambda e, h=h: e.matmul(ps[bank][:, h * 128:(h + 1) * 128], kzt[:, (h // 2) * 128:(h // 2 + 1) * 128], Vr[:, c, h * 128:(h + 1) * 128], start=True, stop=True),
                     reads=["kz%d" % dr], writes=[PS[bank]])
            for j in range(2):
                for hh in range(2):
                    r0 = hh * 64
                    h = 2 * j + hh
                    P.op("vector", lambda e, j=j, r0=r0, h=h: e.scalar_tensor_tensor(S[r0:r0 + 64, j, :], S[r0:r0 + 64, j, :], gch[r0:r0 + 64, dr * 2 + j:dr * 2 + j + 1],
                                                                                   ps[bank][r0:r0 + 64, h * 128:(h + 1) * 128], ALU.mult, ALU.add),
                         reads=[PS[bank], Stok], writes=[Stok])

        order_b = [33, 32] + list(range(31, -1, -1))
        order_f = [32, 33] + list(range(32))
        for c in order_b:
            P.op("vector", lambda e, c=c: e.tensor_copy(Sb_all[:, c], Sb[:]), reads=["Sb"], writes=[("Sb_all", c)])
            state_update(Sb, "Sb", 1, c, 4)
        if done("p3c_%d" % L):
            return True
        yi = 0
        yi_box = [0]
        prev_fw = [None]
        for ci, c in enumerate(order_f):
            def fwd_chunk(ci=ci, c=c):
                u = ci % 2
                sfx = "_%d" % u
                PT, qm, sqr, onr, osb, gs, yrb = PTs[u], qms[u], sqrs[u], onrs[u], osbs[u], gss[u], yrbs[u]
                bR, bO, bT = (0, 1, 2) if u == 0 else (4, 5, 6)
                sfb = Sfb[ci % 2]
                sftok = "Sfb%d" % (ci % 2)
                P.op("vector", lambda e, sfb=sfb: e.tensor_copy(sfb[:], Sf[:]), reads=["Sf"], writes=[sftok])
                emit_out = (c < 32) or need_ctx
                if emit_out:
                    for idx in range(6):
                        eng = "vector" if idx % 2 == 0 else "gpsimd"
                        P.op(eng, lambda e, idx=idx, c=c: e.tensor_tensor(qm[idx][:], QTr[:, :, c * 128:(c + 1) * 128], XIm[:, idx], ALU.mult), reads=[], writes=[("qm", idx, u)])
                    for h in range(4):
                        j, hh = h // 2, h % 2
                        P.op("tensor", lambda e, h=h, j=j, hh=hh, c=c: e.matmul(ps[bR][:, h * 128:(h + 1) * 128], KTr[:, j, c * 128:(c + 1) * 128], qm[4 + hh][:, j, :], start=True, stop=True),
                             reads=[("qm", 4 + hh, u)], writes=[PS[bR]])
                    P.op("vector", lambda e: e.tensor_tensor(PT[:], ps[bR][:], Dcomb[:].rearrange("p h l -> p (h l)"), ALU.mult), reads=[PS[bR]], writes=["PT" + sfx])
                    for h in range(4):
                        j, hh = h // 2, h % 2
                        P.op("tensor", lambda e, h=h, c=c: e.matmul(ps[bO][:, h * 128:(h + 1) * 128], PT[:, h * 128:(h + 1) * 128], Vr[:, c, h * 128:(h + 1) * 128], start=True, stop=False),
                             reads=["PT" + sfx], writes=[PS[bO]])
                        P.op("tensor", lambda e, h=h, j=j, hh=hh, sfb=sfb: e.matmul(ps[bO][:, h * 128:(h + 1) * 128], qm[hh][:, j, :], sfb[:, j, :], start=False, stop=False),
                             reads=[("qm", hh, u), sftok], writes=[PS[bO]])
                        P.op("tensor", lambda e, h=h, j=j, hh=hh, c=c: e.matmul(ps[bO][:, h * 128:(h + 1) * 128], qm[2 + hh][:, j, :], Sb_all[:, c, j, :], start=False, stop=True),
                             reads=[("qm", 2 + hh, u), ("Sb_all", c)], writes=[PS[bO]])
                    state_update(Sf, "Sf", 0, c, 3 if u == 0 else 7)
                    yield
                    yi = yi_box[0]
                    ov = ps[bO][:].rearrange("p (h e) -> p h e", e=128)
                    P.op("scalar", lambda e: e.activation(sqr[:], ps[bO][:], AF.Square), reads=[PS[bO]], writes=["sqr" + sfx])
                    P.op("scalar", lambda e: e.copy(osb[:], ps[bO][:]), reads=[PS[bO]], writes=["osb" + sfx])
                    ov = osb[:].rearrange("p (h e) -> p h e", e=128)
                    P.op("vector", lambda e, ov=ov: e.reduce_sum(gs[:, 0:4], ov, axis=AX.X), reads=["osb" + sfx], writes=["gs0" + sfx])
                    P.op("vector", lambda e: e.reduce_sum(gs[:, 4:8], sqr[:].rearrange("p (h e) -> p h e", e=128), axis=AX.X), reads=["sqr" + sfx], writes=["gs1" + sfx])
                    P.op("vector", lambda e: e.tensor_scalar(gs[:, 8:12], gs[:, 0:4], 1.0 / 128, None, ALU.mult), reads=["gs0" + sfx], writes=["gs2" + sfx])
                    P.op("vector", lambda e: e.tensor_tensor(gs[:, 12:16], gs[:, 8:12], gs[:, 8:12], ALU.mult), reads=["gs2" + sfx], writes=["gs3" + sfx])
                    P.op("vector", lambda e: e.scalar_tensor_tensor(gs[:, 16:20], gs[:, 4:8], 1.0 / 128, gs[:, 12:16], ALU.mult, ALU.subtract), reads=["gs1" + sfx, "gs3" + sfx], writes=["gs4" + sfx])
                    P.op("scalar", lambda e: e.activation(gs[:, 20:24], gs[:, 16:20], AF.Sqrt, bias=GN_EPS, scale=1.0), reads=["gs4" + sfx], writes=["gs5" + sfx])
                    P.op("vector", lambda e: e.reciprocal(gs[:, 20:24], gs[:, 20:24]), reads=["gs5" + sfx], writes=["gs5" + sfx])
                    onv = onr[:].rearrange("p (h e) -> p h e", e=128)
                    P.op("vector", lambda e, ov=ov, onv=onv: e.tensor_tensor(onv, ov, gs[:, 8:12].unsqueeze(2).to_broadcast([128, 4, 128]), ALU.subtract), reads=["osb" + sfx, "gs2" + sfx], writes=["onr" + sfx])
                    P.op("gpsimd", lambda e, onv=onv: e.tensor_tensor(onv, onv, gs[:, 20:24].unsqueeze(2).to_broadcast([128, 4, 128]), ALU.mult), reads=["onr" + sfx, "gs5" + sfx], writes=["onr" + sfx])
                    P.op("gpsimd", lambda e: e.tensor_tensor(onr[:], onr[:], gnw[:], ALU.mult), reads=["onr" + sfx], writes=["onr" + sfx])
                    P.op("vector", lambda e, c=c: e.tensor_tensor(yrb[:], onr[:], SG[:, c, :], ALU.mult), reads=["onr" + sfx], writes=["yrb" + sfx])
                    pbC = ps[bT][:].bitcast(BF16)
                    for h in range(4):
                        P.op("tensor", lambda e, h=h: e.transpose(pbC[:, h * 128:(h + 1) * 128], yrb[:, h * 128:(h + 1) * 128], ident[:]), reads=["yrb" + sfx], writes=[PS[bT]])
                    g = c // 4 if c < 32 else 8
                    t0, n = GROUPS[g]
                    col = (c * 128 - t0)
                    yt = yrT[yi % 2]
                    ytok = "yrT%d" % (yi % 2)
                    P.op("scalar", lambda e, yt=yt, col=col: e.copy(yt[:, :, col:col + 128], pbC[:, 0:512].rearrange("p (h t) -> p h t", h=4)), reads=[PS[bT]], writes=[(ytok, col)])
                    if col + 128 == n:
                        P.dma("sync", yT_d[0, :, :, t0:t0 + n], yt[:, :, 0:n], reads=[(ytok, cc) for cc in range(0, n, 128)], writes=[("yT0", g)])
                        yi_box[0] += 1
                else:
                    state_update(Sf, "Sf", 0, c, 3 if u == 0 else 7)
                    yield
            gen_ = fwd_chunk()
            next(gen_)
            if prev_fw[0] is not None:
                for _ in prev_fw[0]:
                    pass
            prev_fw[0] = gen_
        for _ in prev_fw[0]:
            pass
        P.barrier()
        if done("p3_%d" % L):
            return True

        P.mute = "p4" in skip
        A.reset(A_PERSIST)
        LP = 16 + SEQ + 16 + CTX + 16
        OFFL, OFFC = 16, 16 + SEQ + 16
        Wp = A.alloc("Wp", [128, 8, 512], BF16)
        P.dma("gpsimd", Wp[:], w_in[L, :, 2560:3072].rearrange("(k p) f -> p k f", p=128), writes=["Wp"])
        pw = A.alloc("pw", [128, 4, 128], BF16)
        P.dma("gpsimd", pw[:], pool_w[L].rearrange("g c d -> c g d"), writes=["pw"])
        psc = A.alloc("psc", [128, 4], F32)
        for gi in range(4):
            P.dma("sync", psc[:, gi:gi + 1], pool_scale[L, gi * 128:(gi + 1) * 128].rearrange("(p o) -> p o", o=1), writes=[("psc", gi)])
        pedge = A.alloc("pedge", [128, 4, 2, 8], F32)
        P.dma("sync", pedge[:], k_pedge, writes=["pedge"])
        U = A.alloc("U", [128, 4, LP], F32)
        B1 = A.alloc("B1", [128, LP], F32)
        B2 = A.alloc("B2", [128, LP], F32)
        dT = A.alloc("dT", [128, 4, NT], BF16)
        hgp = [A.alloc("hgp%d" % i, [128, 8, 512], BF16) for i in range(2)]
        yp = [A.alloc("yp%d" % i, [128, 4, 512], BF16) for i in range(2)]
        P.op("gpsimd", lambda e: e.memset(U[:], 0.0), writes=["U"])
        P.op("vector", lambda e: e.memset(B1[:], 0.0), writes=["B1"])
        P.op("vector", lambda e: e.memset(B2[:], 0.0), writes=["B2"])
        for g, (t0, n) in enumerate(GROUPS):
            s = g % 2
            P.dma("sync", hgp[s][:, :, 0:n], hT_d[:, :, t0:t0 + n], reads=[("hT", g)], writes=["hgp%d" % s])
            c0 = OFFL + t0 if g < 8 else OFFC
            for gi in range(4):
                for k in range(8):
                    P.op("tensor", lambda e, k=k, gi=gi, s=s, n=n: e.matmul(ps[gi][:, 0:n], Wp[:, k, gi * 128:(gi + 1) * 128], hgp[s][:, k, 0:n], start=(k == 0), stop=(k == 7)),
                         reads=["hgp%d" % s, "Wp"], writes=[PS[gi]])
                P.op("scalar", lambda e, gi=gi, c0=c0, n=n: e.copy(U[:, gi, c0:c0 + n], ps[gi][:, 0:n]), reads=[PS[gi], "U"], writes=[("U", gi, g)])
        P.barrier()
        for gi, w in enumerate((2, 4, 8, 16)):
            hw = w // 2
            eng = "vector" if gi % 2 == 0 else "gpsimd"
            Ug = U[:, gi, :]
            P.op(eng, lambda e, Ug=Ug: e.tensor_tensor(B1[:, 1:LP], Ug[:, 1:LP], Ug[:, 0:LP - 1], ALU.add), writes=["B1"])
            cur, ctok = B1, "B1"
            if w >= 4:
                P.op(eng, lambda e: e.tensor_tensor(B2[:, 1:LP - 1], B1[:, 0:LP - 2], B1[:, 2:LP], ALU.add), reads=["B1"], writes=["B2"])
                cur, ctok = B2, "B2"
            if w >= 8:
                P.op(eng, lambda e: e.tensor_tensor(B1[:, 2:LP - 2], B2[:, 0:LP - 4], B2[:, 4:LP], ALU.add), reads=["B2"], writes=["B1"])
                cur, ctok = B1, "B1"
            if w >= 16:
                P.op(eng, lambda e: e.tensor_tensor(B2[:, 4:LP - 4], B1[:, 0:LP - 8], B1[:, 8:LP], ALU.add), reads=["B1"], writes=["B2"])
                cur, ctok = B2, "B2"
            for (o0, nn, d0) in ((OFFL, SEQ, 0), (OFFC, CTX, SEQ)):
                P.op(eng, lambda e, cur=cur, o0=o0, gi=gi, hw=hw: e.tensor_tensor(cur[:, o0:o0 + hw], cur[:, o0:o0 + hw], pedge[:, gi, 0, 0:hw], ALU.mult), reads=[ctok, "pedge"], writes=[ctok])
                P.op(eng, lambda e, cur=cur, o0=o0, nn=nn, gi=gi, hw=hw: e.tensor_tensor(cur[:, o0 + nn - hw:o0 + nn], cur[:, o0 + nn - hw:o0 + nn], pedge[:, gi, 1, 0:hw], ALU.mult), reads=[ctok, "pedge"], writes=[ctok])
                P.op("vector", lambda e, cur=cur, o0=o0, nn=nn, d0=d0, gi=gi, w=w, Ug=Ug: e.scalar_tensor_tensor(dT[:, gi, d0:d0 + nn], cur[:, o0:o0 + nn], 1.0 / w, Ug[:, o0:o0 + nn], ALU.mult, ALU.subtract),
                     reads=[ctok], writes=[("dT", gi, d0)])
        P.barrier()
        for g, (t0, n) in enumerate(GROUPS):
            s = g % 2
            for gi in range(4):
                P.op("tensor", lambda e, gi=gi, t0=t0, n=n: e.matmul(ps[gi][:, 0:n], pw[:, gi, :], dT[:, gi, t0:t0 + n], start=True, stop=True), reads=[], writes=[PS[gi]])
                P.op("scalar", lambda e, gi=gi, s=s, n=n: e.activation(yp[s][:, gi, 0:n], ps[gi][:, 0:n], AF.Copy, scale=psc[:, gi:gi + 1]), reads=[PS[gi]], writes=[("yp", s, gi)])
            P.dma("sync", yT_d[2, :, :, t0:t0 + n], yp[s][:, :, 0:n], reads=[("yp", s, gi) for gi in range(4)], writes=[("yT2", g)])
        P.barrier()
        if done("p4_%d" % L):
            return True

        P.mute = "p5" in skip
        A.reset(A_PERSIST)
        Wg = A.alloc("Wg", [128, 8, 3072], BF16)
        Wb = A.alloc("Wb", [128, 12, 1024], BF16)
        for hlf in range(2):
            for cbk in (hlf, 2 + hlf, 4 + hlf):
                P.dma("gpsimd", Wg[:, :, cbk * 512:(cbk + 1) * 512], w_in[L, :, 3072 + cbk * 512:3072 + (cbk + 1) * 512].rearrange("(k p) f -> p k f", p=128), writes=[("Wg", cbk)])
            for b in range(3):
                P.dma("gpsimd", Wb[:, b * 4:(b + 1) * 4, hlf * 512:(hlf + 1) * 512], w_branch[L, b, :, hlf * 512:(hlf + 1) * 512].rearrange("(k p) f -> p k f", p=128), writes=[("Wb", b, hlf)])
        Wo = A.alloc("Wo", [128, 8, 1024], BF16)
        for cbk in range(2):
            P.dma("gpsimd", Wo[:, :, cbk * 512:(cbk + 1) * 512], w_out[L, :, cbk * 512:(cbk + 1) * 512].rearrange("(k p) f -> p k f", p=128), writes=[("Wo", cbk)])
        md = load_mod(L, [2])
        hgms = [A.alloc("hgm%d" % i, [128, 8, 512], BF16) for i in range(2)]
        ygs = [A.alloc("yg%d" % i, [128, 3, 4, 512], BF16) for i in range(2)]
        mixT = A.alloc("mixT", [128, 8, 512], BF16)
        sgm = [A.alloc("sgm%d" % i, [128, 512], F32) for i in range(3)]
        tm = [A.alloc("tm%d" % i, [128, 512], F32) for i in range(3)]
        xtm = [A.alloc("xtm%d" % i, [128, D], F32) for i in range(2)]
        xom = [A.alloc("xom%d" % i, [128, D], F32) for i in range(2)]
        tmo = A.alloc("tmo", [128, 512], F32)
        xi_ = 0
        g5 = [g for g in range(9) if not (g == 8 and not need_ctx)]

        def p5_load(gp):
            g_ = g5[gp]
            t0_, n_ = GROUPS[g_]
            u_ = gp % 2
            P.dma("sync", hgms[u_][:, :, 0:n_], hT_d[:, :, t0_:t0_ + n_], reads=[("hT", g_)], writes=["hgm%d" % u_])
            for b in range(3):
                P.dma("sync", ygs[u_][:, b, :, 0:n_], yT_d[b, :, :, t0_:t0_ + n_], writes=[("yg", b, u_)])

        p5_load(0)
        for gp, g in enumerate(g5):
            t0, n = GROUPS[g]
            which = 0 if g < 8 else 1
            up = gp % 2
            hgm, yg = hgms[up], ygs[up]
            if gp + 1 < len(g5):
                p5_load(gp + 1)
            for jd in range(8):
                for b in range(3):
                    for k in range(8):
                        P.op("tensor", lambda e, k=k, b=b, jd=jd, n=n, hgm=hgm: e.matmul(ps[b][:, 0:n], Wg[:, k, b * 1024 + jd * 128:b * 1024 + (jd + 1) * 128], hgm[:, k, 0:n], start=(k == 0), stop=(k == 7)),
                             reads=["hgm%d" % up, ("Wg", (b * 1024 + jd * 128) // 512)], writes=[PS[b]])
                    for k in range(4):
                        P.op("tensor", lambda e, k=k, b=b, jd=jd, n=n, yg=yg: e.matmul(ps[3 + b][:, 0:n], Wb[:, b * 4 + k, jd * 128:(jd + 1) * 128], yg[:, b, k, 0:n], start=(k == 0), stop=(k == 3)),
                             reads=[("yg", b, up), ("Wb", b, jd // 4)], writes=[PS[3 + b]])
                    P.op("scalar", lambda e, b=b, n=n: e.activation(sgm[b][:, 0:n], ps[b][:, 0:n], AF.Sigmoid), reads=[PS[b]], writes=["sgm%d" % b])
                    P.op("vector", lambda e, b=b, n=n: e.tensor_tensor(tm[b][:, 0:n], ps[3 + b][:, 0:n], sgm[b][:, 0:n], ALU.mult), reads=[PS[3 + b], "sgm%d" % b], writes=["tm%d" % b])
                P.op("gpsimd", lambda e, n=n: e.tensor_tensor(tm[0][:, 0:n], tm[0][:, 0:n], tm[1][:, 0:n], ALU.add), reads=["tm0", "tm1"], writes=["tm0"])
                P.op("gpsimd", lambda e, jd=jd, n=n: e.tensor_tensor(mixT[:, jd, 0:n], tm[0][:, 0:n], tm[2][:, 0:n], ALU.add), reads=["tm0", "tm2"], writes=[("mixT", jd)])
            for j in range(n // 128):
                s = xi_ % 2
                xi_ += 1
                r0 = t0 + j * 128
                P.dma("sync", xtm[s][:], x_src[r0:r0 + 128, :], writes=["xtm%d" % s])
                for half in range(2):
                    for k in range(8):
                        P.op("tensor", lambda e, k=k, j=j, half=half: e.matmul(ps[6 + half][:], mixT[:, k, j * 128:(j + 1) * 128], Wo[:, k, half * 512:(half + 1) * 512], start=(k == 0), stop=(k == 7)),
                             reads=[("mixT", k), ("Wo", half)], writes=[PS[6 + half]])
                    g1 = md[2][which]
                    P.op("vector", lambda e, half=half, g1=g1: e.tensor_tensor(tmo[:], ps[6 + half][:], g1[0][:, half * 512:(half + 1) * 512], ALU.mult), reads=[PS[6 + half], g1[1]], writes=["tmo"])
                    P.op("gpsimd", lambda e, half=half, s=s: e.tensor_tensor(xom[s][:, half * 512:(half + 1) * 512], tmo[:], xtm[s][:, half * 512:(half + 1) * 512], ALU.add), reads=["tmo", "xtm%d" % s], writes=[("xom", s, half)])
                P.dma("sync", x_mix[r0:r0 + 128, :], xom[s][:], reads=[("xom", s, 0), ("xom", s, 1)], writes=[("xmix", r0)])
        P.barrier()
        if done("p5_%d" % L):
            return True

        P.mute = "p6" in skip
        A.reset(A_PERSIST)
        if moe:
            NTL = 24
            NR = NTL * 512
            I32 = mybir.dt.int32
            md = load_mod(L, [3, 4, 5])
            A2m, B2m, G2m = md[4][0], md[3][0], md[5][0]
            idx_all = A.alloc("idx_all", [128, 64], I32)
            sw_all = A.alloc("sw_all", [128, 64], F32)
            widx = A.alloc("widx", [128, NTL], I32)
            m_meta = A.mark()
            abf_all = A.alloc("abf_all", [128, 32, D], BF16)
            mk_all = A.alloc("mk_all", [128, 2, 32, NE], F32)
            pos_all = A.alloc("pos_all", [128, 32, NE], F32)
            rwf = A.alloc("rwf", [128, 8, NE], F32)
            aTfs = [A.alloc("aTf%d" % i, [128, 8, 128], F32) for i in range(2)]
            rbb = A.alloc("rbb", [128, NE], F32)
            trif = A.alloc("trif", [128, 128], F32)
            tri = A.alloc("tri", [128, 128], BF16)
            iot = A.alloc("iot", [128, NTL + 1], F32)
            base = A.alloc("base", [128, NE], F32)
            zt = A.alloc("zt", [128, 8192], BF16)
            xtr = [A.alloc("xtr%d" % i, [128, D], F32) for i in range(2)]
            afs = [A.alloc("af%d" % i, [128, D], F32) for i in range(2)]
            prs = [[A.alloc("pr%d_%d" % (i, q), [128, D], F32) for i in range(2)] for q in range(2)]
            jks = [A.alloc("jk%d" % i, [128, D], BF16) for i in range(2)]
            lgts = [A.alloc("lgt%d" % i, [128, NE], F32) for i in range(2)]
            rss = [A.alloc("rs_%d" % i, [128, 96], F32) for i in range(2)]
            mbfs = [A.alloc("mbf%d" % i, [128, NE], BF16) for i in range(2)]
            rs_ = rss[0]
            P.dma("sync", rwf[:], moe_router_p, writes=["rwf"])
            P.dma("sync", rbb[:], moe_router_b[0].partition_broadcast(128), writes=["rbb"])
            P.dma("sync", trif[:], k_tri, writes=["trif"])
            P.dma("sync", iot[:], k_iot, writes=["iot"])
            P.op("vector", lambda e: e.tensor_copy(tri[:], trif[:]), reads=["trif"], writes=["tri"])
            P.op("vector", lambda e: e.memset(base[:], 0.0), writes=["base"])
            P.op("gpsimd", lambda e: e.memset(zt[:], 0.0), writes=["zt"])
            Gv = G_d.rearrange("(p r) d -> p (r d)", p=128)
            NZ = NR * D // 128 // 8192
            for z in range(NZ):
                P.dma("sync", Gv[:, z * 8192:(z + 1) * 8192], zt[:], reads=["zt"], writes=[("Gz", z)])
            GZ = [("Gz", z) for z in range(NZ)]
            prev_rt = [None]
            for ti in range(SEQ // 128):
                def route_tile(ti=ti):
                    u = ti % 2
                    sfx = "_%d" % u
                    af, jk, lgt, rs_, mbf = afs[u], jks[u], lgts[u], rss[u], mbfs[u]
                    pr = prs[u]
                    bkA, bkB, bkT = (0, 1, 2) if u == 0 else (4, 5, 6)
                    aTf = aTfs[u]
                    s = ti % 2
                    r0 = ti * 128
                    xt_ = xtr[s]
                    P.dma("sync", xt_[:], x_mix[r0:r0 + 128, :], writes=["xtr%d" % s])
                    P.op("scalar", lambda e, xt_=xt_: e.activation(jk[:], xt_[:], AF.Square, accum_out=rs_[:, 0:1]), reads=["xtr%d" % s], writes=["jk" + sfx, "rs0" + sfx])
                    P.op("scalar", lambda e: e.activation(rs_[:, 1:2], rs_[:, 0:1], AF.Sqrt, bias=NORM_EPS, scale=1.0 / D), reads=["rs0" + sfx], writes=["rs1" + sfx])
                    P.op("vector", lambda e: e.reciprocal(rs_[:, 2:3], rs_[:, 1:2]), reads=["rs1" + sfx], writes=["rs2" + sfx])
                    P.op("vector", lambda e, xt_=xt_: e.scalar_tensor_tensor(af[:], xt_[:], rs_[:, 2:3], A2m[0][:], ALU.mult, ALU.mult), reads=["xtr%d" % s, "rs2" + sfx, A2m[1]], writes=["af" + sfx])
                    P.op("gpsimd", lambda e: e.tensor_tensor(af[:], af[:], B2m[0][:], ALU.add), reads=["af" + sfx, B2m[1]], writes=["af" + sfx])
                    P.op("scalar", lambda e, ti=ti: e.copy(abf_all[:, ti, :], af[:]), reads=["af" + sfx], writes=[("abf", ti)])
                    for k in range(8):
                        P.op("tensor", lambda e, k=k: e.transpose(ps[bkT + k // 4][:, (k % 4) * 128:(k % 4 + 1) * 128], af[:, k * 128:(k + 1) * 128], identf[:]),
                             reads=["af" + sfx, "identf"], writes=[PS[bkT + k // 4]])
                    for hh in range(2):
                        P.op("scalar", lambda e, hh=hh: e.copy(aTf[:, hh * 4:(hh + 1) * 4, :], ps[bkT + hh][:].rearrange("p (k t) -> p k t", k=4)), reads=[PS[bkT + hh]], writes=[("aTf", hh, u)])
                    for k in range(8):
                        P.op("tensor", lambda e, k=k: e.matmul(ps[bkA][:, 16:24], aTf[:, k, :], rwf[:, k, :], start=(k == 0), stop=(k == 7)),
                             reads=[("aTf", 0, u), ("aTf", 1, u), "rwf"], writes=[PS[bkA]])
                    P.op("vector", lambda e: e.tensor_tensor(lgt[:], ps[bkA][:, 16:24], rbb[:], ALU.add), reads=[PS[bkA], "rbb"], writes=[("lgt", ex, u) for ex in range(NE)])
                    yield
                    LT = [("lgt", ex, u) for ex in range(NE)]
                    mk1 = mk_all[:, 0, ti, :]
                    mk2 = mk_all[:, 1, ti, :]
                    P.op("vector", lambda e: e.reduce_max(rs_[:, 8:9], lgt[:], axis=AX.X), reads=LT, writes=["m1" + sfx])
                    P.op("vector", lambda e, mk1=mk1: e.tensor_scalar(mk1, lgt[:], rs_[:, 8:9], None, ALU.is_ge), reads=LT + ["m1" + sfx], writes=[("mk1", ti)])
                    P.op("vector", lambda e, mk1=mk1: e.scalar_tensor_tensor(rs_[:, 24:32], mk1, -1e30, lgt[:], ALU.mult, ALU.add), reads=LT + [("mk1", ti)], writes=["l2" + sfx])
                    P.op("vector", lambda e: e.reduce_max(rs_[:, 9:10], rs_[:, 24:32], axis=AX.X), reads=["l2" + sfx], writes=["m2" + sfx])
                    P.op("vector", lambda e, mk2=mk2: e.tensor_scalar(mk2, rs_[:, 24:32], rs_[:, 9:10], None, ALU.is_ge), reads=["l2" + sfx, "m2" + sfx], writes=[("mk2", ti)])
                    P.op("vector", lambda e: e.tensor_tensor(rs_[:, 10:11], rs_[:, 9:10], rs_[:, 8:9], ALU.subtract), reads=["m1" + sfx, "m2" + sfx], writes=["dl" + sfx])
                    P.op("scalar", lambda e, ti=ti: e.activation(sw_all[:, 32 + ti:33 + ti], rs_[:, 10:11], AF.Sigmoid), reads=["dl" + sfx], writes=[("sw2", ti)])
                    P.op("vector", lambda e, ti=ti: e.tensor_scalar(sw_all[:, ti:ti + 1], sw_all[:, 32 + ti:33 + ti], -1.0, 1.0, ALU.mult, ALU.add), reads=[("sw2", ti)], writes=[("sw1", ti)])
                    P.op("vector", lambda e, mk1=mk1, mk2=mk2: e.tensor_tensor(rs_[:, 40:48], mk1, mk2, ALU.add), reads=[("mk1", ti), ("mk2", ti)], writes=["mall" + sfx])
                    P.op("vector", lambda e: e.tensor_copy(mbf[:], rs_[:, 40:48]), reads=["mall" + sfx], writes=["mbf" + sfx])
                    P.op("tensor", lambda e: e.matmul(ps[bkA][:, 0:NE], tri[:], mbf[:], start=True, stop=True), reads=["tri", "mbf" + sfx], writes=[PS[bkA]])
                    P.op("tensor", lambda e: e.matmul(ps[bkB][:, 0:NE], ones_bf[:], mbf[:], start=True, stop=True), reads=["mbf" + sfx], writes=[PS[bkB]])
                    P.op("vector", lambda e, ti=ti: e.tensor_tensor(pos_all[:, ti, :], ps[bkA][:, 0:NE], base[:], ALU.add), reads=[PS[bkA], "base"], writes=[("pos", ti)])
                    P.op("vector", lambda e: e.tensor_tensor(base[:], ps[bkB][:, 0:NE], base[:], ALU.add), reads=[PS[bkB], "base"], writes=["base"])
                gen_ = route_tile()
                next(gen_)
                if prev_rt[0] is not None:
                    for _ in prev_rt[0]:
                        pass
                prev_rt[0] = gen_
            for _ in prev_rt[0]:
                pass
            MKS = [("mk1", ti) for ti in range(32)] + [("mk2", ti) for ti in range(32)]
            POS = [("pos", ti) for ti in range(32)]
            nt_ = rs_[:, 16:24]
            stt = rs_[:, 48:56]
            P.op("vector", lambda e: e.tensor_scalar(nt_, base[:], 0.0, None, ALU.is_gt), reads=["base"], writes=["nt"])
            for m in range(1, 8):
                P.op("vector", lambda e, m=m: e.scalar_tensor_tensor(nt_, base[:], 512.0 * m, nt_, ALU.is_gt, ALU.add), reads=["base", "nt"], writes=["nt"])
            P.op("vector", lambda e: e.memset(stt, 0.0), writes=["stt"])
            for ex in range(1, NE):
                P.op("vector", lambda e, ex=ex: e.tensor_tensor(rs_[:, 48 + ex:49 + ex], rs_[:, 47 + ex:48 + ex], rs_[:, 15 + ex:16 + ex], ALU.add), reads=["stt", "nt"], writes=["stt"])
            P.op("vector", lambda e: e.tensor_tensor(rs_[:, 56:64], stt, nt_, ALU.add), reads=["stt", "nt"], writes=["endt"])
            P.op("vector", lambda e: e.tensor_scalar(rs_[:, 64:72], stt, 512.0, None, ALU.mult), reads=["stt"], writes=["rowb"])
            eidf = A.alloc("eidf", [128, NTL], F32)
            P.op("vector", lambda e: e.tensor_scalar(eidf[:], iot[:, 0:NTL], rs_[:, 56:57], None, ALU.is_ge), reads=["iot", "endt"], writes=["eidf"])
            for ex in range(1, NE - 1):
                P.op("vector", lambda e, ex=ex: e.scalar_tensor_tensor(eidf[:], iot[:, 0:NTL], rs_[:, 56 + ex:57 + ex], eidf[:], ALU.is_ge, ALU.add), reads=["iot", "endt", "eidf"], writes=["eidf"])
            P.op("vector", lambda e: e.tensor_scalar(eidf[:], eidf[:], 128.0, iot[:, NTL:NTL + 1], ALU.mult, ALU.add), reads=["eidf", "iot"], writes=["eidf"])
            P.op("vector", lambda e: e.tensor_copy(widx[:], eidf[:]), reads=["eidf"], writes=["widx"])
            rowi = A.alloc("rowi", [128, 32, NE], F32)
            t8 = A.alloc("t8", [128, 32, NE], F32)
            d32 = A.alloc("d32", [128, 64], F32)
            P.op("vector", lambda e: e.tensor_tensor(rowi[:], pos_all[:], rs_[:, 64:72].unsqueeze(1).to_broadcast([128, 32, NE]), ALU.add), reads=POS + ["rowb"], writes=["rowi"])
            for sl_ in range(2):
                P.op("vector", lambda e, sl_=sl_: e.tensor_tensor(t8[:], mk_all[:, sl_], rowi[:], ALU.mult), reads=MKS + ["rowi"], writes=["t8"])
                P.op("vector", lambda e, sl_=sl_: e.reduce_sum(d32[:, sl_ * 32:(sl_ + 1) * 32], t8[:], axis=AX.X), reads=["t8"], writes=[("d32", sl_)])
            P.op("vector", lambda e: e.tensor_copy(idx_all[:], d32[:]), reads=[("d32", 0), ("d32", 1)], writes=["idx_all"])
            for ti in range(32):
                for sl_ in range(2):
                    col = sl_ * 32 + ti
                    P.op("gpsimd", lambda e, col=col, ti=ti: e.indirect_dma_start(out=G_d[:, :], out_offset=bass.IndirectOffsetOnAxis(ap=idx_all[:, col:col + 1], axis=0),
                                                                               in_=abf_all[:, ti, :], in_offset=None),
                         reads=["idx_all", ("abf", ti)] + GZ, writes=[("Gs", col)], dma=True)
            P.barrier()
            if done("p6a_%d" % L):
                return True
            A.reset(m_meta)
            gt = [A.alloc("gt%d" % i, [128, 1024], BF16) for i in range(8)]
            aT = A.alloc("aTm", [128, 8, 512], BF16)
            hid = A.alloc("hidm", [128, 28, 512], BF16)
            wA = [A.alloc("wAm%d" % i, [128, 8, 512], BF16) for i in range(6)]
            wB = [A.alloc("wBm%d" % i, [128, 4, 1024], BF16) for i in range(4)]
            slm = [A.alloc("slm%d" % i, [128, 512], F32) for i in range(2)]
            yo = [A.alloc("yom%d" % i, [128, D], F32) for i in range(2)]
            wa_i = 0
            wb_i = 0
            gi_ = 0
            yo_i = 0
            pk = 0
            def g_load(i_):
                for j_ in range(4):
                    q_ = (i_ * 4 + j_) % 8
                    rr_ = i_ * 512 + j_ * 128
                    P.dma("sync", gt[q_][:], G_d[rr_:rr_ + 128, :], writes=["gt%d" % q_])

            g_load(0)
            for i in range(NTL):
                ixc = widx[:, i:i + 1]
                if i + 1 < NTL:
                    g_load(i + 1)
                for j in range(4):
                    g_ = gt[(i * 4 + j) % 8]
                    gtok = "gt%d" % ((i * 4 + j) % 8)
                    bank = 6 + (j % 2)
                    pbm = ps[bank][:].bitcast(BF16)
                    for k in range(8):
                        P.op("tensor", lambda e, k=k, g_=g_, pbm=pbm: e.transpose(pbm[:, k * 128:(k + 1) * 128], g_[:, k * 128:(k + 1) * 128], ident[:]), reads=[gtok, "ident"], writes=[PS[bank]])
                    P.op("scalar", lambda e, j=j, pbm=pbm: e.copy(aT[:, :, j * 128:(j + 1) * 128], pbm[:, 0:1024].rearrange("p (k t) -> p k t", k=8)), reads=[PS[bank]], writes=[("aTm", j)])
                AT = [("aTm", j) for j in range(4)]
                for fb in range(7):
                    sl_w = []
                    for wn, Wq in enumerate((w1q, w3q)):
                        s = wa_i % 6
                        wa_i += 1
                        for h in range(2):
                            P.op("gpsimd", lambda e, s=s, h=h, fb=fb, Wq=Wq, ixc=ixc: e.indirect_dma_start(out=wA[s][:, h * 4:(h + 1) * 4, :].rearrange("p k f -> p (k f)"), out_offset=None,
                                                                                                   in_=Wq[fb][h][:, :], in_offset=bass.IndirectOffsetOnAxis(ap=ixc, axis=0)),
                                 reads=[], writes=[("wAm", s, h)], dma=True)
                        sl_w.append(s)
                    for fc in range(4):
                        b1, b3 = (pk % 2) * 2, (pk % 2) * 2 + 1
                        sls = slm[pk % 2]
                        stok = "slm%d" % (pk % 2)
                        pk += 1
                        for (bank, s) in ((b1, sl_w[0]), (b3, sl_w[1])):
                            for k in range(8):
                                P.op("tensor", lambda e, k=k, bank=bank, s=s, fc=fc: e.matmul(ps[bank][:], wA[s][:, k, fc * 128:(fc + 1) * 128], aT[:, k, :], start=(k == 0), stop=(k == 7)),
                                     reads=AT + [("wAm", s, 0), ("wAm", s, 1)], writes=[PS[bank]])
                        P.op("scalar", lambda e, b1=b1, sls=sls: e.activation(sls[:], ps[b1][:], AF.Silu), reads=[PS[b1]], writes=[stok])
                        P.op("vector", lambda e, b3=b3, sls=sls, fb=fb, fc=fc: e.tensor_tensor(hid[:, fb * 4 + fc, :], ps[b3][:], sls[:], ALU.mult),
                             reads=[PS[b3], stok], writes=[("hidm", fb * 4 + fc)])
                for fb in range(7):
                    s = wb_i % 4
                    wb_i += 1
                    for h in range(2):
                        P.op("gpsimd", lambda e, s=s, h=h, fb=fb, ixc=ixc: e.indirect_dma_start(out=wB[s][:, h * 2:(h + 1) * 2, :].rearrange("p k f -> p (k f)"), out_offset=None,
                                                                                         in_=w2q[fb][h][:, :], in_offset=bass.IndirectOffsetOnAxis(ap=ixc, axis=0)),
                             reads=[], writes=[("wBm", s, h)], dma=True)
                    for j in range(4):
                        for half in range(2):
                            bank = j * 2 + half
                            for k in range(4):
                                P.op("tensor", lambda e, k=k, j=j, half=half, bank=bank, s=s, fb=fb: e.matmul(ps[bank][:], hid[:, fb * 4 + k, j * 128:(j + 1) * 128], wB[s][:, k, half * 512:(half + 1) * 512],
                                                                                               start=(fb == 0 and k == 0), stop=(fb == 6 and k == 3)),
                                     reads=[("hidm", fb * 4 + k), ("wBm", s, k // 2)], writes=[PS[bank]])
                for j in range(4):
                    y_ = yo[yo_i % 2]
                    ytok = "yom%d" % (yo_i % 2)
                    yo_i += 1
                    for half in range(2):
                        bank = j * 2 + half
                        if half == 0:
                            P.op("scalar", lambda e, y_=y_, bank=bank: e.copy(y_[:, 0:512], ps[bank][:]), reads=[PS[bank]], writes=[(ytok, 0)])
                        else:
                            P.op("vector", lambda e, y_=y_, bank=bank: e.tensor_copy(y_[:, 512:1024], ps[bank][:]), reads=[PS[bank]], writes=[(ytok, 1)])
                    rr = i * 512 + j * 128
                    P.dma("sync", Y_d[rr:rr + 128, :], y_[:], reads=[(ytok, 0), (ytok, 1)], writes=[("Y", rr)])
            P.barrier()
            if done("p6b_%d" % L):
                return True
            A.reset(m_meta)
            y1 = [A.alloc("y1_%d" % i, [128, D], F32) for i in range(2)]
            y2 = [A.alloc("y2_%d" % i, [128, D], F32) for i in range(2)]
            xtc = [A.alloc("xtc%d" % i, [128, D], F32) for i in range(2)]
            cmb = [A.alloc("cmb%d" % i, [128, D], F32) for i in range(2)]
            xoz = [A.alloc("xozm%d" % i, [128, D], F32) for i in range(2)]
            jzz = [A.alloc("jzz%d" % i, [128, D], BF16) for i in range(2)]
            szz = [A.alloc("szz%d" % i, [128, 4], F32) for i in range(2)]
            fngm = A.alloc("fngm", [128, D], F32)
            P.dma("sync", fngm[:], final_norm.partition_broadcast(128), writes=["fngm"])
            prev_c = [None]
            for ti in range(SEQ // 128):
                def comb_tile(ti=ti):
                    s = ti % 2
                    r0 = ti * 128
                    for (yt_, nm, col) in ((y1[s], "y1_%d" % s, ti), (y2[s], "y2_%d" % s, 32 + ti)):
                        P.op("gpsimd", lambda e, yt_=yt_, col=col: e.indirect_dma_start(out=yt_[:, :], out_offset=None, in_=Y_d[:, :],
                                                                                     in_offset=bass.IndirectOffsetOnAxis(ap=idx_all[:, col:col + 1], axis=0)),
                             reads=[], writes=[nm], dma=True)
                    P.dma("sync", xtc[s][:], x_mix[r0:r0 + 128, :], writes=["xtc%d" % s])
                    cm = cmb[s]
                    P.op("vector", lambda e, cm=cm, s=s, ti=ti: e.tensor_scalar(cm[:], y1[s][:], sw_all[:, ti:ti + 1], None, ALU.mult), reads=["y1_%d" % s], writes=["cmb%d" % s])
                    P.op("vector", lambda e, cm=cm, s=s, ti=ti: e.scalar_tensor_tensor(cm[:], y2[s][:], sw_all[:, 32 + ti:33 + ti], cm[:], ALU.mult, ALU.add), reads=["y2_%d" % s, "cmb%d" % s], writes=["cmb%d" % s])
                    P.op("gpsimd", lambda e, cm=cm: e.tensor_tensor(cm[:], cm[:], G2m[0][:], ALU.mult), reads=["cmb%d" % s, G2m[1]], writes=["cmb%d" % s])
                    P.op("gpsimd", lambda e, cm=cm, s=s: e.tensor_tensor(cm[:], cm[:], xtc[s][:], ALU.add), reads=["cmb%d" % s, "xtc%d" % s], writes=["cmb%d" % s])
                    yield
                    szs = szz[s]
                    P.op("scalar", lambda e, cm=cm, s=s, szs=szs: e.activation(jzz[s][:], cm[:], AF.Square, accum_out=szs[:, 0:1]), reads=["cmb%d" % s], writes=["jzz%d" % s, "szz0_%d" % s])
                    P.op("scalar", lambda e, szs=szs: e.activation(szs[:, 1:2], szs[:, 0:1], AF.Sqrt, bias=NORM_EPS, scale=1.0 / D), reads=["szz0_%d" % s], writes=["szz1_%d" % s])
                    P.op("vector", lambda e, szs=szs: e.reciprocal(szs[:, 2:3], szs[:, 1:2]), reads=["szz1_%d" % s], writes=["szz2_%d" % s])
                    P.op("vector", lambda e, cm=cm, s=s, szs=szs: e.scalar_tensor_tensor(xoz[s][:], cm[:], szs[:, 2:3], fngm[:], ALU.mult, ALU.mult), reads=["cmb%d" % s, "szz2_%d" % s, "fngm"], writes=["xozm%d" % s])
                    P.dma("sync", out[r0:r0 + 128, :], xoz[s][:], reads=["xozm%d" % s], writes=[("out", r0)])
                gen_ = comb_tile()
                next(gen_)
                if prev_c[0] is not None:
                    for _ in prev_c[0]:
                        pass
                prev_c[0] = gen_
            for _ in prev_c[0]:
                pass
            P.barrier()
            return False
        md = load_mod(L, [3, 4, 5])
        normFA, normFB, normF = make_normT("p6", md[4], md[3], 7)
        aT = A.alloc("aT", [128, 8, 512], BF16)
        hid = A.alloc("hid", [128, 28, 512], BF16)
        wA = [A.alloc("wA%d" % i, [128, 8, 512], BF16) for i in range(4)]
        W2res = A.alloc("W2res", [128, 28, 1024], BF16)
        for fb in range(7):
            for cbk in range(2):
                P.dma("gpsimd", W2res[:, fb * 4:(fb + 1) * 4, cbk * 512:(cbk + 1) * 512], ffn_w2[0][fb * 512:(fb + 1) * 512, cbk * 512:(cbk + 1) * 512].rearrange("(k p) f -> p k f", p=128), writes=[("W2res", fb, cbk)])
        sl = [A.alloc("sl%d" % i, [128, 512], F32) for i in range(2)]
        xtf = [A.alloc("xtf%d" % i, [128, D], F32) for i in range(2)]
        xof = [A.alloc("xof%d" % i, [128, D], F32) for i in range(2)]
        tmf = A.alloc("tmf", [128, 512], F32)
        if moe:
            acc = A.alloc("acc", [128, 4, D], F32)
            rwb = A.alloc("rwb", [128, NE, D], F32)
            rbb = A.alloc("rbb", [128, NE], F32)
            af = A.alloc("af", [128, D], F32)
            pr = A.alloc("pr", [128, D], F32)
            lgt = A.alloc("lgt", [128, 4, NE], F32)
            wgt = A.alloc("wgt", [128, 4, NE], F32)
            rs_ = A.alloc("rs_", [128, 40], F32)
            for ex in range(NE):
                P.dma("sync", rwb[:, ex, :], moe_router_t[ex].partition_broadcast(128), writes=[("rwb", ex)])
            P.dma("sync", rbb[:], moe_router_b[0].partition_broadcast(128), writes=["rbb"])
        wa_i = [0]
        wb_i = [0]
        xf_i = 0
        glist = [g for g in range(9) if not (g == 8 and (moe or not need_ctx))]
        pend = [None]
        for gpos, g in enumerate(glist):
            t0, n = GROUPS[g]
            which = 0 if g < 8 else 1
            nt_ = n // 128
            if pend[0] is None:
                normFB(normFA(x_mix, t0, nt_, which), aT, "aT")
            else:
                normFB(pend[0], aT, "aT")
                pend[0] = None
            if moe:
                for j in range(nt_):
                    r0 = t0 + j * 128
                    P.dma("sync", xtf[0][:], x_mix[r0:r0 + 128, :], writes=["xtf0"])
                    P.op("scalar", lambda e: e.activation(pr[:], xtf[0][:], AF.Square, accum_out=rs_[:, 0:1]), reads=["xtf0"], writes=["pr", "rs0"])
                    P.op("scalar", lambda e: e.activation(rs_[:, 1:2], rs_[:, 0:1], AF.Sqrt, bias=NORM_EPS, scale=1.0 / D), reads=["rs0"], writes=["rs1"])
                    P.op("vector", lambda e: e.reciprocal(rs_[:, 2:3], rs_[:, 1:2]), reads=["rs1"], writes=["rs2"])
                    am, bm = md[4][which], md[3][which]
                    P.op("vector", lambda e, am=am: e.scalar_tensor_tensor(af[:], xtf[0][:], rs_[:, 2:3], am[0][:], ALU.mult, ALU.mult), reads=["xtf0", "rs2", am[1]], writes=["af"])
                    P.op("gpsimd", lambda e, bm=bm: e.tensor_tensor(af[:], af[:], bm[0][:], ALU.add), reads=["af", bm[1]], writes=["af"])
                    for ex in range(NE):
                        eng = "vector" if ex % 2 == 0 else "gpsimd"
                        P.op(eng, lambda e, ex=ex: e.tensor_tensor(pr[:], af[:], rwb[:, ex, :], ALU.mult), reads=["af", ("rwb", ex)], writes=["pr"])
                        P.op("vector", lambda e, ex=ex, j=j: e.reduce_sum(lgt[:, j, ex:ex + 1], pr[:], axis=AX.X), reads=["pr"], writes=[("lgt", j, ex)])
                    LT = [("lgt", j, ex) for ex in range(NE)]
                    lj = lgt[:, j, :]
                    P.op("vector", lambda e, lj=lj: e.tensor_tensor(lj, lj, rbb[:], ALU.add), reads=LT + ["rbb"], writes=LT)
                    P.op("vector", lambda e, lj=lj: e.reduce_max(rs_[:, 8:9], lj, axis=AX.X), reads=LT, writes=["m1"])
                    P.op("vector", lambda e, lj=lj: e.tensor_scalar(rs_[:, 16:24], lj, rs_[:, 8:9], None, ALU.is_ge), reads=LT + ["m1"], writes=["mk1"])
                    P.op("vector", lambda e, lj=lj: e.scalar_tensor_tensor(rs_[:, 24:32], rs_[:, 16:24], -1e30, lj, ALU.mult, ALU.add), reads=LT + ["mk1"], writes=["l2"])
                    P.op("vector", lambda e: e.reduce_max(rs_[:, 9:10], rs_[:, 24:32], axis=AX.X), reads=["l2"], writes=["m2"])
                    P.op("vector", lambda e: e.tensor_scalar(rs_[:, 32:40], rs_[:, 24:32], rs_[:, 9:10], None, ALU.is_ge), reads=["l2", "m2"], writes=["mk2"])
                    P.op("vector", lambda e: e.tensor_tensor(rs_[:, 10:11], rs_[:, 9:10], rs_[:, 8:9], ALU.subtract), reads=["m1", "m2"], writes=["dl"])
                    P.op("scalar", lambda e: e.activation(rs_[:, 11:12], rs_[:, 10:11], AF.Sigmoid), reads=["dl"], writes=["s2"])
                    P.op("vector", lambda e: e.tensor_scalar(rs_[:, 12:13], rs_[:, 11:12], -1.0, 1.0, ALU.mult, ALU.add), reads=["s2"], writes=["s1"])
                    P.op("vector", lambda e, j=j: e.tensor_scalar(wgt[:, j, :], rs_[:, 16:24], rs_[:, 12:13], None, ALU.mult), reads=["mk1", "s1"], writes=[("wgt", j)])
                    P.op("vector", lambda e, j=j: e.scalar_tensor_tensor(wgt[:, j, :], rs_[:, 32:40], rs_[:, 11:12], wgt[:, j, :], ALU.mult, ALU.add), reads=["mk2", "s2", ("wgt", j)], writes=[("wgt", j)])
            nexp = NE if moe else 1
            for ex in range(nexp):
                if moe:
                    W1, W3, W2 = moe_w1[0, ex], moe_w3[0, ex], moe_w2[0, ex]
                else:
                    W1, W3, W2 = ffn_w1[0], ffn_w3[0], ffn_w2[0]
                for fb in range(7):
                    sl_w = []
                    for Wsrc in (W1, W3):
                        s = wa_i[0] % 4
                        wa_i[0] += 1
                        P.dma("gpsimd", wA[s][:], Wsrc[:, fb * 512:(fb + 1) * 512].rearrange("(k p) f -> p k f", p=128), writes=["wA%d" % s])
                        sl_w.append(s)
                    for fc in range(4):
                        b1, b3 = (fc % 2) * 2, (fc % 2) * 2 + 1
                        for (bank, s) in ((b1, sl_w[0]), (b3, sl_w[1])):
                            for k in range(8):
                                P.op("tensor", lambda e, k=k, bank=bank, s=s, fc=fc, n=n: e.matmul(ps[bank][:, 0:n], wA[s][:, k, fc * 128:(fc + 1) * 128], aT[:, k, 0:n], start=(k == 0), stop=(k == 7)),
                                     reads=["aT", "wA%d" % s], writes=[PS[bank]])
                        sls = sl[fc % 2]
                        P.op("scalar", lambda e, b1=b1, sls=sls, n=n: e.activation(sls[:, 0:n], ps[b1][:, 0:n], AF.Silu), reads=[PS[b1]], writes=["sl%d" % (fc % 2)])
                        P.op("vector", lambda e, b3=b3, sls=sls, fb=fb, fc=fc, n=n: e.tensor_tensor(hid[:, fb * 4 + fc, 0:n], ps[b3][:, 0:n], sls[:, 0:n], ALU.mult),
                             reads=[PS[b3], "sl%d" % (fc % 2)], writes=[("hid", fb * 4 + fc)])
                if (not moe) and gpos + 1 < len(glist):
                    g2 = glist[gpos + 1]
                    pend[0] = normFA(x_mix, GROUPS[g2][0], GROUPS[g2][1] // 128, 0 if g2 < 8 else 1)
                for fb in range(7):
                    for j in range(nt_):
                        for half in range(2):
                            bank = j * 2 + half
                            for k in range(4):
                                P.op("tensor", lambda e, k=k, j=j, half=half, bank=bank, fb=fb: e.matmul(ps[bank][:], hid[:, fb * 4 + k, j * 128:(j + 1) * 128], W2res[:, fb * 4 + k, half * 512:(half + 1) * 512],
                                                                                          start=(fb == 0 and k == 0), stop=(fb == 6 and k == 3)),
                                     reads=[("hid", fb * 4 + k), ("W2res", fb, half)], writes=[PS[bank]])
                for j in range(nt_):
                    r0 = t0 + j * 128
                    if not moe or ex == nexp - 1:
                        sx = xf_i % 2
                        xf_i += 1
                        P.dma("sync", xtf[sx][:], x_mix[r0:r0 + 128, :], writes=["xtf%d" % sx])
                    for half in range(2):
                        bank = j * 2 + half
                        hs = slice(half * 512, (half + 1) * 512)
                        g2 = md[5][which]
                        if moe:
                            wc = wgt[:, j, ex:ex + 1]
                            if ex == 0:
                                P.op("vector", lambda e, j=j, hs=hs, bank=bank, wc=wc: e.tensor_scalar(acc[:, j, hs], ps[bank][:], wc, None, ALU.mult), reads=[PS[bank], ("wgt", j)], writes=[("acc", j, half)])
                            else:
                                P.op("vector", lambda e, j=j, hs=hs, bank=bank, wc=wc: e.scalar_tensor_tensor(acc[:, j, hs], ps[bank][:], wc, acc[:, j, hs], ALU.mult, ALU.add), reads=[PS[bank], ("wgt", j), ("acc", j, half)], writes=[("acc", j, half)])
                            if ex == nexp - 1:
                                P.op("gpsimd", lambda e, j=j, hs=hs, g2=g2: e.tensor_tensor(tmf[:], acc[:, j, hs], g2[0][:, hs], ALU.mult), reads=[("acc", j, half), g2[1]], writes=["tmf"])
                                P.op("gpsimd", lambda e, hs=hs, sx=sx: e.tensor_tensor(xof[sx][:, hs], tmf[:], xtf[sx][:, hs], ALU.add), reads=["tmf", "xtf%d" % sx], writes=[("xof", sx, half)])
                        else:
                            P.op("vector", lambda e, hs=hs, bank=bank, g2=g2: e.tensor_tensor(tmf[:], ps[bank][:], g2[0][:, hs], ALU.mult), reads=[PS[bank], g2[1]], writes=["tmf"])
                            P.op("gpsimd", lambda e, hs=hs, sx=sx: e.tensor_tensor(xof[sx][:, hs], tmf[:], xtf[sx][:, hs], ALU.add), reads=["tmf", "xtf%d" % sx], writes=[("xof", sx, half)])
                    if not moe or ex == nexp - 1:
                        P.dma("sync", x_out[r0:r0 + 128, :], xof[sx][:], reads=[("xof", sx, 0), ("xof", sx, 1)], writes=[("xout", r0)])
        P.barrier()
        if done("p6_%d" % L):
            return True
        return False

    if layer(0, xin, xa_d, xb_d, True, False):
        return finish()
    if layer(1, xb_d, xc_d, xd_d, False, True):
        return finish()
    return finish()
    A.reset(A_PERSIST)
    fng = A.alloc("fng", [128, D], F32)
    P.dma("sync", fng[:], final_norm.partition_broadcast(128), writes=["fng"])
    xtz = [A.alloc("xtz%d" % i, [128, D], F32) for i in range(2)]
    xoz = [A.alloc("xoz%d" % i, [128, D], F32) for i in range(2)]
    jz = A.alloc("jz", [128, D], BF16)
    sz = A.alloc("sz", [128, 4], F32)
    for i in range(SEQ // 128):
        s = i % 2
        P.dma("sync", xtz[s][:], xd_d[i * 128:(i + 1) * 128, :], writes=["xtz%d" % s])
        P.op("scalar", lambda e, s=s: e.activation(jz[:], xtz[s][:], AF.Square, accum_out=sz[:, 0:1]), reads=["xtz%d" % s], writes=["jz", "sz0"])
        P.op("scalar", lambda e: e.activation(sz[:, 1:2], sz[:, 0:1], AF.Sqrt, bias=NORM_EPS, scale=1.0 / D), reads=["sz0"], writes=["sz1"])
        P.op("vector", lambda e: e.reciprocal(sz[:, 2:3], sz[:, 1:2]), reads=["sz1"], writes=["sz2"])
        P.op("vector", lambda e, s=s: e.scalar_tensor_tensor(xoz[s][:], xtz[s][:], sz[:, 2:3], fng[:], ALU.mult, ALU.mult), reads=["xtz%d" % s, "sz2", "fng"], writes=["xoz%d" % s])
        P.dma("sync", out[i * 128:(i + 1) * 128, :], xoz[s][:], reads=["xoz%d" % s], writes=[("out", i)])
    return finish()


_CACHE = {}


def _moe_layout(inputs):
    f = np.float32
    out = {}
    for nm, key in (("w1q", "moe_w1"), ("w3q", "moe_w3")):
        w = np.asarray(inputs[key], dtype=f)[0].reshape(NE, 2, 4, 128, 7, 512)
        w = w.transpose(4, 1, 0, 3, 2, 5)
        for fb in range(7):
            for h in range(2):
                out["%s_%d_%d" % (nm, fb, h)] = np.ascontiguousarray(w[fb, h]).reshape(NE * 128, 2048)
    w = np.asarray(inputs["moe_w2"], dtype=f)[0].reshape(NE, 7, 2, 2, 128, D)
    w = w.transpose(1, 2, 0, 4, 3, 5)
    for fb in range(7):
        for h in range(2):
            out["w2q_%d_%d" % (fb, h)] = np.ascontiguousarray(w[fb, h]).reshape(NE * 128, 2048)
    return out


def _core_inputs(inputs, b, consts, moe_l=None):
    f = np.float32
    m = {}
    m.update(moe_l if moe_l is not None else _moe_layout(inputs))
    m["xin"] = np.ascontiguousarray(np.concatenate([inputs["x"][b], inputs["ctx"][b]], axis=0), dtype=f)
    cc = np.concatenate([np.asarray(inputs["c"][b]).reshape(8, 128).T, np.asarray(inputs["c_ctx"]).reshape(8, 128).T], axis=1)
    m["cT"] = np.ascontiguousarray(cc, dtype=f)
    for k in ("w_ada", "b_ada", "norm_mix", "norm_ffn", "w_in", "ret_gn", "attn_qn", "attn_kn", "pool_w", "pool_scale",
              "w_branch", "w_out", "ffn_w1", "ffn_w3", "ffn_w2", "moe_router_b", "final_norm"):
        m[k] = np.ascontiguousarray(inputs[k], dtype=f)
    m["ret_decay"] = np.ascontiguousarray(np.asarray(inputs["ret_decay"]).reshape(2, 8), dtype=f)
    m["k_ident"] = consts["ident"]
    m["k_acos"] = consts["acos"]
    m["k_asin"] = consts["asin"]
    m["k_rt4"] = np.ascontiguousarray(np.stack([consts["rcos"], consts["rsin"], consts["rcosk"], consts["rsink"]], axis=2))
    m["moe_router_t"] = np.ascontiguousarray(np.asarray(inputs["moe_router"])[0].T, dtype=f)
    m["moe_router_p"] = np.ascontiguousarray(np.asarray(inputs["moe_router"], dtype=f)[0].reshape(8, 128, NE).transpose(1, 0, 2))
    m["k_rtab"] = consts["rtab"]
    m["k_pcol"] = consts["pcol"]
    m["k_pedge"] = consts["pedge"]
    m["k_tri"] = consts["tri"]
    m["k_iot"] = consts["iot"]
    return m


def kernel(**inputs):
    inputs = {k: np.asarray(v) for k, v in inputs.items()}
    consts = _host_consts()
    if "nc" not in _CACHE:
        _CACHE["nc"] = build()
    nc = _CACHE["nc"]
    moe_l = _moe_layout(inputs)
    in_maps = [_core_inputs(inputs, b, consts, moe_l) for b in range(8)]
    res = run_bass_kernel_spmd(nc, in_maps, core_ids=list(range(8)))
    outs = [np.asarray(r["out"], dtype=np.float32) for r in res.results]
    return np.stack(outs, axis=0)
```

```python
import os
import numpy as np
import ml_dtypes
CUT = int(os.environ.get('MK_CUT', '99'))
from contextlib import ExitStack
import concourse.bass as bass
import concourse.mybir as mybir
from concourse.bass_utils import run_bass_kernel_spmd

F32 = mybir.dt.float32
BF16 = mybir.dt.bfloat16
AF = mybir.ActivationFunctionType
ALU = mybir.AluOpType
AX = mybir.AxisListType

ALL_Q = ("tensor", "vector", "scalar", "gpsimd", "sync")
N_DMA_SEMS = 8

D = 1024
SEQ = 4096
CTX = 256
NT = SEQ + CTX
NTILE = NT // 128
FFN = 3584
NE = 8
INW = 6144
SB_BASE = 16640
SB_END = 229376
NORM_EPS = 1e-6
GN_EPS = 1e-5


class Op:
    __slots__ = ("q", "fn", "is_dma", "deps", "signal", "sem", "target", "prev_on_sem")

    def __init__(self, q, fn, is_dma):
        self.q = q
        self.fn = fn
        self.is_dma = is_dma
        self.deps = []
        self.signal = False
        self.sem = None
        self.target = 0
        self.prev_on_sem = None


class Prog:
    def __init__(self, nc, same_engine_sync=True):
        self.nc = nc
        self.ops = {q: [] for q in ALL_Q}
        self.last_writer = {}
        self.readers = {}
        self.same_engine_sync = same_engine_sync
        self.dma_rr = {q: 0 for q in ALL_Q}
        self.dma_last = {}
        self.mute = False

    def op(self, q, fn, reads=(), writes=(), dma=False, extra_deps=()):
        if self.mute:
            return None
        o = Op(q, fn, dma)
        deps = list(extra_deps)
        for r in reads:
            w = self.last_writer.get(r)
            if w is not None:
                deps.append(w)
        for w_ in writes:
            w = self.last_writer.get(w_)
            if w is not None:
                deps.append(w)
            deps.extend(self.readers.get(w_, ()))
        seen = set()
        for d in deps:
            if id(d) in seen or d is o:
                continue
            seen.add(id(d))
            if d.q == q and not d.is_dma:
                if q == "tensor" or not self.same_engine_sync:
                    continue
            o.deps.append(d)
            d.signal = True
        for r in reads:
            self.readers.setdefault(r, []).append(o)
        for w_ in writes:
            self.last_writer[w_] = o
            self.readers[w_] = []
        if dma:
            k = self.dma_rr[q]
            self.dma_rr[q] = k + 1
            slot = (q, k % N_DMA_SEMS)
            o.sem = slot
            o.prev_on_sem = self.dma_last.get(slot)
            o.target = (o.prev_on_sem.target if o.prev_on_sem is not None else 0) + 16
            self.dma_last[slot] = o
        self.ops[q].append(o)
        return o

    def dma(self, q, out, in_, reads=(), writes=(), **kw):
        return self.op(q, lambda e: e.dma_start(out=out, in_=in_, **kw), reads, writes, dma=True)

    def barrier(self):
        if self.mute:
            return
        deps = []
        for q in ALL_Q:
            for o in reversed(self.ops[q]):
                if not o.is_dma:
                    deps.append(o)
                    break
        deps.extend(self.dma_last.values())
        b = self.op("sync", lambda e: e.nop(), extra_deps=deps)
        for q in ALL_Q:
            if q != "sync":
                self.op(q, lambda e: e.nop(), extra_deps=[b])
        self.last_writer = {}
        self.readers = {}

    def emit(self):
        nc = self.nc
        for q in ALL_Q:
            c = 0
            for o in self.ops[q]:
                if o.is_dma:
                    continue
                if o.signal:
                    c += 1
                    o.target = c
        with ExitStack() as st:
            qsem = {q: st.enter_context(nc.semaphore("s_" + q)) for q in ALL_Q}
            dsem = {}
            for q in ALL_Q:
                for k in range(min(N_DMA_SEMS, self.dma_rr[q])):
                    dsem[(q, k)] = st.enter_context(nc.semaphore("d_%s_%d" % (q, k)))
            block = st.enter_context(nc.Block())

            def run_queue(q, eng):
                known = {}

                def wait_for(d):
                    if d.is_dma:
                        key = d.sem
                        sem = dsem[key]
                    else:
                        key = d.q
                        sem = qsem[d.q]
                    if known.get(key, 0) >= d.target:
                        return
                    eng.wait_ge(sem, d.target)
                    known[key] = d.target

                for o in self.ops[q]:
                    for d in o.deps:
                        wait_for(d)
                    if o.is_dma and o.prev_on_sem is not None:
                        wait_for(o.prev_on_sem)
                    ins = o.fn(eng)
                    if o.is_dma:
                        ins.then_inc(dsem[o.sem], 16)
                    elif o.signal:
                        ins.then_inc(qsem[q], 1)

            @block.tensor
            def _(e):
                run_queue("tensor", e)

            @block.vector
            def _(e):
                run_queue("vector", e)

            @block.scalar
            def _(e):
                run_queue("scalar", e)

            @block.gpsimd
            def _(e):
                run_queue("gpsimd", e)

            @block.sync
            def _(e):
                run_queue("sync", e)


class Arena:
    def __init__(self, nc, base=SB_BASE, end=SB_END):
        self.nc = nc
        self.base = base
        self.off = base
        self.end = end
        self.n = 0

    def reset(self, to=None):
        self.off = self.base if to is None else to

    def mark(self):
        return self.off

    def alloc(self, name, shape, dt):
        esz = 2 if dt == BF16 else 4
        nbytes = int(np.prod(shape[1:])) * esz
        nbytes = (nbytes + 63) // 64 * 64
        assert self.off + nbytes <= self.end, ("SBUF overflow", name, self.off, nbytes)
        self.n += 1
        t = self.nc.alloc_sbuf_tensor_at("%s_%d" % (name, self.n), list(shape), dt, offset=self.off)
        self.off += nbytes
        return t


def _host_consts():
    c = {}
    c["ident"] = np.eye(128, dtype=np.float32)
    half = 64
    fr = (1.0 / (10000.0 ** (np.arange(0, half, 2, dtype=np.float32) / np.float32(half)))).astype(np.float32)
    t = np.arange(SEQ)
    row = (t // 64).astype(np.float32)
    col = (t % 64).astype(np.float32)
    ang = np.concatenate([row[:, None] * fr[None, :], col[:, None] * fr[None, :]], axis=-1).astype(np.float32)
    cos = np.concatenate([np.cos(ang), np.ones((CTX, 64), np.float32)], 0).astype(np.float32)
    sin = np.concatenate([np.sin(ang), np.zeros((CTX, 64), np.float32)], 0).astype(np.float32)

    def tl(a):
        return np.ascontiguousarray(a.reshape(NTILE, 128, -1).transpose(1, 0, 2))

    c["acos"] = tl(cos)
    c["asin"] = tl(sin)
    fr2 = (1.0 / (10000.0 ** (np.arange(0, 64, 2, dtype=np.float32) / np.float32(64)))).astype(np.float32)
    ang2 = (np.arange(SEQ, dtype=np.float32)[:, None] * fr2[None, :]).astype(np.float32)
    rc = np.concatenate([np.cos(ang2), np.ones((CTX, 32), np.float32)], 0).astype(np.float32)
    rs = np.concatenate([np.sin(ang2), np.zeros((CTX, 32), np.float32)], 0).astype(np.float32)
    c["rcos"] = tl(rc)
    c["rsin"] = tl(rs)
    c["rcosk"] = tl(rc * np.float32(0.125))
    c["rsink"] = tl(rs * np.float32(0.125))
    m = np.arange(128, dtype=np.float32)[:, None]
    l = np.arange(128, dtype=np.float32)[None, :]
    rt = np.zeros((128, 6, 128), np.float32)
    rt[:, 0, :] = np.maximum(l - m, 0)
    rt[:, 1, :] = (l >= m)
    rt[:, 2, :] = np.maximum(m - l, 0)
    rt[:, 3, :] = (m >= l)
    rt[:, 4, :] = l + 1.0
    rt[:, 5, :] = 128.0 - l
    c["rtab"] = rt
    pc = np.zeros((128, 2), np.float32)
    pc[:, 0] = 127.0 - np.arange(128)
    pc[:, 1] = np.arange(128)
    c["pcol"] = pc
    pe = np.ones((128, 4, 2, 8), np.float32)
    for gi, w in enumerate((2, 4, 8, 16)):
        hw = w // 2
        for j in range(hw):
            pe[:, gi, 0, j] = w / float(j + hw)
            cnt = min(j + 1 + hw, w)
            pe[:, gi, 1, hw - 1 - j] = w / float(cnt)
    c["pedge"] = pe
    c["tri"] = np.triu(np.ones((128, 128), np.float32), 1)
    io = np.zeros((128, 25), np.float32)
    io[:, 0:24] = np.arange(24, dtype=np.float32)[None, :]
    io[:, 24] = np.arange(128, dtype=np.float32)
    c["iot"] = io
    c["prow"] = np.ascontiguousarray(np.broadcast_to((NE * 1280.0 + np.arange(128, dtype=np.float32))[:, None], (128, NE)))
    c["ec"] = np.ascontiguousarray(np.broadcast_to((np.arange(NE, dtype=np.float32) * 1280.0)[None, :], (128, NE)))
    return c


def build(dbg=False, stop_after=None, skip=(), ext=None):
    nc = bass.Bass("TRN2", target_bir_lowering=False)
    P = Prog(nc, same_engine_sync=(os.environ.get('MK_SES', '1') == '1'))
    A = Arena(nc)

    def din(name, shape, dt=F32):
        kind = "ExternalInput" if (ext is None or name in ext) else "Internal"
        return nc.dram_tensor(name, list(shape), dt, kind=kind).ap()

    skind = "ExternalOutput" if dbg else "Internal"

    def dscr(name, shape, dt=F32):
        return nc.dram_tensor(name, list(shape), dt, kind=skind).ap()

    xin = din("xin", [NT, D])
    cT = din("cT", [128, 16])
    w_ada = din("w_ada", [2, D, INW])
    b_ada = din("b_ada", [2, INW])
    norm_mix = din("norm_mix", [2, D])
    norm_ffn = din("norm_ffn", [2, D])
    w_in = din("w_in", [2, D, INW])
    ret_decay = din("ret_decay", [2, 8])
    ret_gn = din("ret_gn", [2, 512])
    attn_qn = din("attn_qn", [2, 128])
    attn_kn = din("attn_kn", [2, 128])
    pool_w = din("pool_w", [2, 4, 128, 128])
    pool_scale = din("pool_scale", [2, 512])
    w_branch = din("w_branch", [2, 3, 512, D])
    w_out = din("w_out", [2, D, D])
    ffn_w1 = din("ffn_w1", [1, D, FFN])
    ffn_w3 = din("ffn_w3", [1, D, FFN])
    ffn_w2 = din("ffn_w2", [1, FFN, D])
    moe_router_t = din("moe_router_t", [NE, D])
    moe_router_p = din("moe_router_p", [128, 8, NE])
    moe_router_b = din("moe_router_b", [1, NE])
    w1q = [[din("w1q_%d_%d" % (fb, h), [NE * 128, 2048]) for h in range(2)] for fb in range(7)]
    w3q = [[din("w3q_%d_%d" % (fb, h), [NE * 128, 2048]) for h in range(2)] for fb in range(7)]
    w2q = [[din("w2q_%d_%d" % (fb, h), [NE * 128, 2048]) for h in range(2)] for fb in range(7)]
    final_norm = din("final_norm", [D])
    k_ident = din("k_ident", [128, 128])
    k_acos = din("k_acos", [128, NTILE, 64])
    k_asin = din("k_asin", [128, NTILE, 64])
    k_rt4 = din("k_rt4", [128, NTILE, 4, 32])
    k_rtab = din("k_rtab", [128, 6, 128])
    k_pcol = din("k_pcol", [128, 2])
    k_pedge = din("k_pedge", [128, 4, 2, 8])
    k_tri = din("k_tri", [128, 128])
    k_iot = din("k_iot", [128, 25])

    out = nc.dram_tensor("out", [SEQ, D], F32, kind="ExternalOutput").ap()

    modd = dscr("modd", [2, 2, 6, 128, D])
    hT_d = dscr("hT_d", [128, 8, NT], BF16)
    yT_d = dscr("yT_d", [3, 128, 4, NT], BF16)
    xa_d = dscr("xa_d", [NT, D])
    xb_d = dscr("xb_d", [NT, D])
    xc_d = dscr("xc_d", [NT, D])
    xd_d = dscr("xd_d", [NT, D])
    G_d = dscr("G_d", [24 * 512, D], BF16)
    Y_d = dscr("Y_d", [24 * 512, D])

    ps = [nc.alloc_psum_tensor("ps%d" % i, [128, 512], F32) for i in range(8)]
    PS = ["ps%d" % i for i in range(8)]

    GROUPS = [(g * 512, 512) for g in range(8)] + [(SEQ, CTX)]

    ident = A.alloc("ident", [128, 128], BF16)
    identf = A.alloc("identf", [128, 128], F32)
    ones_bf = A.alloc("ones", [128, 128], BF16)
    P.dma("sync", identf[:], k_ident, writes=["identf"])
    P.op("vector", lambda e: e.tensor_copy(ident[:], identf[:]), reads=["identf"], writes=["ident"])
    P.op("vector", lambda e: e.memset(ones_bf[:], 1.0), writes=["ones"])
    P.barrier()
    A_PERSIST = A.mark()

    def done(tag):
        return stop_after is not None and stop_after == tag

    def finish():
        P.mute = False
        P.barrier()
        P.emit()
        return nc

    def make_normT(pfx, Amod, Bmod, psbank):
        xt = [A.alloc(pfx + "xt%d" % i, [128, D], F32) for i in range(2)]
        junks = [A.alloc(pfx + "junk%d" % i, [128, D], BF16) for i in range(2)]
        tmps = [A.alloc(pfx + "tmp%d" % i, [128, D], F32) for i in range(2)]
        hb = [A.alloc(pfx + "hb%d" % i, [128, D], BF16) for i in range(4)]
        sts = [A.alloc(pfx + "st%d" % i, [128, 4], F32) for i in range(2)]
        cnt = [0]

        def runA(src_d, t0, ntl, which):
            hs = []
            for j in range(ntl):
                i = cnt[0]
                cnt[0] += 1
                s = i % 2
                x_t, h_b = xt[s], hb[i % 4]
                junk, tmp, st = junks[s], tmps[s], sts[s]
                xtok, htok = pfx + "xt%d" % s, pfx + "hb%d" % (i % 4)
                sfx = "_%d" % s
                r0 = t0 + j * 128
                P.dma("sync", x_t[:], src_d[r0:r0 + 128, :], writes=[xtok])
                P.op("scalar", lambda e, x_t=x_t, junk=junk, st=st: e.activation(junk[:], x_t[:], AF.Square, accum_out=st[:, 0:1]),
                     reads=[xtok], writes=[pfx + "junk" + sfx, pfx + "st" + sfx])
                P.op("scalar", lambda e, st=st: e.activation(st[:, 1:2], st[:, 0:1], AF.Sqrt, bias=NORM_EPS, scale=1.0 / D),
                     reads=[pfx + "st" + sfx], writes=[pfx + "st1" + sfx])
                P.op("vector", lambda e, st=st: e.reciprocal(st[:, 2:3], st[:, 1:2]),
                     reads=[pfx + "st1" + sfx], writes=[pfx + "st2" + sfx])
                am, bm = Amod[which], Bmod[which]
                P.op("vector", lambda e, x_t=x_t, am=am, tmp=tmp, st=st: e.scalar_tensor_tensor(tmp[:], x_t[:], st[:, 2:3], am[0][:], ALU.mult, ALU.mult),
                     reads=[xtok, pfx + "st2" + sfx, am[1]], writes=[pfx + "tmp" + sfx])
                P.op("gpsimd", lambda e, h_b=h_b, bm=bm, tmp=tmp: e.tensor_tensor(h_b[:], tmp[:], bm[0][:], ALU.add),
                     reads=[pfx + "tmp" + sfx, bm[1]], writes=[htok])
                hs.append((h_b, htok))
            return hs

        def runB(hs, dst, dst_tok):
            pb = ps[psbank][:].bitcast(BF16)
            for j, (h_b, htok) in enumerate(hs):
                for k in range(8):
                    P.op("tensor", lambda e, k=k, h_b=h_b: e.transpose(pb[:, k * 128:(k + 1) * 128], h_b[:, k * 128:(k + 1) * 128], ident[:]),
                         reads=[htok, "ident"], writes=[PS[psbank]])
                P.op("scalar", lambda e, j=j: e.copy(dst[:, :, j * 128:(j + 1) * 128], pb[:, 0:1024].rearrange("p (k t) -> p k t", k=8)),
                     reads=[PS[psbank]], writes=[dst_tok])

        def run(src_d, t0, ntl, dst, dst_tok, which):
            prev = None
            for j in range(ntl):
                cur = (runA(src_d, t0 + j * 128, 1, which), j)
                if prev is not None:
                    runB(prev[0], dst[:, :, prev[1] * 128:(prev[1] + 1) * 128], dst_tok)
                prev = cur
            runB(prev[0], dst[:, :, prev[1] * 128:(prev[1] + 1) * 128], dst_tok)

        return runA, runB, run

    def load_mod(L, idxs):
        res = {}
        for idx in idxs:
            pair = []
            for w in range(2):
                t = A.alloc("mod%d_%d" % (idx, w), [128, D], F32)
                tok = "mod%d_%d" % (idx, w)
                P.dma("sync", t[:], modd[L, w, idx], reads=[("modd", L, w, idx)], writes=[tok])
                pair.append((t, tok))
            res[idx] = pair
        return res

    P.mute = "mod" in skip
    A.reset(A_PERSIST)
    cs = A.alloc("cs", [128, 16], F32)
    ss = A.alloc("ss", [128, 16], F32)
    sbl = A.alloc("sbl", [128, 16, 128], BF16)
    P.dma("sync", cs[:], cT, writes=["cs"])
    P.op("scalar", lambda e: e.activation(ss[:], cs[:], AF.Silu), reads=["cs"], writes=["ss"])
    P.op("vector", lambda e: e.tensor_copy(sbl[:], ss[:].unsqueeze(2).to_broadcast([128, 16, 128])), reads=["ss"], writes=["sbl"])
    wad = [A.alloc("wad%d" % i, [128, 8, 512], BF16) for i in range(2)]
    bt = [A.alloc("bt%d" % i, [128, 512], F32) for i in range(2)]
    gn = [A.alloc("gnm%d" % i, [128, D], F32) for i in range(2)]
    mo = [A.alloc("mo%d" % i, [128, 512], F32) for i in range(4)]
    it = 0
    for L in range(2):
        P.dma("sync", gn[0][:], norm_mix[L].partition_broadcast(128), writes=["gn0"])
        P.dma("sync", gn[1][:], norm_ffn[L].partition_broadcast(128), writes=["gn1"])
        for cb in range(12):
            s = it % 2
            it += 1
            idx = cb // 2
            c0 = (cb % 2) * 512
            P.dma("gpsimd", wad[s][:], w_ada[L, :, cb * 512:(cb + 1) * 512].rearrange("(k p) f -> p k f", p=128), writes=["wad%d" % s])
            P.dma("sync", bt[s][:], b_ada[L, cb * 512:(cb + 1) * 512].partition_broadcast(128), writes=["bt%d" % s])
            for w in range(2):
                bank = 2 * s + w
                for k in range(8):
                    P.op("tensor", lambda e, k=k, w=w, s=s, bank=bank: e.matmul(ps[bank][:], sbl[:, w * 8 + k, :], wad[s][:, k, :], start=(k == 0), stop=(k == 7)),
                         reads=["sbl", "wad%d" % s], writes=[PS[bank]])
                m = mo[bank]
                mtok = "mo%d" % bank
                P.op("vector", lambda e, m=m, bank=bank, s=s: e.tensor_tensor(m[:], ps[bank][:], bt[s][:], ALU.add),
                     reads=[PS[bank], "bt%d" % s], writes=[mtok])
                if idx in (1, 4):
                    g = gn[0] if idx == 1 else gn[1]
                    gtok = "gn0" if idx == 1 else "gn1"
                    P.op("vector", lambda e, m=m, g=g, c0=c0: e.scalar_tensor_tensor(m[:], m[:], 1.0, g[:, c0:c0 + 512], ALU.add, ALU.mult),
                         reads=[mtok, gtok], writes=[mtok])
                P.dma("sync", modd[L, w, idx, :, c0:c0 + 512], m[:], reads=[mtok], writes=[("modd", L, w, idx, c0)])
    P.barrier()
    if done("mod"):
        return finish()

    def layer(L, x_src, x_mix, x_out, need_ctx, moe):
        ngrp = 9
        P.mute = "p1" in skip
        A.reset(A_PERSIST)
        md = load_mod(L, [0, 1])
        _, _, normT = make_normT("p1", md[1], md[0], 0)
        hTt = [A.alloc("hTt%d" % i, [128, 8, 512], BF16) for i in range(2)]
        for g, (t0, n) in enumerate(GROUPS):
            s = g % 2
            normT(x_src, t0, n // 128, hTt[s], "hTt%d" % s, 0 if g < 8 else 1)
            P.dma("sync", hT_d[:, :, t0:t0 + n], hTt[s][:, :, 0:n], reads=["hTt%d" % s], writes=[("hT", g)])
        P.barrier()
        if done("p1_%d" % L):
            return True

        P.mute = "p2" in skip
        A.reset(A_PERSIST)
        Wa = A.alloc("Wa", [128, 8, 1024], BF16)
        for cbk in range(2):
            P.dma("gpsimd", Wa[:, :, cbk * 512:(cbk + 1) * 512], w_in[L, :, 1536 + cbk * 512:1536 + (cbk + 1) * 512].rearrange("(k p) f -> p k f", p=128), writes=[("Wa", cbk)])
        QT = A.alloc("QT", [128, 4, NT], BF16)
        KT = A.alloc("KT", [128, 2, NT], BF16)
        Vres = A.alloc("Vres", [128, NTILE, 256], BF16)
        acos = A.alloc("acos", [128, NTILE, 64], F32)
        asin = A.alloc("asin", [128, NTILE, 64], F32)
        P.dma("sync", acos[:], k_acos, writes=["acos"])
        P.dma("sync", asin[:], k_asin, writes=["asin"])
        gq = A.alloc("gq", [128, 128], F32)
        gk = A.alloc("gk", [128, 128], F32)
        P.dma("sync", gq[:], attn_qn[L].partition_broadcast(128), writes=["gq"])
        P.dma("sync", gk[:], attn_kn[L].partition_broadcast(128), writes=["gk"])
        m_prep = A.mark()
        hg = [A.alloc("hg%d" % i, [128, 8, 512], BF16) for i in range(2)]
        sqs = [A.alloc("sq%d" % i, [128, 768], F32) for i in range(2)]
        st6s = [A.alloc("st6_%d" % i, [128, 6], F32) for i in range(2)]
        st6bs = [A.alloc("st6b%d" % i, [128, 6], F32) for i in range(2)]
        rs6s = [A.alloc("rs6_%d" % i, [128, 6], F32) for i in range(2)]
        qns = [A.alloc("qn%d" % i, [128, 6, 128], F32) for i in range(2)]
        tas = [A.alloc("ta%d" % i, [128, 6, 64], F32) for i in range(2)]
        tbs = [A.alloc("tb%d" % i, [128, 6, 64], F32) for i in range(2)]
        tcs = [A.alloc("tc%d" % i, [128, 6, 64], F32) for i in range(2)]
        tds = [A.alloc("td%d" % i, [128, 6, 64], F32) for i in range(2)]
        qrs = [A.alloc("qr%d" % i, [128, 6, 128], BF16) for i in range(2)]
        prev_gen = [None]
        for g, (t0, n) in enumerate(GROUPS):
            s = g % 2
            P.dma("sync", hg[s][:, :, 0:n], hT_d[:, :, t0:t0 + n], reads=[("hT", g)], writes=["hg%d" % s])
            for j in range(n // 128):
                def tile_body(g=g, s=s, j=j, ti=(t0 // 128) + j):
                    u = ti % 2
                    sfx = "_%d" % u
                    sq, st6, st6b, rs6, qn = sqs[u], st6s[u], st6bs[u], rs6s[u], qns[u]
                    ta, tb, tc_, td, qr = tas[u], tbs[u], tcs[u], tds[u], qrs[u]
                    bk0, bk1, bk2 = (0, 1, 2) if u == 0 else (3, 4, 5)
                    for k in range(8):
                        P.op("tensor", lambda e, k=k, s=s, j=j: e.matmul(ps[bk0][:], hg[s][:, k, j * 128:(j + 1) * 128], Wa[:, k, 0:512], start=(k == 0), stop=(k == 7)),
                             reads=["hg%d" % s, ("Wa", 0)], writes=[PS[bk0]])
                    for k in range(8):
                        P.op("tensor", lambda e, k=k, s=s, j=j: e.matmul(ps[bk1][:], hg[s][:, k, j * 128:(j + 1) * 128], Wa[:, k, 512:1024], start=(k == 0), stop=(k == 7)),
                             reads=["hg%d" % s, ("Wa", 1)], writes=[PS[bk1]])
                    P.op("scalar", lambda e: e.activation(sq[:, 0:512], ps[bk0][:], AF.Square), reads=[PS[bk0]], writes=["sq" + sfx])
                    P.op("scalar", lambda e: e.activation(sq[:, 512:768], ps[bk1][:, 0:256], AF.Square), reads=[PS[bk1]], writes=["sqb" + sfx])
                    P.op("vector", lambda e: e.reduce_sum(st6[:], sq[:].rearrange("p (h d) -> p h d", d=128), axis=AX.X),
                         reads=["sq" + sfx, "sqb" + sfx], writes=["st6" + sfx])
                    P.op("scalar", lambda e: e.activation(st6b[:], st6[:], AF.Sqrt, bias=NORM_EPS, scale=1.0 / 128), reads=["st6" + sfx], writes=["st6b" + sfx])
                    P.op("vector", lambda e: e.reciprocal(rs6[:], st6b[:]), reads=["st6b" + sfx], writes=["rs6" + sfx])
                    P.op("vector", lambda e: e.tensor_tensor(qn[:, 0:4, :], ps[bk0][:].rearrange("p (h d) -> p h d", d=128),
                                                             rs6[:, 0:4].unsqueeze(2).to_broadcast([128, 4, 128]), ALU.mult),
                         reads=[PS[bk0], "rs6" + sfx], writes=["qn_q" + sfx])
                    P.op("vector", lambda e: e.tensor_tensor(qn[:, 4:6, :], ps[bk1][:, 0:256].rearrange("p (h d) -> p h d", d=128),
                                                             rs6[:, 4:6].unsqueeze(2).to_broadcast([128, 2, 128]), ALU.mult),
                         reads=[PS[bk1], "rs6" + sfx], writes=["qn_k" + sfx])
                    P.op("scalar", lambda e, ti=ti: e.copy(Vres[:, ti, :], ps[bk1][:, 256:512]), reads=[PS[bk1]], writes=[("Vres", ti)])
                    P.op("gpsimd", lambda e: e.tensor_tensor(qn[:, 0:4, :], qn[:, 0:4, :], gq[:].unsqueeze(1).to_broadcast([128, 4, 128]), ALU.mult),
                         reads=["qn_q" + sfx, "gq"], writes=["qn_q" + sfx])
                    P.op("gpsimd", lambda e: e.tensor_tensor(qn[:, 4:6, :], qn[:, 4:6, :], gk[:].unsqueeze(1).to_broadcast([128, 2, 128]), ALU.mult),
                         reads=["qn_k" + sfx, "gk"], writes=["qn_k" + sfx])
                    yield
                    x0 = qn[:, :, 0::2]
                    x1 = qn[:, :, 1::2]
                    cb_ = acos[:, ti, :].unsqueeze(1).to_broadcast([128, 6, 64])
                    sb_ = asin[:, ti, :].unsqueeze(1).to_broadcast([128, 6, 64])
                    P.op("vector", lambda e, x0=x0, cb_=cb_: e.tensor_tensor(ta[:], x0, cb_, ALU.mult), reads=["qn_q" + sfx, "qn_k" + sfx, "acos"], writes=["ta" + sfx])
                    P.op("gpsimd", lambda e, x1=x1, sb_=sb_: e.tensor_tensor(tb[:], x1, sb_, ALU.mult), reads=["qn_q" + sfx, "qn_k" + sfx, "asin"], writes=["tb" + sfx])
                    P.op("vector", lambda e, x0=x0, sb_=sb_: e.tensor_tensor(tc_[:], x0, sb_, ALU.mult), reads=["qn_q" + sfx, "qn_k" + sfx, "asin"], writes=["tc" + sfx])
                    P.op("gpsimd", lambda e, x1=x1, cb_=cb_: e.tensor_tensor(td[:], x1, cb_, ALU.mult), reads=["qn_q" + sfx, "qn_k" + sfx, "acos"], writes=["td" + sfx])
                    P.op("vector", lambda e: e.tensor_tensor(qr[:, :, 0::2], ta[:], tb[:], ALU.subtract), reads=["ta" + sfx, "tb" + sfx], writes=["qr0" + sfx])
                    P.op("gpsimd", lambda e: e.tensor_tensor(qr[:, :, 1::2], tc_[:], td[:], ALU.add), reads=["tc" + sfx, "td" + sfx], writes=["qr1" + sfx])
                    pbA = ps[bk2][:].bitcast(BF16)
                    for h in range(6):
                        P.op("tensor", lambda e, h=h: e.transpose(pbA[:, h * 128:(h + 1) * 128], qr[:, h, :], ident[:]),
                             reads=["qr0" + sfx, "qr1" + sfx, "ident"], writes=[PS[bk2]])
                    P.op("scalar", lambda e, ti=ti: e.copy(QT[:, :, ti * 128:(ti + 1) * 128], pbA[:, 0:512].rearrange("p (h t) -> p h t", h=4)),
                         reads=[PS[bk2]], writes=[("QT", ti)])
                    P.op("scalar", lambda e, ti=ti: e.copy(KT[:, :, ti * 128:(ti + 1) * 128], pbA[:, 512:768].rearrange("p (h t) -> p h t", h=2)),
                         reads=[PS[bk2]], writes=[("KT", ti)])
                gen_ = tile_body()
                next(gen_)
                if prev_gen[0] is not None:
                    for _ in prev_gen[0]:
                        pass
                prev_gen[0] = gen_
        for _ in prev_gen[0]:
            pass
        P.barrier()
        if done("p2a_%d" % L):
            return True
        A.reset(m_prep)
        pT = [A.alloc("pT%d" % i, [128, 512], BF16) for i in range(3)]
        rden = A.alloc("rden", [128, 512], F32)
        yo = [A.alloc("yo%d" % i, [128, 512], BF16) for i in range(2)]
        scale = float(128 ** -0.5)
        qgroups = list(range(8)) + ([8] if need_ctx else [])
        blocks = []
        oc = 0
        for g in qgroups:
            t0, n = GROUPS[g]
            keys = list(range(NTILE)) if g < 8 else [32, 33]
            for h in range(4):
                for ji, j in enumerate(keys):
                    blocks.append((g, t0, n, h, ji, j, len(keys), oc))
                oc += 1
        LOOK = 2

        def emit_score(bi):
            g, t0, n, h, ji, j, nk, oc_ = blocks[bi]
            sbk = bi % 3
            kvh = h // 2
            P.op("tensor", lambda e: e.matmul(ps[sbk][:, 0:n], KT[:, kvh, j * 128:(j + 1) * 128], QT[:, h, t0:t0 + n], start=True, stop=True),
                 reads=[], writes=[PS[sbk]])
            P.op("scalar", lambda e: e.activation(pT[sbk][:, 0:n], ps[sbk][:, 0:n], AF.Exp, scale=scale),
                 reads=[PS[sbk]], writes=["pT%d" % sbk])

        def emit_pv(bi):
            g, t0, n, h, ji, j, nk, oc_ = blocks[bi]
            sbk = bi % 3
            kvh = h // 2
            ob = 4 + (oc_ % 2) * 2
            db = ob + 1
            pt = pT[sbk]
            P.op("tensor", lambda e: e.matmul(ps[ob][:, 0:n], Vres[:, j, kvh * 128:(kvh + 1) * 128], pt[:, 0:n], start=(ji == 0), stop=(ji == nk - 1)),
                 reads=["pT%d" % sbk], writes=[PS[ob]])
            P.op("tensor", lambda e: e.matmul(ps[db][:, 0:n], ones_bf[:], pt[:, 0:n], start=(ji == 0), stop=(ji == nk - 1)),
                 reads=["pT%d" % sbk], writes=[PS[db]])
            if ji == nk - 1:
                y_o = yo[oc_ % 2]
                ytok = "yo%d" % (oc_ % 2)
                P.op("vector", lambda e: e.reciprocal(rden[:, 0:n], ps[db][:, 0:n]), reads=[PS[db]], writes=["rden"])
                P.op("vector", lambda e: e.tensor_tensor(y_o[:, 0:n], ps[ob][:, 0:n], rden[:, 0:n], ALU.mult),
                     reads=[PS[ob], "rden"], writes=[ytok])
                P.dma("sync", yT_d[1, :, h, t0:t0 + n], y_o[:, 0:n], reads=[ytok], writes=[("yT1", g, h)])

        for bi in range(len(blocks) + LOOK):
            if bi < len(blocks):
                emit_score(bi)
            if bi - LOOK >= 0:
                emit_pv(bi - LOOK)
        P.barrier()
        if done("p2_%d" % L):
            return True
        P.mute = "p3" in skip
        A.reset(A_PERSIST)
        Wr = A.alloc("Wr", [128, 8, 1536], BF16)
        m_wr = A.mark()
        for cbk in range(3):
            P.dma("gpsimd", Wr[:, :, cbk * 512:(cbk + 1) * 512], w_in[L, :, cbk * 512:(cbk + 1) * 512].rearrange("(k p) f -> p k f", p=128), writes=[("Wr", cbk)])
        QTr = A.alloc("QTr", [128, 2, NT], BF16)
        KTr = A.alloc("KTr", [128, 2, NT], BF16)
        Kres = A.alloc("Kres", [128, NTILE, 256], BF16)
        Vr = A.alloc("Vr", [128, NTILE, 512], BF16)
        SG = A.alloc("SG", [128, NTILE, 512], BF16)
        rtab = A.alloc("rtab", [128, 6, 128], F32)
        pcol = A.alloc("pcol", [128, 2], F32)
        rd8 = A.alloc("rd8", [128, 8], F32)
        lg8 = A.alloc("lg8", [128, 8], F32)
        lgp = A.alloc("lgp", [128, 4], F32)
        gch = A.alloc("gch", [128, 4], F32)
        Dcomb = A.alloc("Dcomb", [128, 4, 128], F32)
        XI = A.alloc("XI", [128, 4, 128], F32)
        ZZ = A.alloc("ZZ", [128, 2, 256], F32)
        XIm = A.alloc("XIm", [128, 6, 2, 128], F32)
        mk = A.alloc("mk", [128, 2], F32)
        P.op("vector", lambda e: e.memset(mk[:], 0.0), writes=["mk"])
        P.op("vector", lambda e: e.memset(mk[0:64, 0:1], 1.0), reads=["mk"], writes=["mk"])
        P.op("vector", lambda e: e.memset(mk[64:128, 1:2], 1.0), reads=["mk"], writes=["mk"])
        gnw = A.alloc("gnw", [128, 512], F32)
        t1 = A.alloc("t1", [128, 128], F32)
        t2 = A.alloc("t2", [128, 128], F32)
        P.dma("sync", rtab[:], k_rtab, writes=["rtab"])
        P.dma("sync", pcol[:], k_pcol, writes=["pcol"])
        P.dma("sync", rd8[:], ret_decay[L].partition_broadcast(128), writes=["rd8"])
        P.dma("sync", gnw[:], ret_gn[L].partition_broadcast(128), writes=["gnw"])
        P.op("scalar", lambda e: e.activation(lg8[:], rd8[:], AF.Exp), reads=["rd8"], writes=["lg8a"])
        P.op("vector", lambda e: e.tensor_scalar(lg8[:], lg8[:], -1.0, None, ALU.mult), reads=["lg8a"], writes=["lg8"])
        for j in range(2):
            for dr in range(2):
                P.op("vector", lambda e, j=j, dr=dr: e.tensor_copy(lgp[0:64, dr * 2 + j:dr * 2 + j + 1], lg8[0:64, dr * 4 + 2 * j:dr * 4 + 2 * j + 1]), reads=["lg8"], writes=[("lgp", j, dr, 0)])
                P.op("vector", lambda e, j=j, dr=dr: e.tensor_copy(lgp[64:128, dr * 2 + j:dr * 2 + j + 1], lg8[64:128, dr * 4 + 2 * j + 1:dr * 4 + 2 * j + 2]), reads=["lg8"], writes=[("lgp", j, dr, 1)])
        LGP = [("lgp", j, dr, hh) for j in range(2) for dr in range(2) for hh in range(2)]
        P.op("scalar", lambda e: e.activation(gch[:], lgp[:], AF.Exp, scale=128.0), reads=LGP, writes=["gch"])
        for h in range(4):
            P.op("scalar", lambda e, h=h: e.activation(t1[:], rtab[:, 0, :], AF.Exp, scale=lg8[:, h:h + 1]), reads=["rtab", "lg8"], writes=["t1"])
            P.op("vector", lambda e: e.tensor_tensor(t1[:], t1[:], rtab[:, 1, :], ALU.mult), reads=["t1", "rtab"], writes=["t1"])
            P.op("scalar", lambda e, h=h: e.activation(t2[:], rtab[:, 2, :], AF.Exp, scale=lg8[:, 4 + h:5 + h]), reads=["rtab", "lg8"], writes=["t2"])
            P.op("vector", lambda e: e.tensor_tensor(t2[:], t2[:], rtab[:, 3, :], ALU.mult), reads=["t2", "rtab"], writes=["t2"])
            P.op("vector", lambda e, h=h: e.tensor_tensor(Dcomb[:, h, :], t1[:], t2[:], ALU.add), reads=["t1", "t2"], writes=[("Dcomb", h)])
            for dr in range(2):
                P.op("scalar", lambda e, h=h, dr=dr: e.activation(ZZ[:, dr, h * 64:(h + 1) * 64], lg8[:, dr * 4 + h:dr * 4 + h + 1].to_broadcast([128, 64]), AF.Exp, scale=pcol[:, dr:dr + 1]),
                     reads=["lg8", "pcol"], writes=[("ZZ", dr, h)])
        for j in range(2):
            for dr in range(2):
                P.op("scalar", lambda e, j=j, dr=dr: e.activation(XI[:, dr * 2 + j, :], rtab[:, 4 + dr, :], AF.Exp, scale=lgp[:, dr * 2 + j:dr * 2 + j + 1]),
                     reads=["rtab"] + LGP, writes=[("XI", dr, j)])
        XIT = [("XI", dr, j) for dr in range(2) for j in range(2)]
        for hh in range(2):
            for dr in range(2):
                P.op("vector", lambda e, hh=hh, dr=dr: e.tensor_scalar(XIm[:, 2 * dr + hh], XI[:, dr * 2:dr * 2 + 2, :], mk[:, hh:hh + 1], None, ALU.mult), reads=XIT + ["mk"], writes=[("XIm", 2 * dr + hh)])
            P.op("vector", lambda e, hh=hh: e.tensor_copy(XIm[:, 4 + hh].rearrange("p j l -> p (j l)"), mk[:, hh:hh + 1].to_broadcast([128, 256])), reads=["mk"], writes=[("XIm", 4 + hh)])
        if done("p3a_%d" % L):
            return True
        m_rprep = A.mark()
        hgrs = [A.alloc("hgr%d" % i, [128, 8, 512], BF16) for i in range(2)]
        rt4 = [A.alloc("rt4_%d" % i, [128, 4, 32], F32) for i in range(2)]
        ras = [A.alloc("ra%d" % i, [128, 8, 32], F32) for i in range(2)]
        rbs = [A.alloc("rb%d" % i, [128, 8, 32], F32) for i in range(2)]
        rcs = [A.alloc("rc%d" % i, [128, 8, 32], F32) for i in range(2)]
        rdds = [A.alloc("rdd%d" % i, [128, 8, 32], F32) for i in range(2)]
        qrrs = [A.alloc("qrr%d" % i, [128, 256], BF16) for i in range(2)]
        prev_r = [None]

        def r_load(g_):
            t0_, n_ = GROUPS[g_]
            P.dma("sync", hgrs[g_ % 2][:, :, 0:n_], hT_d[:, :, t0_:t0_ + n_], reads=[("hT", g_)], writes=["hgr%d" % (g_ % 2)])

        r_load(0)
        for g, (t0, n) in enumerate(GROUPS):
            if g + 1 < len(GROUPS):
                r_load(g + 1)
            for j in range(n // 128):
                def r_tile(g=g, j=j, ti=(t0 // 128) + j):
                    hgr = hgrs[g % 2]
                    hgtok = "hgr%d" % (g % 2)
                    u = ti % 2
                    s = u
                    sfx = "_%d" % u
                    ra, rb, rc_, rdd, qrr = ras[u], rbs[u], rcs[u], rdds[u], qrrs[u]
                    b0 = 0 if u == 0 else 4
                    P.dma("sync", rt4[s][:], k_rt4[:, ti], writes=["rt4_%d" % s])
                    for blk in range(3):
                        for k in range(8):
                            P.op("tensor", lambda e, k=k, j=j, blk=blk: e.matmul(ps[b0 + blk][:], hgr[:, k, j * 128:(j + 1) * 128], Wr[:, k, blk * 512:(blk + 1) * 512], start=(k == 0), stop=(k == 7)),
                                 reads=[hgtok, ("Wr", blk)], writes=[PS[b0 + blk]])
                    P.op("scalar", lambda e, ti=ti: e.copy(Vr[:, ti, :], ps[b0 + 1][:]), reads=[PS[b0 + 1]], writes=[("Vr", ti)])
                    P.op("scalar", lambda e, ti=ti: e.activation(SG[:, ti, :], ps[b0 + 2][:], AF.Silu), reads=[PS[b0 + 2]], writes=[("SG", ti)])
                    pv = ps[b0][:].rearrange("p (h d) -> p h d", d=64)
                    for hs, (ci, si) in ((slice(0, 4), (0, 1)), (slice(4, 8), (2, 3))):
                        x0 = pv[:, hs, 0::2]
                        x1 = pv[:, hs, 1::2]
                        cb_ = rt4[s][:, ci, :].unsqueeze(1).to_broadcast([128, 4, 32])
                        sb_ = rt4[s][:, si, :].unsqueeze(1).to_broadcast([128, 4, 32])
                        tk = "q" if ci == 0 else "k"
                        P.op("vector", lambda e, x0=x0, cb_=cb_, hs=hs: e.tensor_tensor(ra[:, hs, :], x0, cb_, ALU.mult), reads=[PS[b0], "rt4_%d" % s], writes=["ra" + tk + sfx])
                        P.op("vector", lambda e, x1=x1, sb_=sb_, hs=hs: e.tensor_tensor(rb[:, hs, :], x1, sb_, ALU.mult), reads=[PS[b0], "rt4_%d" % s], writes=["rb" + tk + sfx])
                        P.op("vector", lambda e, x0=x0, sb_=sb_, hs=hs: e.tensor_tensor(rc_[:, hs, :], x0, sb_, ALU.mult), reads=[PS[b0], "rt4_%d" % s], writes=["rc" + tk + sfx])
                        P.op("vector", lambda e, x1=x1, cb_=cb_, hs=hs: e.tensor_tensor(rdd[:, hs, :], x1, cb_, ALU.mult), reads=[PS[b0], "rt4_%d" % s], writes=["rd" + tk + sfx])
                    yield
                    qv = qrr[:].rearrange("p (h d) -> p h d", d=64)
                    kv_ = Kres[:, ti, :].rearrange("p (h d) -> p h d", d=64)
                    P.op("gpsimd", lambda e, qv=qv: e.tensor_tensor(qv[:, :, 0::2], ra[:, 0:4, :], rb[:, 0:4, :], ALU.subtract), reads=["raq" + sfx, "rbq" + sfx], writes=["qrr0" + sfx])
                    P.op("gpsimd", lambda e, qv=qv: e.tensor_tensor(qv[:, :, 1::2], rc_[:, 0:4, :], rdd[:, 0:4, :], ALU.add), reads=["rcq" + sfx, "rdq" + sfx], writes=["qrr1" + sfx])
                    P.op("gpsimd", lambda e, kv_=kv_: e.tensor_tensor(kv_[:, :, 0::2], ra[:, 4:8, :], rb[:, 4:8, :], ALU.subtract), reads=["rak" + sfx, "rbk" + sfx], writes=[("Kres0", ti)])
                    P.op("gpsimd", lambda e, kv_=kv_: e.tensor_tensor(kv_[:, :, 1::2], rc_[:, 4:8, :], rdd[:, 4:8, :], ALU.add), reads=["rck" + sfx, "rdk" + sfx], writes=[("Kres1", ti)])
                    pbB = ps[b0 + 3][:].bitcast(BF16)
                    for jj in range(2):
                        P.op("tensor", lambda e, jj=jj: e.transpose(pbB[:, jj * 128:(jj + 1) * 128], qrr[:, jj * 128:(jj + 1) * 128], ident[:]), reads=["qrr0" + sfx, "qrr1" + sfx, "ident"], writes=[PS[b0 + 3]])
                    for jj in range(2):
                        P.op("tensor", lambda e, jj=jj, ti=ti: e.transpose(pbB[:, 256 + jj * 128:256 + (jj + 1) * 128], Kres[:, ti, jj * 128:(jj + 1) * 128], ident[:]), reads=[("Kres0", ti), ("Kres1", ti), "ident"], writes=[PS[b0 + 3]])
                    P.op("scalar", lambda e, ti=ti: e.copy(QTr[:, :, ti * 128:(ti + 1) * 128], pbB[:, 0:256].rearrange("p (h t) -> p h t", h=2)), reads=[PS[b0 + 3]], writes=[("QTr", ti)])
                    P.op("scalar", lambda e, ti=ti: e.copy(KTr[:, :, ti * 128:(ti + 1) * 128], pbB[:, 256:512].rearrange("p (h t) -> p h t", h=2)), reads=[PS[b0 + 3]], writes=[("KTr", ti)])
                gen_ = r_tile()
                next(gen_)
                if prev_r[0] is not None:
                    for _ in prev_r[0]:
                        pass
                prev_r[0] = gen_
        for _ in prev_r[0]:
            pass
        P.barrier()
        if done("p3b_%d" % L):
            return True
        A.reset(m_rprep)
        Sb_all = nc.alloc_sbuf_tensor_at("Sb_all_%d" % L, [128, NTILE, 2, 128], BF16, offset=A_PERSIST)
        Sf = A.alloc("Sf", [128, 2, 128], F32)
        Sb = A.alloc("Sb", [128, 2, 128], F32)
        Sfb = [A.alloc("Sfb%d" % i, [128, 2, 128], BF16) for i in range(2)]
        kz = [A.alloc("kz%d" % i, [128, 256], BF16) for i in range(2)]
        PTs = [A.alloc("PT%d" % i, [128, 512], BF16) for i in range(2)]
        qms = [[A.alloc("qm%d_%d" % (i, q), [128, 2, 128], BF16) for i in range(6)] for q in range(2)]
        sqrs = [A.alloc("sqr%d" % i, [128, 512], F32) for i in range(2)]
        onrs = [A.alloc("onr%d" % i, [128, 512], F32) for i in range(2)]
        osbs = [A.alloc("osb%d" % i, [128, 512], F32) for i in range(2)]
        gss = [A.alloc("gs%d" % i, [128, 24], F32) for i in range(2)]
        yrbs = [A.alloc("yrb%d" % i, [128, 512], BF16) for i in range(2)]
        yrT = [A.alloc("yrT%d" % i, [128, 4, 512], BF16) for i in range(2)]
        P.op("vector", lambda e: e.memset(Sf[:], 0.0), writes=["Sf"])
        P.op("vector", lambda e: e.memset(Sb[:], 0.0), writes=["Sb"])

        def state_update(S, Stok, dr, c, bank):
            kzt = kz[dr]
            P.op("gpsimd", lambda e: e.tensor_tensor(kzt[:], Kres[:, c, :], ZZ[:, dr, :], ALU.mult), reads=[], writes=["kz%d" % dr])
            for h in range(4):
                P.op("tensor", lambda e, h=h: e.matmul(ps[bank][:, h * 128:(h + 1) * 128], kzt[:, (h // 2) * 128:(h // 2 + 1) * 128], Vr[:, c, h * 128:(h + 1) * 128], start=True, stop=True),
                     reads=["kz%d" % dr], writes=[PS[bank]])
            for j in range(2):
                for hh in range(2):
                    r0 = hh * 64
                    h = 2 * j + hh
                    P.op("vector", lambda e, j=j, r0=r0, h=h: e.scalar_tensor_tensor(S[r0:r0 + 64, j, :], S[r0:r0 + 64, j, :], gch[r0:r0 + 64, dr * 2 + j:dr * 2 + j + 1],
                                                                                   ps[bank][r0:r0 + 64, h * 128:(h + 1) * 128], ALU.mult, ALU.add),
                         reads=[PS[bank], Stok], writes=[Stok])

        order_b = [33, 32] + list(range(31, -1, -1))
        order_f = [32, 33] + list(range(32))
        for c in order_b:
            P.op("vector", lambda e, c=c: e.tensor_copy(Sb_all[:, c], Sb[:]), reads=["Sb"], writes=[("Sb_all", c)])
            state_update(Sb, "Sb", 1, c, 4)
        if done("p3c_%d" % L):
            return True
        yi = 0
        yi_box = [0]
        prev_fw = [None]
        for ci, c in enumerate(order_f):
            def fwd_chunk(ci=ci, c=c):
                u = ci % 2
                sfx = "_%d" % u
                PT, qm, sqr, onr, osb, gs, yrb = PTs[u], qms[u], sqrs[u], onrs[u], osbs[u], gss[u], yrbs[u]
                bR, bO, bT = (0, 1, 2) if u == 0 else (4, 5, 6)
                sfb = Sfb[ci % 2]
                sftok = "Sfb%d" % (ci % 2)
                P.op("vector", lambda e, sfb=sfb: e.tensor_copy(sfb[:], Sf[:]), reads=["Sf"], writes=[sftok])
                emit_out = (c < 32) or need_ctx
                if emit_out:
                    for idx in range(6):
                        eng = "vector" if idx % 2 == 0 else "gpsimd"
                        P.op(eng, lambda e, idx=idx, c=c: e.tensor_tensor(qm[idx][:], QTr[:, :, c * 128:(c + 1) * 128], XIm[:, idx], ALU.mult), reads=[], writes=[("qm", idx, u)])
                    for h in range(4):
                        j, hh = h // 2, h % 2
                        P.op("tensor", lambda e, h=h, j=j, hh=hh, c=c: e.matmul(ps[bR][:, h * 128:(h + 1) * 128], KTr[:, j, c * 128:(c + 1) * 128], qm[4 + hh][:, j, :], start=True, stop=True),
                             reads=[("qm", 4 + hh, u)], writes=[PS[bR]])
                    P.op("vector", lambda e: e.tensor_tensor(PT[:], ps[bR][:], Dcomb[:].rearrange("p h l -> p (h l)"), ALU.mult), reads=[PS[bR]], writes=["PT" + sfx])
                    for h in range(4):
                        j, hh = h // 2, h % 2
                        P.op("tensor", lambda e, h=h, c=c: e.matmul(ps[bO][:, h * 128:(h + 1) * 128], PT[:, h * 128:(h + 1) * 128], Vr[:, c, h * 128:(h + 1) * 128], start=True, stop=False),
                             reads=["PT" + sfx], writes=[PS[bO]])
                        P.op("tensor", lambda e, h=h, j=j, hh=hh, sfb=sfb: e.matmul(ps[bO][:, h * 128:(h + 1) * 128], qm[hh][:, j, :], sfb[:, j, :], start=False, stop=False),
                             reads=[("qm", hh, u), sftok], writes=[PS[bO]])
                        P.op("tensor", lambda e, h=h, j=j, hh=hh, c=c: e.matmul(ps[bO][:, h * 128:(h + 1) * 128], qm[2 + hh][:, j, :], Sb_all[:, c, j, :], start=False, stop=True),
                             reads=[("qm", 2 + hh, u), ("Sb_all", c)], writes=[PS[bO]])
                    state_update(Sf, "Sf", 0, c, 3 if u == 0 else 7)
                    yield
                    yi = yi_box[0]
                    ov = ps[bO][:].rearrange("p (h e) -> p h e", e=128)
                    P.op("scalar", lambda e: e.activation(sqr[:], ps[bO][:], AF.Square), reads=[PS[bO]], writes=["sqr" + sfx])
                    P.op("scalar", lambda e: e.copy(osb[:], ps[bO][:]), reads=[PS[bO]], writes=["osb" + sfx])
                    ov = osb[:].rearrange("p (h e) -> p h e", e=128)
                    P.op("vector", lambda e, ov=ov: e.reduce_sum(gs[:, 0:4], ov, axis=AX.X), reads=["osb" + sfx], writes=["gs0" + sfx])
                    P.op("vector", lambda e: e.reduce_sum(gs[:, 4:8], sqr[:].rearrange("p (h e) -> p h e", e=128), axis=AX.X), reads=["sqr" + sfx], writes=["gs1" + sfx])
                    P.op("vector", lambda e: e.tensor_scalar(gs[:, 8:12], gs[:, 0:4], 1.0 / 128, None, ALU.mult), reads=["gs0" + sfx], writes=["gs2" + sfx])
                    P.op("vector", lambda e: e.tensor_tensor(gs[:, 12:16], gs[:, 8:12], gs[:, 8:12], ALU.mult), reads=["gs2" + sfx], writes=["gs3" + sfx])
                    P.op("vector", lambda e: e.scalar_tensor_tensor(gs[:, 16:20], gs[:, 4:8], 1.0 / 128, gs[:, 12:16], ALU.mult, ALU.subtract), reads=["gs1" + sfx, "gs3" + sfx], writes=["gs4" + sfx])
                    P.op("scalar", lambda e: e.activation(gs[:, 20:24], gs[:, 16:20], AF.Sqrt, bias=GN_EPS, scale=1.0), reads=["gs4" + sfx], writes=["gs5" + sfx])
                    P.op("vector", lambda e: e.reciprocal(gs[:, 20:24], gs[:, 20:24]), reads=["gs5" + sfx], writes=["gs5" + sfx])
                    onv = onr[:].rearrange("p (h e) -> p h e", e=128)
                    P.op("vector", lambda e, ov=ov, onv=onv: e.tensor_tensor(onv, ov, gs[:, 8:12].unsqueeze(2).to_broadcast([128, 4, 128]), ALU.subtract), reads=["osb" + sfx, "gs2" + sfx], writes=["onr" + sfx])
                    P.op("gpsimd", lambda e, onv=onv: e.tensor_tensor(onv, onv, gs[:, 20:24].unsqueeze(2).to_broadcast([128, 4, 128]), ALU.mult), reads=["onr" + sfx, "gs5" + sfx], writes=["onr" + sfx])
                    P.op("gpsimd", lambda e: e.tensor_tensor(onr[:], onr[:], gnw[:], ALU.mult), reads=["onr" + sfx], writes=["onr" + sfx])
                    P.op("vector", lambda e, c=c: e.tensor_tensor(yrb[:], onr[:], SG[:, c, :], ALU.mult), reads=["onr" + sfx], writes=["yrb" + sfx])
                    pbC = ps[bT][:].bitcast(BF16)
                    for h in range(4):
                        P.op("tensor", lambda e, h=h: e.transpose(pbC[:, h * 128:(h + 1) * 128], yrb[:, h * 128:(h + 1) * 128], ident[:]), reads=["yrb" + sfx], writes=[PS[bT]])
                    g = c // 4 if c < 32 else 8
                    t0, n = GROUPS[g]
                    col = (c * 128 - t0)
                    yt = yrT[yi % 2]
                    ytok = "yrT%d" % (yi % 2)
                    P.op("scalar", lambda e, yt=yt, col=col: e.copy(yt[:, :, col:col + 128], pbC[:, 0:512].rearrange("p (h t) -> p h t", h=4)), reads=[PS[bT]], writes=[(ytok, col)])
                    if col + 128 == n:
                        P.dma("sync", yT_d[0, :, :, t0:t0 + n], yt[:, :, 0:n], reads=[(ytok, cc) for cc in range(0, n, 128)], writes=[("yT0", g)])
                        yi_box[0] += 1
                else:
                    state_update(Sf, "Sf", 0, c, 3 if u == 0 else 7)
                    yield
            gen_ = fwd_chunk()
            next(gen_)
            if prev_fw[0] is not None:
                for _ in prev_fw[0]:
                    pass
            prev_fw[0] = gen_
        for _ in prev_fw[0]:
            pass
        P.barrier()
        if done("p3_%d" % L):
            return True

        P.mute = "p4" in skip
        A.reset(A_PERSIST)
        LP = 16 + SEQ + 16 + CTX + 16
        OFFL, OFFC = 16, 16 + SEQ + 16
        Wp = A.alloc("Wp", [128, 8, 512], BF16)
        P.dma("gpsimd", Wp[:], w_in[L, :, 2560:3072].rearrange("(k p) f -> p k f", p=128), writes=["Wp"])
        pw = A.alloc("pw", [128, 4, 128], BF16)
        P.dma("gpsimd", pw[:], pool_w[L].rearrange("g c d -> c g d"), writes=["pw"])
        psc = A.alloc("psc", [128, 4], F32)
        for gi in range(4):
            P.dma("sync", psc[:, gi:gi + 1], pool_scale[L, gi * 128:(gi + 1) * 128].rearrange("(p o) -> p o", o=1), writes=[("psc", gi)])
        pedge = A.alloc("pedge", [128, 4, 2, 8], F32)
        P.dma("sync", pedge[:], k_pedge, writes=["pedge"])
        U = A.alloc("U", [128, 4, LP], F32)
        B1 = A.alloc("B1", [128, LP], F32)
        B2 = A.alloc("B2", [128, LP], F32)
        dT = A.alloc("dT", [128, 4, NT], BF16)
        hgp = [A.alloc("hgp%d" % i, [128, 8, 512], BF16) for i in range(2)]
        yp = [A.alloc("yp%d" % i, [128, 4, 512], BF16) for i in range(2)]
        P.op("gpsimd", lambda e: e.memset(U[:], 0.0), writes=["U"])
        P.op("vector", lambda e: e.memset(B1[:], 0.0), writes=["B1"])
        P.op("vector", lambda e: e.memset(B2[:], 0.0), writes=["B2"])
        for g, (t0, n) in enumerate(GROUPS):
            s = g % 2
            P.dma("sync", hgp[s][:, :, 0:n], hT_d[:, :, t0:t0 + n], reads=[("hT", g)], writes=["hgp%d" % s])
            c0 = OFFL + t0 if g < 8 else OFFC
            for gi in range(4):
                for k in range(8):
                    P.op("tensor", lambda e, k=k, gi=gi, s=s, n=n: e.matmul(ps[gi][:, 0:n], Wp[:, k, gi * 128:(gi + 1) * 128], hgp[s][:, k, 0:n], start=(k == 0), stop=(k == 7)),
                         reads=["hgp%d" % s, "Wp"], writes=[PS[gi]])
                P.op("scalar", lambda e, gi=gi, c0=c0, n=n: e.copy(U[:, gi, c0:c0 + n], ps[gi][:, 0:n]), reads=[PS[gi], "U"], writes=[("U", gi, g)])
        P.barrier()
        for gi, w in enumerate((2, 4, 8, 16)):
            hw = w // 2
            eng = "vector" if gi % 2 == 0 else "gpsimd"
            Ug = U[:, gi, :]
            P.op(eng, lambda e, Ug=Ug: e.tensor_tensor(B1[:, 1:LP], Ug[:, 1:LP], Ug[:, 0:LP - 1], ALU.add), writes=["B1"])
            cur, ctok = B1, "B1"
            if w >= 4:
                P.op(eng, lambda e: e.tensor_tensor(B2[:, 1:LP - 1], B1[:, 0:LP - 2], B1[:, 2:LP], ALU.add), reads=["B1"], writes=["B2"])
                cur, ctok = B2, "B2"
            if w >= 8:
                P.op(eng, lambda e: e.tensor_tensor(B1[:, 2:LP - 2], B2[:, 0:LP - 4], B2[:, 4:LP], ALU.add), reads=["B2"], writes=["B1"])
                cur, ctok = B1, "B1"
            if w >= 16:
                P.op(eng, lambda e: e.tensor_tensor(B2[:, 4:LP - 4], B1[:, 0:LP - 8], B1[:, 8:LP], ALU.add), reads=["B1"], writes=["B2"])
                cur, ctok = B2, "B2"
            for (o0, nn, d0) in ((OFFL, SEQ, 0), (OFFC, CTX, SEQ)):
                P.op(eng, lambda e, cur=cur, o0=o0, gi=gi, hw=hw: e.tensor_tensor(cur[:, o0:o0 + hw], cur[:, o0:o0 + hw], pedge[:, gi, 0, 0:hw], ALU.mult), reads=[ctok, "pedge"], writes=[ctok])
                P.op(eng, lambda e, cur=cur, o0=o0, nn=nn, gi=gi, hw=hw: e.tensor_tensor(cur[:, o0 + nn - hw:o0 + nn], cur[:, o0 + nn - hw:o0 + nn], pedge[:, gi, 1, 0:hw], ALU.mult), reads=[ctok, "pedge"], writes=[ctok])
                P.op("vector", lambda e, cur=cur, o0=o0, nn=nn, d0=d0, gi=gi, w=w, Ug=Ug: e.scalar_tensor_tensor(dT[:, gi, d0:d0 + nn], cur[:, o0:o0 + nn], 1.0 / w, Ug[:, o0:o0 + nn], ALU.mult, ALU.subtract),
                     reads=[ctok], writes=[("dT", gi, d0)])
        P.barrier()
        for g, (t0, n) in enumerate(GROUPS):
            s = g % 2
            for gi in range(4):
                P.op("tensor", lambda e, gi=gi, t0=t0, n=n: e.matmul(ps[gi][:, 0:n], pw[:, gi, :], dT[:, gi, t0:t0 + n], start=True, stop=True), reads=[], writes=[PS[gi]])
                P.op("scalar", lambda e, gi=gi, s=s, n=n: e.activation(yp[s][:, gi, 0:n], ps[gi][:, 0:n], AF.Copy, scale=psc[:, gi:gi + 1]), reads=[PS[gi]], writes=[("yp", s, gi)])
            P.dma("sync", yT_d[2, :, :, t0:t0 + n], yp[s][:, :, 0:n], reads=[("yp", s, gi) for gi in range(4)], writes=[("yT2", g)])
        P.barrier()
        if done("p4_%d" % L):
            return True

        P.mute = "p5" in skip
        A.reset(A_PERSIST)
        Wg = A.alloc("Wg", [128, 8, 3072], BF16)
        Wb = A.alloc("Wb", [128, 12, 1024], BF16)
        for hlf in range(2):
            for cbk in (hlf, 2 + hlf, 4 + hlf):
                P.dma("gpsimd", Wg[:, :, cbk * 512:(cbk + 1) * 512], w_in[L, :, 3072 + cbk * 512:3072 + (cbk + 1) * 512].rearrange("(k p) f -> p k f", p=128), writes=[("Wg", cbk)])
            for b in range(3):
                P.dma("gpsimd", Wb[:, b * 4:(b + 1) * 4, hlf * 512:(hlf + 1) * 512], w_branch[L, b, :, hlf * 512:(hlf + 1) * 512].rearrange("(k p) f -> p k f", p=128), writes=[("Wb", b, hlf)])
        Wo = A.alloc("Wo", [128, 8, 1024], BF16)
        for cbk in range(2):
            P.dma("gpsimd", Wo[:, :, cbk * 512:(cbk + 1) * 512], w_out[L, :, cbk * 512:(cbk + 1) * 512].rearrange("(k p) f -> p k f", p=128), writes=[("Wo", cbk)])
        md = load_mod(L, [2])
        hgms = [A.alloc("hgm%d" % i, [128, 8, 512], BF16) for i in range(2)]
        ygs = [A.alloc("yg%d" % i, [128, 3, 4, 512], BF16) for i in range(2)]
        mixT = A.alloc("mixT", [128, 8, 512], BF16)
        sgm = [A.alloc("sgm%d" % i, [128, 512], F32) for i in range(3)]
        tm = [A.alloc("tm%d" % i, [128, 512], F32) for i in range(3)]
        xtm = [A.alloc("xtm%d" % i, [128, D], F32) for i in range(2)]
        xom = [A.alloc("xom%d" % i, [128, D], F32) for i in range(2)]
        tmo = A.alloc("tmo", [128, 512], F32)
        xi_ = 0
        g5 = [g for g in range(9) if not (g == 8 and not need_ctx)]

        def p5_load(gp):
            g_ = g5[gp]
            t0_, n_ = GROUPS[g_]
            u_ = gp % 2
            P.dma("sync", hgms[u_][:, :, 0:n_], hT_d[:, :, t0_:t0_ + n_], reads=[("hT", g_)], writes=["hgm%d" % u_])
            for b in range(3):
                P.dma("sync", ygs[u_][:, b, :, 0:n_], yT_d[b, :, :, t0_:t0_ + n_], writes=[("yg", b, u_)])

        p5_load(0)
        for gp, g in enumerate(g5):
            t0, n = GROUPS[g]
            which = 0 if g < 8 else 1
            up = gp % 2
            hgm, yg = hgms[up], ygs[up]
            if gp + 1 < len(g5):
                p5_load(gp + 1)
            for jd in range(8):
                for b in range(3):
                    for k in range(8):
                        P.op("tensor", lambda e, k=k, b=b, jd=jd, n=n, hgm=hgm: e.matmul(ps[b][:, 0:n], Wg[:, k, b * 1024 + jd * 128:b * 1024 + (jd + 1) * 128], hgm[:, k, 0:n], start=(k == 0), stop=(k == 7)),
                             reads=["hgm%d" % up, ("Wg", (b * 1024 + jd * 128) // 512)], writes=[PS[b]])
                    for k in range(4):
                        P.op("tensor", lambda e, k=k, b=b, jd=jd, n=n, yg=yg: e.matmul(ps[3 + b][:, 0:n], Wb[:, b * 4 + k, jd * 128:(jd + 1) * 128], yg[:, b, k, 0:n], start=(k == 0), stop=(k == 3)),
                             reads=[("yg", b, up), ("Wb", b, jd // 4)], writes=[PS[3 + b]])
                    P.op("scalar", lambda e, b=b, n=n: e.activation(sgm[b][:, 0:n], ps[b][:, 0:n], AF.Sigmoid), reads=[PS[b]], writes=["sgm%d" % b])
                    P.op("vector", lambda e, b=b, n=n: e.tensor_tensor(tm[b][:, 0:n], ps[3 + b][:, 0:n], sgm[b][:, 0:n], ALU.mult), reads=[PS[3 + b], "sgm%d" % b], writes=["tm%d" % b])
                P.op("gpsimd", lambda e, n=n: e.tensor_tensor(tm[0][:, 0:n], tm[0][:, 0:n], tm[1][:, 0:n], ALU.add), reads=["tm0", "tm1"], writes=["tm0"])
                P.op("gpsimd", lambda e, jd=jd, n=n: e.tensor_tensor(mixT[:, jd, 0:n], tm[0][:, 0:n], tm[2][:, 0:n], ALU.add), reads=["tm0", "tm2"], writes=[("mixT", jd)])
            for j in range(n // 128):
                s = xi_ % 2
                xi_ += 1
                r0 = t0 + j * 128
                P.dma("sync", xtm[s][:], x_src[r0:r0 + 128, :], writes=["xtm%d" % s])
                for half in range(2):
                    for k in range(8):
                        P.op("tensor", lambda e, k=k, j=j, half=half: e.matmul(ps[6 + half][:], mixT[:, k, j * 128:(j + 1) * 128], Wo[:, k, half * 512:(half + 1) * 512], start=(k == 0), stop=(k == 7)),
                             reads=[("mixT", k), ("Wo", half)], writes=[PS[6 + half]])
                    g1 = md[2][which]
                    P.op("vector", lambda e, half=half, g1=g1: e.tensor_tensor(tmo[:], ps[6 + half][:], g1[0][:, half * 512:(half + 1) * 512], ALU.mult), reads=[PS[6 + half], g1[1]], writes=["tmo"])
                    P.op("gpsimd", lambda e, half=half, s=s: e.tensor_tensor(xom[s][:, half * 512:(half + 1) * 512], tmo[:], xtm[s][:, half * 512:(half + 1) * 512], ALU.add), reads=["tmo", "xtm%d" % s], writes=[("xom", s, half)])
                P.dma("gpsimd", x_mix[r0:r0 + 128, :], xom[s][:], reads=[("xom", s, 0), ("xom", s, 1)], writes=[("xmix", r0)])
        P.barrier()
        if done("p5_%d" % L):
            return True

        P.mute = "p6" in skip
        A.reset(A_PERSIST)
        if moe:
            NTL = 24
            NR = NTL * 512
            I32 = mybir.dt.int32
            md = load_mod(L, [3, 4, 5])
            A2m, B2m, G2m = md[4][0], md[3][0], md[5][0]
            idx_all = A.alloc("idx_all", [128, 64], I32)
            sw_all = A.alloc("sw_all", [128, 64], F32)
            widx = A.alloc("widx", [128, NTL], I32)
            m_meta = A.mark()
            abf_all = A.alloc("abf_all", [128, 32, D], BF16)
            mk_all = A.alloc("mk_all", [128, 2, 32, NE], F32)
            pos_all = A.alloc("pos_all", [128, 32, NE], F32)
            rwf = A.alloc("rwf", [128, 8, NE], F32)
            aTfs = [A.alloc("aTf%d" % i, [128, 8, 128], F32) for i in range(2)]
            rbb = A.alloc("rbb", [128, NE], F32)
            trif = A.alloc("trif", [128, 128], F32)
            tri = A.alloc("tri", [128, 128], BF16)
            iot = A.alloc("iot", [128, NTL + 1], F32)
            base = A.alloc("base", [128, NE], F32)
            zt = A.alloc("zt", [128, 8192], BF16)
            xtr = [A.alloc("xtr%d" % i, [128, D], F32) for i in range(2)]
            afs = [A.alloc("af%d" % i, [128, D], F32) for i in range(2)]
            prs = [[A.alloc("pr%d_%d" % (i, q), [128, D], F32) for i in range(2)] for q in range(2)]
            jks = [A.alloc("jk%d" % i, [128, D], BF16) for i in range(2)]
            lgts = [A.alloc("lgt%d" % i, [128, NE], F32) for i in range(2)]
            rss = [A.alloc("rs_%d" % i, [128, 96], F32) for i in range(2)]
            mbfs = [A.alloc("mbf%d" % i, [128, NE], BF16) for i in range(2)]
            rs_ = rss[0]
            P.dma("sync", rwf[:], moe_router_p, writes=["rwf"])
            P.dma("sync", rbb[:], moe_router_b[0].partition_broadcast(128), writes=["rbb"])
            P.dma("sync", trif[:], k_tri, writes=["trif"])
            P.dma("sync", iot[:], k_iot, writes=["iot"])
            P.op("vector", lambda e: e.tensor_copy(tri[:], trif[:]), reads=["trif"], writes=["tri"])
            P.op("vector", lambda e: e.memset(base[:], 0.0), writes=["base"])
            P.op("gpsimd", lambda e: e.memset(zt[:], 0.0), writes=["zt"])
            Gv = G_d.rearrange("(p r) d -> p (r d)", p=128)
            NZ = NR * D // 128 // 8192
            for z in range(NZ):
                P.dma("sync", Gv[:, z * 8192:(z + 1) * 8192], zt[:], reads=["zt"], writes=[("Gz", z)])
            GZ = [("Gz", z) for z in range(NZ)]
            prev_rt = [None]
            for ti in range(SEQ // 128):
                def route_tile(ti=ti):
                    u = ti % 2
                    sfx = "_%d" % u
                    af, jk, lgt, rs_, mbf = afs[u], jks[u], lgts[u], rss[u], mbfs[u]
                    pr = prs[u]
                    bkA, bkB, bkT = (0, 1, 2) if u == 0 else (4, 5, 6)
                    aTf = aTfs[u]
                    s = ti % 2
                    r0 = ti * 128
                    xt_ = xtr[s]
                    P.dma("sync", xt_[:], x_mix[r0:r0 + 128, :], writes=["xtr%d" % s])
                    P.op("scalar", lambda e, xt_=xt_: e.activation(jk[:], xt_[:], AF.Square, accum_out=rs_[:, 0:1]), reads=["xtr%d" % s], writes=["jk" + sfx, "rs0" + sfx])
                    P.op("scalar", lambda e: e.activation(rs_[:, 1:2], rs_[:, 0:1], AF.Sqrt, bias=NORM_EPS, scale=1.0 / D), reads=["rs0" + sfx], writes=["rs1" + sfx])
                    P.op("vector", lambda e: e.reciprocal(rs_[:, 2:3], rs_[:, 1:2]), reads=["rs1" + sfx], writes=["rs2" + sfx])
                    P.op("vector", lambda e, xt_=xt_: e.scalar_tensor_tensor(af[:], xt_[:], rs_[:, 2:3], A2m[0][:], ALU.mult, ALU.mult), reads=["xtr%d" % s, "rs2" + sfx, A2m[1]], writes=["af" + sfx])
                    P.op("gpsimd", lambda e: e.tensor_tensor(af[:], af[:], B2m[0][:], ALU.add), reads=["af" + sfx, B2m[1]], writes=["af" + sfx])
                    P.op("scalar", lambda e, ti=ti: e.copy(abf_all[:, ti, :], af[:]), reads=["af" + sfx], writes=[("abf", ti)])
                    for k in range(8):
                        P.op("tensor", lambda e, k=k: e.transpose(ps[bkT + k // 4][:, (k % 4) * 128:(k % 4 + 1) * 128], af[:, k * 128:(k + 1) * 128], identf[:]),
                             reads=["af" + sfx, "identf"], writes=[PS[bkT + k // 4]])
                    for hh in range(2):
                        P.op("scalar", lambda e, hh=hh: e.copy(aTf[:, hh * 4:(hh + 1) * 4, :], ps[bkT + hh][:].rearrange("p (k t) -> p k t", k=4)), reads=[PS[bkT + hh]], writes=[("aTf", hh, u)])
                    for k in range(8):
                        P.op("tensor", lambda e, k=k: e.matmul(ps[bkA][:, 16:24], aTf[:, k, :], rwf[:, k, :], start=(k == 0), stop=(k == 7)),
                             reads=[("aTf", 0, u), ("aTf", 1, u), "rwf"], writes=[PS[bkA]])
                    P.op("vector", lambda e: e.tensor_tensor(lgt[:], ps[bkA][:, 16:24], rbb[:], ALU.add), reads=[PS[bkA], "rbb"], writes=[("lgt", ex, u) for ex in range(NE)])
                    yield
                    LT = [("lgt", ex, u) for ex in range(NE)]
                    mk1 = mk_all[:, 0, ti, :]
                    mk2 = mk_all[:, 1, ti, :]
                    P.op("vector", lambda e: e.reduce_max(rs_[:, 8:9], lgt[:], axis=AX.X), reads=LT, writes=["m1" + sfx])
                    P.op("vector", lambda e, mk1=mk1: e.tensor_scalar(mk1, lgt[:], rs_[:, 8:9], None, ALU.is_ge), reads=LT + ["m1" + sfx], writes=[("mk1", ti)])
                    P.op("vector", lambda e, mk1=mk1: e.scalar_tensor_tensor(rs_[:, 24:32], mk1, -1e30, lgt[:], ALU.mult, ALU.add), reads=LT + [("mk1", ti)], writes=["l2" + sfx])
                    P.op("vector", lambda e: e.reduce_max(rs_[:, 9:10], rs_[:, 24:32], axis=AX.X), reads=["l2" + sfx], writes=["m2" + sfx])
                    P.op("vector", lambda e, mk2=mk2: e.tensor_scalar(mk2, rs_[:, 24:32], rs_[:, 9:10], None, ALU.is_ge), reads=["l2" + sfx, "m2" + sfx], writes=[("mk2", ti)])
                    P.op("vector", lambda e: e.tensor_tensor(rs_[:, 10:11], rs_[:, 9:10], rs_[:, 8:9], ALU.subtract), reads=["m1" + sfx, "m2" + sfx], writes=["dl" + sfx])
                    P.op("scalar", lambda e, ti=ti: e.activation(sw_all[:, 32 + ti:33 + ti], rs_[:, 10:11], AF.Sigmoid), reads=["dl" + sfx], writes=[("sw2", ti)])
                    P.op("vector", lambda e, ti=ti: e.tensor_scalar(sw_all[:, ti:ti + 1], sw_all[:, 32 + ti:33 + ti], -1.0, 1.0, ALU.mult, ALU.add), reads=[("sw2", ti)], writes=[("sw1", ti)])
                    P.op("vector", lambda e, mk1=mk1, mk2=mk2: e.tensor_tensor(rs_[:, 40:48], mk1, mk2, ALU.add), reads=[("mk1", ti), ("mk2", ti)], writes=["mall" + sfx])
                    P.op("vector", lambda e: e.tensor_copy(mbf[:], rs_[:, 40:48]), reads=["mall" + sfx], writes=["mbf" + sfx])
                    P.op("tensor", lambda e: e.matmul(ps[bkA][:, 0:NE], tri[:], mbf[:], start=True, stop=True), reads=["tri", "mbf" + sfx], writes=[PS[bkA]])
                    P.op("tensor", lambda e: e.matmul(ps[bkB][:, 0:NE], ones_bf[:], mbf[:], start=True, stop=True), reads=["mbf" + sfx], writes=[PS[bkB]])
                    P.op("vector", lambda e, ti=ti: e.tensor_tensor(pos_all[:, ti, :], ps[bkA][:, 0:NE], base[:], ALU.add), reads=[PS[bkA], "base"], writes=[("pos", ti)])
                    P.op("vector", lambda e: e.tensor_tensor(base[:], ps[bkB][:, 0:NE], base[:], ALU.add), reads=[PS[bkB], "base"], writes=["base"])
                gen_ = route_tile()
                next(gen_)
                if prev_rt[0] is not None:
                    for _ in prev_rt[0]:
                        pass
                prev_rt[0] = gen_
            for _ in prev_rt[0]:
                pass
            MKS = [("mk1", ti) for ti in range(32)] + [("mk2", ti) for ti in range(32)]
            POS = [("pos", ti) for ti in range(32)]
            nt_ = rs_[:, 16:24]
            stt = rs_[:, 48:56]
            P.op("vector", lambda e: e.tensor_scalar(nt_, base[:], 0.0, None, ALU.is_gt), reads=["base"], writes=["nt"])
            for m in range(1, 8):
                P.op("vector", lambda e, m=m: e.scalar_tensor_tensor(nt_, base[:], 512.0 * m, nt_, ALU.is_gt, ALU.add), reads=["base", "nt"], writes=["nt"])
            P.op("vector", lambda e: e.memset(stt, 0.0), writes=["stt"])
            for ex in range(1, NE):
                P.op("vector", lambda e, ex=ex: e.tensor_tensor(rs_[:, 48 + ex:49 + ex], rs_[:, 47 + ex:48 + ex], rs_[:, 15 + ex:16 + ex], ALU.add), reads=["stt", "nt"], writes=["stt"])
            P.op("vector", lambda e: e.tensor_tensor(rs_[:, 56:64], stt, nt_, ALU.add), reads=["stt", "nt"], writes=["endt"])
            P.op("vector", lambda e: e.tensor_scalar(rs_[:, 64:72], stt, 512.0, None, ALU.mult), reads=["stt"], writes=["rowb"])
            eidf = A.alloc("eidf", [128, NTL], F32)
            P.op("vector", lambda e: e.tensor_scalar(eidf[:], iot[:, 0:NTL], rs_[:, 56:57], None, ALU.is_ge), reads=["iot", "endt"], writes=["eidf"])
            for ex in range(1, NE - 1):
                P.op("vector", lambda e, ex=ex: e.scalar_tensor_tensor(eidf[:], iot[:, 0:NTL], rs_[:, 56 + ex:57 + ex], eidf[:], ALU.is_ge, ALU.add), reads=["iot", "endt", "eidf"], writes=["eidf"])
            P.op("vector", lambda e: e.tensor_scalar(eidf[:], eidf[:], 128.0, iot[:, NTL:NTL + 1], ALU.mult, ALU.add), reads=["eidf", "iot"], writes=["eidf"])
            P.op("vector", lambda e: e.tensor_copy(widx[:], eidf[:]), reads=["eidf"], writes=["widx"])
            rowi = A.alloc("rowi", [128, 32, NE], F32)
            t8 = A.alloc("t8", [128, 32, NE], F32)
            d32 = A.alloc("d32", [128, 64], F32)
            P.op("vector", lambda e: e.tensor_tensor(rowi[:], pos_all[:], rs_[:, 64:72].unsqueeze(1).to_broadcast([128, 32, NE]), ALU.add), reads=POS + ["rowb"], writes=["rowi"])
            for sl_ in range(2):
                P.op("vector", lambda e, sl_=sl_: e.tensor_tensor(t8[:], mk_all[:, sl_], rowi[:], ALU.mult), reads=MKS + ["rowi"], writes=["t8"])
                P.op("vector", lambda e, sl_=sl_: e.reduce_sum(d32[:, sl_ * 32:(sl_ + 1) * 32], t8[:], axis=AX.X), reads=["t8"], writes=[("d32", sl_)])
            P.op("vector", lambda e: e.tensor_copy(idx_all[:], d32[:]), reads=[("d32", 0), ("d32", 1)], writes=["idx_all"])
            for ti in range(32):
                for sl_ in range(2):
                    col = sl_ * 32 + ti
                    P.op("gpsimd", lambda e, col=col, ti=ti: e.indirect_dma_start(out=G_d[:, :], out_offset=bass.IndirectOffsetOnAxis(ap=idx_all[:, col:col + 1], axis=0),
                                                                               in_=abf_all[:, ti, :], in_offset=None),
                         reads=["idx_all", ("abf", ti)] + GZ, writes=[("Gs", col)], dma=True)
            P.barrier()
            if done("p6a_%d" % L):
                return True
            A.reset(m_meta)
            gt = [A.alloc("gt%d" % i, [128, 1024], BF16) for i in range(8)]
            aT = A.alloc("aTm", [128, 8, 512], BF16)
            hid = A.alloc("hidm", [128, 28, 512], BF16)
            wA = [A.alloc("wAm%d" % i, [128, 8, 512], BF16) for i in range(4)]
            wB = [A.alloc("wBm%d" % i, [128, 4, 1024], BF16) for i in range(3)]
            slm = [A.alloc("slm%d" % i, [128, 512], F32) for i in range(2)]
            yo = [A.alloc("yom%d" % i, [128, D], F32) for i in range(2)]
            wa_i = 0
            wb_i = 0
            gi_ = 0
            yo_i = 0
            pk = 0
            def g_load(i_):
                for j_ in range(4):
                    q_ = (i_ * 4 + j_) % 8
                    rr_ = i_ * 512 + j_ * 128
                    P.dma("sync", gt[q_][:], G_d[rr_:rr_ + 128, :], writes=["gt%d" % q_])

            g_load(0)
            for i in range(NTL):
                ixc = widx[:, i:i + 1]
                if i + 1 < NTL:
                    g_load(i + 1)
                for j in range(4):
                    g_ = gt[(i * 4 + j) % 8]
                    gtok = "gt%d" % ((i * 4 + j) % 8)
                    bank = 6 + (j % 2)
                    pbm = ps[bank][:].bitcast(BF16)
                    for k in range(8):
                        P.op("tensor", lambda e, k=k, g_=g_, pbm=pbm: e.transpose(pbm[:, k * 128:(k + 1) * 128], g_[:, k * 128:(k + 1) * 128], ident[:]), reads=[gtok, "ident"], writes=[PS[bank]])
                    P.op("scalar", lambda e, j=j, pbm=pbm: e.copy(aT[:, :, j * 128:(j + 1) * 128], pbm[:, 0:1024].rearrange("p (k t) -> p k t", k=8)), reads=[PS[bank]], writes=[("aTm", j)])
                AT = [("aTm", j) for j in range(4)]
                for fb in range(7):
                    sl_w = []
                    for wn, Wq in enumerate((w1q, w3q)):
                        s = wa_i % 4
                        wa_i += 1
                        for h in range(2):
                            P.op("gpsimd", lambda e, s=s, h=h, fb=fb, Wq=Wq, ixc=ixc: e.indirect_dma_start(out=wA[s][:, h * 4:(h + 1) * 4, :].rearrange("p k f -> p (k f)"), out_offset=None,
                                                                                                   in_=Wq[fb][h][:, :], in_offset=bass.IndirectOffsetOnAxis(ap=ixc, axis=0)),
                                 reads=[], writes=[("wAm", s, h)], dma=True)
                        sl_w.append(s)
                    for fc in range(4):
                        b1, b3 = (pk % 2) * 2, (pk % 2) * 2 + 1
                        sls = slm[pk % 2]
                        stok = "slm%d" % (pk % 2)
                        pk += 1
                        for (bank, s) in ((b1, sl_w[0]), (b3, sl_w[1])):
                            for k in range(8):
                                P.op("tensor", lambda e, k=k, bank=bank, s=s, fc=fc: e.matmul(ps[bank][:], wA[s][:, k, fc * 128:(fc + 1) * 128], aT[:, k, :], start=(k == 0), stop=(k == 7)),
                                     reads=AT + [("wAm", s, 0), ("wAm", s, 1)], writes=[PS[bank]])
                        P.op("scalar", lambda e, b1=b1, sls=sls: e.activation(sls[:], ps[b1][:], AF.Silu), reads=[PS[b1]], writes=[stok])
                        P.op("vector", lambda e, b3=b3, sls=sls, fb=fb, fc=fc: e.tensor_tensor(hid[:, fb * 4 + fc, :], ps[b3][:], sls[:], ALU.mult),
                             reads=[PS[b3], stok], writes=[("hidm", fb * 4 + fc)])
                for fb in range(7):
                    s = wb_i % 3
                    wb_i += 1
                    for h in range(2):
                        P.op("gpsimd", lambda e, s=s, h=h, fb=fb, ixc=ixc: e.indirect_dma_start(out=wB[s][:, h * 2:(h + 1) * 2, :].rearrange("p k f -> p (k f)"), out_offset=None,
                                                                                         in_=w2q[fb][h][:, :], in_offset=bass.IndirectOffsetOnAxis(ap=ixc, axis=0)),
                             reads=[], writes=[("wBm", s, h)], dma=True)
                    for j in range(4):
                        for half in range(2):
                            bank = j * 2 + half
                            for k in range(4):
                                P.op("tensor", lambda e, k=k, j=j, half=half, bank=bank, s=s, fb=fb: e.matmul(ps[bank][:], hid[:, fb * 4 + k, j * 128:(j + 1) * 128], wB[s][:, k, half * 512:(half + 1) * 512],
                                                                                               start=(fb == 0 and k == 0), stop=(fb == 6 and k == 3)),
                                     reads=[("hidm", fb * 4 + k), ("wBm", s, k // 2)], writes=[PS[bank]])
                for j in range(4):
                    y_ = yo[yo_i % 2]
                    ytok = "yom%d" % (yo_i % 2)
                    yo_i += 1
                    for half in range(2):
                        bank = j * 2 + half
                        if half == 0:
                            P.op("scalar", lambda e, y_=y_, bank=bank: e.copy(y_[:, 0:512], ps[bank][:]), reads=[PS[bank]], writes=[(ytok, 0)])
                        else:
                            P.op("vector", lambda e, y_=y_, bank=bank: e.tensor_copy(y_[:, 512:1024], ps[bank][:]), reads=[PS[bank]], writes=[(ytok, 1)])
                    rr = i * 512 + j * 128
                    P.dma("sync", Y_d[rr:rr + 128, :], y_[:], reads=[(ytok, 0), (ytok, 1)], writes=[("Y", rr)])
            P.barrier()
            if done("p6b_%d" % L):
                return True
            A.reset(m_meta)
            y1 = [A.alloc("y1_%d" % i, [128, D], F32) for i in range(2)]
            y2 = [A.alloc("y2_%d" % i, [128, D], F32) for i in range(2)]
            xtc = [A.alloc("xtc%d" % i, [128, D], F32) for i in range(2)]
            cmb = [A.alloc("cmb%d" % i, [128, D], F32) for i in range(2)]
            xoz = [A.alloc("xozm%d" % i, [128, D], F32) for i in range(2)]
            jzz = [A.alloc("jzz%d" % i, [128, D], BF16) for i in range(2)]
            szz = [A.alloc("szz%d" % i, [128, 4], F32) for i in range(2)]
            fngm = A.alloc("fngm", [128, D], F32)
            P.dma("sync", fngm[:], final_norm.partition_broadcast(128), writes=["fngm"])
            prev_c = [None]
            for ti in range(SEQ // 128):
                def comb_tile(ti=ti):
                    s = ti % 2
                    r0 = ti * 128
                    for (yt_, nm, col) in ((y1[s], "y1_%d" % s, ti), (y2[s], "y2_%d" % s, 32 + ti)):
                        P.op("gpsimd", lambda e, yt_=yt_, col=col: e.indirect_dma_start(out=yt_[:, :], out_offset=None, in_=Y_d[:, :],
                                                                                     in_offset=bass.IndirectOffsetOnAxis(ap=idx_all[:, col:col + 1], axis=0)),
                             reads=[], writes=[nm], dma=True)
                    P.dma("sync", xtc[s][:], x_mix[r0:r0 + 128, :], writes=["xtc%d" % s])
                    cm = cmb[s]
                    P.op("vector", lambda e, cm=cm, s=s, ti=ti: e.tensor_scalar(cm[:], y1[s][:], sw_all[:, ti:ti + 1], None, ALU.mult), reads=["y1_%d" % s], writes=["cmb%d" % s])
                    P.op("vector", lambda e, cm=cm, s=s, ti=ti: e.scalar_tensor_tensor(cm[:], y2[s][:], sw_all[:, 32 + ti:33 + ti], cm[:], ALU.mult, ALU.add), reads=["y2_%d" % s, "cmb%d" % s], writes=["cmb%d" % s])
                    P.op("gpsimd", lambda e, cm=cm: e.tensor_tensor(cm[:], cm[:], G2m[0][:], ALU.mult), reads=["cmb%d" % s, G2m[1]], writes=["cmb%d" % s])
                    P.op("gpsimd", lambda e, cm=cm, s=s: e.tensor_tensor(cm[:], cm[:], xtc[s][:], ALU.add), reads=["cmb%d" % s, "xtc%d" % s], writes=["cmb%d" % s])
                    yield
                    szs = szz[s]
                    P.op("scalar", lambda e, cm=cm, s=s, szs=szs: e.activation(jzz[s][:], cm[:], AF.Square, accum_out=szs[:, 0:1]), reads=["cmb%d" % s], writes=["jzz%d" % s, "szz0_%d" % s])
                    P.op("scalar", lambda e, szs=szs: e.activation(szs[:, 1:2], szs[:, 0:1], AF.Sqrt, bias=NORM_EPS, scale=1.0 / D), reads=["szz0_%d" % s], writes=["szz1_%d" % s])
                    P.op("vector", lambda e, szs=szs: e.reciprocal(szs[:, 2:3], szs[:, 1:2]), reads=["szz1_%d" % s], writes=["szz2_%d" % s])
                    P.op("vector", lambda e, cm=cm, s=s, szs=szs: e.scalar_tensor_tensor(xoz[s][:], cm[:], szs[:, 2:3], fngm[:], ALU.mult, ALU.mult), reads=["cmb%d" % s, "szz2_%d" % s, "fngm"], writes=["xozm%d" % s])
                    P.dma("sync", out[r0:r0 + 128, :], xoz[s][:], reads=["xozm%d" % s], writes=[("out", r0)])
                gen_ = comb_tile()
                next(gen_)
                if prev_c[0] is not None:
                    for _ in prev_c[0]:
                        pass
                prev_c[0] = gen_
            for _ in prev_c[0]:
                pass
            P.barrier()
            return False
        md = load_mod(L, [3, 4, 5])
        normFA, normFB, normF = make_normT("p6", md[4], md[3], 7)
        aT = A.alloc("aT", [128, 8, 512], BF16)
        hid = A.alloc("hid", [128, 28, 512], BF16)
        wA = [A.alloc("wA%d" % i, [128, 8, 512], BF16) for i in range(4)]
        W2res = A.alloc("W2res", [128, 28, 1024], BF16)
        for fb in range(7):
            for cbk in range(2):
                P.dma("gpsimd", W2res[:, fb * 4:(fb + 1) * 4, cbk * 512:(cbk + 1) * 512], ffn_w2[0][fb * 512:(fb + 1) * 512, cbk * 512:(cbk + 1) * 512].rearrange("(k p) f -> p k f", p=128), writes=[("W2res", fb, cbk)])
        sl = [A.alloc("sl%d" % i, [128, 512], F32) for i in range(2)]
        xtf = [A.alloc("xtf%d" % i, [128, D], F32) for i in range(2)]
        xof = [A.alloc("xof%d" % i, [128, D], F32) for i in range(2)]
        tmf = A.alloc("tmf", [128, 512], F32)
        if moe:
            acc = A.alloc("acc", [128, 4, D], F32)
            rwb = A.alloc("rwb", [128, NE, D], F32)
            rbb = A.alloc("rbb", [128, NE], F32)
            af = A.alloc("af", [128, D], F32)
            pr = A.alloc("pr", [128, D], F32)
            lgt = A.alloc("lgt", [128, 4, NE], F32)
            wgt = A.alloc("wgt", [128, 4, NE], F32)
            rs_ = A.alloc("rs_", [128, 40], F32)
            for ex in range(NE):
                P.dma("sync", rwb[:, ex, :], moe_router_t[ex].partition_broadcast(128), writes=[("rwb", ex)])
            P.dma("sync", rbb[:], moe_router_b[0].partition_broadcast(128), writes=["rbb"])
        wa_i = [0]
        wb_i = [0]
        xf_i = 0
        glist = [g for g in range(9) if not (g == 8 and (moe or not need_ctx))]
        pend = [None]
        for gpos, g in enumerate(glist):
            t0, n = GROUPS[g]
            which = 0 if g < 8 else 1
            nt_ = n // 128
            if pend[0] is None:
                normFB(normFA(x_mix, t0, nt_, which), aT, "aT")
            else:
                normFB(pend[0], aT, "aT")
                pend[0] = None
            if moe:
                for j in range(nt_):
                    r0 = t0 + j * 128
                    P.dma("sync", xtf[0][:], x_mix[r0:r0 + 128, :], writes=["xtf0"])
                    P.op("scalar", lambda e: e.activation(pr[:], xtf[0][:], AF.Square, accum_out=rs_[:, 0:1]), reads=["xtf0"], writes=["pr", "rs0"])
                    P.op("scalar", lambda e: e.activation(rs_[:, 1:2], rs_[:, 0:1], AF.Sqrt, bias=NORM_EPS, scale=1.0 / D), reads=["rs0"], writes=["rs1"])
                    P.op("vector", lambda e: e.reciprocal(rs_[:, 2:3], rs_[:, 1:2]), reads=["rs1"], writes=["rs2"])
                    am, bm = md[4][which], md[3][which]
                    P.op("vector", lambda e, am=am: e.scalar_tensor_tensor(af[:], xtf[0][:], rs_[:, 2:3], am[0][:], ALU.mult, ALU.mult), reads=["xtf0", "rs2", am[1]], writes=["af"])
                    P.op("gpsimd", lambda e, bm=bm: e.tensor_tensor(af[:], af[:], bm[0][:], ALU.add), reads=["af", bm[1]], writes=["af"])
                    for ex in range(NE):
                        eng = "vector" if ex % 2 == 0 else "gpsimd"
                        P.op(eng, lambda e, ex=ex: e.tensor_tensor(pr[:], af[:], rwb[:, ex, :], ALU.mult), reads=["af", ("rwb", ex)], writes=["pr"])
                        P.op("vector", lambda e, ex=ex, j=j: e.reduce_sum(lgt[:, j, ex:ex + 1], pr[:], axis=AX.X), reads=["pr"], writes=[("lgt", j, ex)])
                    LT = [("lgt", j, ex) for ex in range(NE)]
                    lj = lgt[:, j, :]
                    P.op("vector", lambda e, lj=lj: e.tensor_tensor(lj, lj, rbb[:], ALU.add), reads=LT + ["rbb"], writes=LT)
                    P.op("vector", lambda e, lj=lj: e.reduce_max(rs_[:, 8:9], lj, axis=AX.X), reads=LT, writes=["m1"])
                    P.op("vector", lambda e, lj=lj: e.tensor_scalar(rs_[:, 16:24], lj, rs_[:, 8:9], None, ALU.is_ge), reads=LT + ["m1"], writes=["mk1"])
                    P.op("vector", lambda e, lj=lj: e.scalar_tensor_tensor(rs_[:, 24:32], rs_[:, 16:24], -1e30, lj, ALU.mult, ALU.add), reads=LT + ["mk1"], writes=["l2"])
                    P.op("vector", lambda e: e.reduce_max(rs_[:, 9:10], rs_[:, 24:32], axis=AX.X), reads=["l2"], writes=["m2"])
                    P.op("vector", lambda e: e.tensor_scalar(rs_[:, 32:40], rs_[:, 24:32], rs_[:, 9:10], None, ALU.is_ge), reads=["l2", "m2"], writes=["mk2"])
                    P.op("vector", lambda e: e.tensor_tensor(rs_[:, 10:11], rs_[:, 9:10], rs_[:, 8:9], ALU.subtract), reads=["m1", "m2"], writes=["dl"])
                    P.op("scalar", lambda e: e.activation(rs_[:, 11:12], rs_[:, 10:11], AF.Sigmoid), reads=["dl"], writes=["s2"])
                    P.op("vector", lambda e: e.tensor_scalar(rs_[:, 12:13], rs_[:, 11:12], -1.0, 1.0, ALU.mult, ALU.add), reads=["s2"], writes=["s1"])
                    P.op("vector", lambda e, j=j: e.tensor_scalar(wgt[:, j, :], rs_[:, 16:24], rs_[:, 12:13], None, ALU.mult), reads=["mk1", "s1"], writes=[("wgt", j)])
                    P.op("vector", lambda e, j=j: e.scalar_tensor_tensor(wgt[:, j, :], rs_[:, 32:40], rs_[:, 11:12], wgt[:, j, :], ALU.mult, ALU.add), reads=["mk2", "s2", ("wgt", j)], writes=[("wgt", j)])
            nexp = NE if moe else 1
            for ex in range(nexp):
                if moe:
                    W1, W3, W2 = moe_w1[0, ex], moe_w3[0, ex], moe_w2[0, ex]
                else:
                    W1, W3, W2 = ffn_w1[0], ffn_w3[0], ffn_w2[0]
                for fb in range(7):
                    sl_w = []
                    for Wsrc in (W1, W3):
                        s = wa_i[0] % 4
                        wa_i[0] += 1
                        P.dma("gpsimd", wA[s][:], Wsrc[:, fb * 512:(fb + 1) * 512].rearrange("(k p) f -> p k f", p=128), writes=["wA%d" % s])
                        sl_w.append(s)
                    for fc in range(4):
                        b1, b3 = (fc % 2) * 2, (fc % 2) * 2 + 1
                        for (bank, s) in ((b1, sl_w[0]), (b3, sl_w[1])):
                            for k in range(8):
                                P.op("tensor", lambda e, k=k, bank=bank, s=s, fc=fc, n=n: e.matmul(ps[bank][:, 0:n], wA[s][:, k, fc * 128:(fc + 1) * 128], aT[:, k, 0:n], start=(k == 0), stop=(k == 7)),
                                     reads=["aT", "wA%d" % s], writes=[PS[bank]])
                        sls = sl[fc % 2]
                        P.op("scalar", lambda e, b1=b1, sls=sls, n=n: e.activation(sls[:, 0:n], ps[b1][:, 0:n], AF.Silu), reads=[PS[b1]], writes=["sl%d" % (fc % 2)])
                        P.op("vector", lambda e, b3=b3, sls=sls, fb=fb, fc=fc, n=n: e.tensor_tensor(hid[:, fb * 4 + fc, 0:n], ps[b3][:, 0:n], sls[:, 0:n], ALU.mult),
                             reads=[PS[b3], "sl%d" % (fc % 2)], writes=[("hid", fb * 4 + fc)])
                if (not moe) and gpos + 1 < len(glist):
                    g2 = glist[gpos + 1]
                    pend[0] = normFA(x_mix, GROUPS[g2][0], GROUPS[g2][1] // 128, 0 if g2 < 8 else 1)
                for fb in range(7):
                    for j in range(nt_):
                        for half in range(2):
                            bank = j * 2 + half
                            for k in range(4):
                                P.op("tensor", lambda e, k=k, j=j, half=half, bank=bank, fb=fb: e.matmul(ps[bank][:], hid[:, fb * 4 + k, j * 128:(j + 1) * 128], W2res[:, fb * 4 + k, half * 512:(half + 1) * 512],
                                                                                          start=(fb == 0 and k == 0), stop=(fb == 6 and k == 3)),
                                     reads=[("hid", fb * 4 + k), ("W2res", fb, half)], writes=[PS[bank]])
                for j in range(nt_):
                    r0 = t0 + j * 128
                    if not moe or ex == nexp - 1:
                        sx = xf_i % 2
                        xf_i += 1
                        P.dma("sync", xtf[sx][:], x_mix[r0:r0 + 128, :], writes=["xtf%d" % sx])
                    for half in range(2):
                        bank = j * 2 + half
                        hs = slice(half * 512, (half + 1) * 512)
                        g2 = md[5][which]
                        if moe:
                            wc = wgt[:, j, ex:ex + 1]
                            if ex == 0:
                                P.op("vector", lambda e, j=j, hs=hs, bank=bank, wc=wc: e.tensor_scalar(acc[:, j, hs], ps[bank][:], wc, None, ALU.mult), reads=[PS[bank], ("wgt", j)], writes=[("acc", j, half)])
                            else:
                                P.op("vector", lambda e, j=j, hs=hs, bank=bank, wc=wc: e.scalar_tensor_tensor(acc[:, j, hs], ps[bank][:], wc, acc[:, j, hs], ALU.mult, ALU.add), reads=[PS[bank], ("wgt", j), ("acc", j, half)], writes=[("acc", j, half)])
                            if ex == nexp - 1:
                                P.op("gpsimd", lambda e, j=j, hs=hs, g2=g2: e.tensor_tensor(tmf[:], acc[:, j, hs], g2[0][:, hs], ALU.mult), reads=[("acc", j, half), g2[1]], writes=["tmf"])
                                P.op("gpsimd", lambda e, hs=hs, sx=sx: e.tensor_tensor(xof[sx][:, hs], tmf[:], xtf[sx][:, hs], ALU.add), reads=["tmf", "xtf%d" % sx], writes=[("xof", sx, half)])
                        else:
                            P.op("vector", lambda e, hs=hs, bank=bank, g2=g2: e.tensor_tensor(tmf[:], ps[bank][:], g2[0][:, hs], ALU.mult), reads=[PS[bank], g2[1]], writes=["tmf"])
                            P.op("gpsimd", lambda e, hs=hs, sx=sx: e.tensor_tensor(xof[sx][:, hs], tmf[:], xtf[sx][:, hs], ALU.add), reads=["tmf", "xtf%d" % sx], writes=[("xof", sx, half)])
                    if not moe or ex == nexp - 1:
                        P.dma("gpsimd", x_out[r0:r0 + 128, :], xof[sx][:], reads=[("xof", sx, 0), ("xof", sx, 1)], writes=[("xout", r0)])
        P.barrier()
        if done("p6_%d" % L):
            return True
        return False

    if layer(0, xin, xa_d, xb_d, True, False):
        return finish()
    if layer(1, xb_d, xc_d, xd_d, False, True):
        return finish()
    return finish()
    A.reset(A_PERSIST)
    fng = A.alloc("fng", [128, D], F32)
    P.dma("sync", fng[:], final_norm.partition_broadcast(128), writes=["fng"])
    xtz = [A.alloc("xtz%d" % i, [128, D], F32) for i in range(2)]
    xoz = [A.alloc("xoz%d" % i, [128, D], F32) for i in range(2)]
    jz = A.alloc("jz", [128, D], BF16)
    sz = A.alloc("sz", [128, 4], F32)
    for i in range(SEQ // 128):
        s = i % 2
        P.dma("sync", xtz[s][:], xd_d[i * 128:(i + 1) * 128, :], writes=["xtz%d" % s])
        P.op("scalar", lambda e, s=s: e.activation(jz[:], xtz[s][:], AF.Square, accum_out=sz[:, 0:1]), reads=["xtz%d" % s], writes=["jz", "sz0"])
        P.op("scalar", lambda e: e.activation(sz[:, 1:2], sz[:, 0:1], AF.Sqrt, bias=NORM_EPS, scale=1.0 / D), reads=["sz0"], writes=["sz1"])
        P.op("vector", lambda e: e.reciprocal(sz[:, 2:3], sz[:, 1:2]), reads=["sz1"], writes=["sz2"])
        P.op("vector", lambda e, s=s: e.scalar_tensor_tensor(xoz[s][:], xtz[s][:], sz[:, 2:3], fng[:], ALU.mult, ALU.mult), reads=["xtz%d" % s, "sz2", "fng"], writes=["xoz%d" % s])
        P.dma("sync", out[i * 128:(i + 1) * 128, :], xoz[s][:], reads=["xoz%d" % s], writes=[("out", i)])
    return finish()


_CACHE = {}


def _moe_layout(inputs):
    f = np.float32
    out = {}
    for nm, key in (("w1q", "moe_w1"), ("w3q", "moe_w3")):
        w = np.asarray(inputs[key], dtype=f)[0].reshape(NE, 2, 4, 128, 7, 512)
        w = w.transpose(4, 1, 0, 3, 2, 5)
        for fb in range(7):
            for h in range(2):
                out["%s_%d_%d" % (nm, fb, h)] = np.ascontiguousarray(w[fb, h]).reshape(NE * 128, 2048)
    w = np.asarray(inputs["moe_w2"], dtype=f)[0].reshape(NE, 7, 2, 2, 128, D)
    w = w.transpose(1, 2, 0, 4, 3, 5)
    for fb in range(7):
        for h in range(2):
            out["w2q_%d_%d" % (fb, h)] = np.ascontiguousarray(w[fb, h]).reshape(NE * 128, 2048)
    return out


def _core_inputs(inputs, b, consts, moe_l=None):
    f = np.float32
    m = {}
    m.update(moe_l if moe_l is not None else _moe_layout(inputs))
    m["xin"] = np.ascontiguousarray(np.concatenate([inputs["x"][b], inputs["ctx"][b]], axis=0), dtype=f)
    cc = np.concatenate([np.asarray(inputs["c"][b]).reshape(8, 128).T, np.asarray(inputs["c_ctx"]).reshape(8, 128).T], axis=1)
    m["cT"] = np.ascontiguousarray(cc, dtype=f)
    for k in ("w_ada", "b_ada", "norm_mix", "norm_ffn", "w_in", "ret_gn", "attn_qn", "attn_kn", "pool_w", "pool_scale",
              "w_branch", "w_out", "ffn_w1", "ffn_w3", "ffn_w2", "moe_router_b", "final_norm"):
        m[k] = np.ascontiguousarray(inputs[k], dtype=f)
    m["ret_decay"] = np.ascontiguousarray(np.asarray(inputs["ret_decay"]).reshape(2, 8), dtype=f)
    m["k_ident"] = consts["ident"]
    m["k_acos"] = consts["acos"]
    m["k_asin"] = consts["asin"]
    m["k_rt4"] = np.ascontiguousarray(np.stack([consts["rcos"], consts["rsin"], consts["rcosk"], consts["rsink"]], axis=2))
    m["moe_router_t"] = np.ascontiguousarray(np.asarray(inputs["moe_router"])[0].T, dtype=f)
    m["moe_router_p"] = np.ascontiguousarray(np.asarray(inputs["moe_router"], dtype=f)[0].reshape(8, 128, NE).transpose(1, 0, 2))
    m["k_rtab"] = consts["rtab"]
    m["k_pcol"] = consts["pcol"]
    m["k_pedge"] = consts["pedge"]
    m["k_tri"] = consts["tri"]
    m["k_iot"] = consts["iot"]
    return m


def kernel(**inputs):
    inputs = {k: np.asarray(v) for k, v in inputs.items()}
    consts = _host_consts()
    if "nc" not in _CACHE:
        _CACHE["nc"] = build()
    nc = _CACHE["nc"]
    moe_l = _moe_layout(inputs)
    in_maps = [_core_inputs(inputs, b, consts, moe_l) for b in range(8)]
    res = run_bass_kernel_spmd(nc, in_maps, core_ids=list(range(8)))
    outs = [np.asarray(r["out"], dtype=np.float32) for r in res.results]
    return np.stack(outs, axis=0)
```

```python
import os
import numpy as np
import ml_dtypes
CUT = int(os.environ.get('MK_CUT', '99'))
from contextlib import ExitStack
import concourse.bass as bass
import concourse.mybir as mybir
from concourse.bass_utils import run_bass_kernel_spmd

F32 = mybir.dt.float32
BF16 = mybir.dt.bfloat16
AF = mybir.ActivationFunctionType
ALU = mybir.AluOpType
AX = mybir.AxisListType

ALL_Q = ("tensor", "vector", "scalar", "gpsimd", "sync")
N_DMA_SEMS = 8

D = 1024
SEQ = 4096
CTX = 256
NT = SEQ + CTX
NTILE = NT // 128
FFN = 3584
NE = 8
INW = 6144
SB_BASE = 16640
SB_END = 229376
NORM_EPS = 1e-6
GN_EPS = 1e-5


class Op:
    __slots__ = ("q", "fn", "is_dma", "deps", "signal", "sem", "target", "prev_on_sem")

    def __init__(self, q, fn, is_dma):
        self.q = q
        self.fn = fn
        self.is_dma = is_dma
        self.deps = []
        self.signal = False
        self.sem = None
        self.target = 0
        self.prev_on_sem = None


class Prog:
    def __init__(self, nc, same_engine_sync=True):
        self.nc = nc
        self.ops = {q: [] for q in ALL_Q}
        self.last_writer = {}
        self.readers = {}
        self.same_engine_sync = same_engine_sync
        self.dma_rr = {q: 0 for q in ALL_Q}
        self.dma_last = {}
        self.mute = False

    def op(self, q, fn, reads=(), writes=(), dma=False, extra_deps=()):
        if self.mute:
            return None
        o = Op(q, fn, dma)
        deps = list(extra_deps)
        for r in reads:
            w = self.last_writer.get(r)
            if w is not None:
                deps.append(w)
        for w_ in writes:
            w = self.last_writer.get(w_)
            if w is not None:
                deps.append(w)
            deps.extend(self.readers.get(w_, ()))
        seen = set()
        for d in deps:
            if id(d) in seen or d is o:
                continue
            seen.add(id(d))
            if d.q == q and not d.is_dma:
                if q == "tensor" or not self.same_engine_sync:
                    continue
            o.deps.append(d)
            d.signal = True
        for r in reads:
            self.readers.setdefault(r, []).append(o)
        for w_ in writes:
            self.last_writer[w_] = o
            self.readers[w_] = []
        if dma:
            k = self.dma_rr[q]
            self.dma_rr[q] = k + 1
            slot = (q, k % N_DMA_SEMS)
            o.sem = slot
            o.prev_on_sem = self.dma_last.get(slot)
            o.target = (o.prev_on_sem.target if o.prev_on_sem is not None else 0) + 16
            self.dma_last[slot] = o
        self.ops[q].append(o)
        return o

    def dma(self, q, out, in_, reads=(), writes=(), **kw):
        return self.op(q, lambda e: e.dma_start(out=out, in_=in_, **kw), reads, writes, dma=True)

    def barrier(self):
        if self.mute:
            return
        deps = []
        for q in ALL_Q:
            for o in reversed(self.ops[q]):
                if not o.is_dma:
                    deps.append(o)
                    break
        deps.extend(self.dma_last.values())
        b = self.op("sync", lambda e: e.nop(), extra_deps=deps)
        for q in ALL_Q:
            if q != "sync":
                self.op(q, lambda e: e.nop(), extra_deps=[b])
        self.last_writer = {}
        self.readers = {}

    def emit(self):
        nc = self.nc
        for q in ALL_Q:
            c = 0
            for o in self.ops[q]:
                if o.is_dma:
                    continue
                if o.signal:
                    c += 1
                    o.target = c
        with ExitStack() as st:
            qsem = {q: st.enter_context(nc.semaphore("s_" + q)) for q in ALL_Q}
            dsem = {}
            for q in ALL_Q:
                for k in range(min(N_DMA_SEMS, self.dma_rr[q])):
                    dsem[(q, k)] = st.enter_context(nc.semaphore("d_%s_%d" % (q, k)))
            block = st.enter_context(nc.Block())

            def run_queue(q, eng):
                known = {}

                def wait_for(d):
                    if d.is_dma:
                        key = d.sem
                        sem = dsem[key]
                    else:
                        key = d.q
                        sem = qsem[d.q]
                    if known.get(key, 0) >= d.target:
                        return
                    eng.wait_ge(sem, d.target)
                    known[key] = d.target

                for o in self.ops[q]:
                    for d in o.deps:
                        wait_for(d)
                    if o.is_dma and o.prev_on_sem is not None:
                        wait_for(o.prev_on_sem)
                    ins = o.fn(eng)
                    if o.is_dma:
                        ins.then_inc(dsem[o.sem], 16)
                    elif o.signal:
                        ins.then_inc(qsem[q], 1)

            @block.tensor
            def _(e):
                run_queue("tensor", e)

            @block.vector
            def _(e):
                run_queue("vector", e)

            @block.scalar
            def _(e):
                run_queue("scalar", e)

            @block.gpsimd
            def _(e):
                run_queue("gpsimd", e)

            @block.sync
            def _(e):
                run_queue("sync", e)


class Arena:
    def __init__(self, nc, base=SB_BASE, end=SB_END):
        self.nc = nc
        self.base = base
        self.off = base
        self.end = end
        self.n = 0

    def reset(self, to=None):
        self.off = self.base if to is None else to

    def mark(self):
        return self.off

    def alloc(self, name, shape, dt):
        esz = 2 if dt == BF16 else 4
        nbytes = int(np.prod(shape[1:])) * esz
        nbytes = (nbytes + 63) // 64 * 64
        assert self.off + nbytes <= self.end, ("SBUF overflow", name, self.off, nbytes)
        self.n += 1
        t = self.nc.alloc_sbuf_tensor_at("%s_%d" % (name, self.n), list(shape), dt, offset=self.off)
        self.off += nbytes
        return t


def _host_consts():
    c = {}
    c["ident"] = np.eye(128, dtype=np.float32)
    half = 64
    fr = (1.0 / (10000.0 ** (np.arange(0, half, 2, dtype=np.float32) / np.float32(half)))).astype(np.float32)
    t = np.arange(SEQ)
    row = (t // 64).astype(np.float32)
    col = (t % 64).astype(np.float32)
    ang = np.concatenate([row[:, None] * fr[None, :], col[:, None] * fr[None, :]], axis=-1).astype(np.float32)
    cos = np.concatenate([np.cos(ang), np.ones((CTX, 64), np.float32)], 0).astype(np.float32)
    sin = np.concatenate([np.sin(ang), np.zeros((CTX, 64), np.float32)], 0).astype(np.float32)

    def tl(a):
        return np.ascontiguousarray(a.reshape(NTILE, 128, -1).transpose(1, 0, 2))

    c["acos"] = tl(cos)
    c["asin"] = tl(sin)
    fr2 = (1.0 / (10000.0 ** (np.arange(0, 64, 2, dtype=np.float32) / np.float32(64)))).astype(np.float32)
    ang2 = (np.arange(SEQ, dtype=np.float32)[:, None] * fr2[None, :]).astype(np.float32)
    rc = np.concatenate([np.cos(ang2), np.ones((CTX, 32), np.float32)], 0).astype(np.float32)
    rs = np.concatenate([np.sin(ang2), np.zeros((CTX, 32), np.float32)], 0).astype(np.float32)
    c["rcos"] = tl(rc)
    c["rsin"] = tl(rs)
    c["rcosk"] = tl(rc * np.float32(0.125))
    c["rsink"] = tl(rs * np.float32(0.125))
    m = np.arange(128, dtype=np.float32)[:, None]
    l = np.arange(128, dtype=np.float32)[None, :]
    rt = np.zeros((128, 6, 128), np.float32)
    rt[:, 0, :] = np.maximum(l - m, 0)
    rt[:, 1, :] = (l >= m)
    rt[:, 2, :] = np.maximum(m - l, 0)
    rt[:, 3, :] = (m >= l)
    rt[:, 4, :] = l + 1.0
    rt[:, 5, :] = 128.0 - l
    c["rtab"] = rt
    pc = np.zeros((128, 2), np.float32)
    pc[:, 0] = 127.0 - np.arange(128)
    pc[:, 1] = np.arange(128)
    c["pcol"] = pc
    pe = np.ones((128, 4, 2, 8), np.float32)
    for gi, w in enumerate((2, 4, 8, 16)):
        hw = w // 2
        for j in range(hw):
            pe[:, gi, 0, j] = w / float(j + hw)
            cnt = min(j + 1 + hw, w)
            pe[:, gi, 1, hw - 1 - j] = w / float(cnt)
    c["pedge"] = pe
    c["tri"] = np.triu(np.ones((128, 128), np.float32), 1)
    io = np.zeros((128, 25), np.float32)
    io[:, 0:24] = np.arange(24, dtype=np.float32)[None, :]
    io[:, 24] = np.arange(128, dtype=np.float32)
    c["iot"] = io
    c["prow"] = np.ascontiguousarray(np.broadcast_to((NE * 1280.0 + np.arange(128, dtype=np.float32))[:, None], (128, NE)))
    c["ec"] = np.ascontiguousarray(np.broadcast_to((np.arange(NE, dtype=np.float32) * 1280.0)[None, :], (128, NE)))
    return c


def build(dbg=False, stop_after=None, skip=(), ext=None):
    nc = bass.Bass("TRN2", target_bir_lowering=False)
    P = Prog(nc, same_engine_sync=(os.environ.get('MK_SES', '1') == '1'))
    A = Arena(nc)

    def din(name, shape, dt=F32):
        kind = "ExternalInput" if (ext is None or name in ext) else "Internal"
        return nc.dram_tensor(name, list(shape), dt, kind=kind).ap()

    skind = "ExternalOutput" if dbg else "Internal"

    def dscr(name, shape, dt=F32):
        return nc.dram_tensor(name, list(shape), dt, kind=skind).ap()

    xin = din("xin", [NT, D])
    cT = din("cT", [128, 16])
    w_ada = din("w_ada", [2, D, INW])
    b_ada = din("b_ada", [2, INW])
    norm_mix = din("norm_mix", [2, D])
    norm_ffn = din("norm_ffn", [2, D])
    w_in = din("w_in", [2, D, INW])
    ret_decay = din("ret_decay", [2, 8])
    ret_gn = din("ret_gn", [2, 512])
    attn_qn = din("attn_qn", [2, 128])
    attn_kn = din("attn_kn", [2, 128])
    pool_w = din("pool_w", [2, 4, 128, 128])
    pool_scale = din("pool_scale", [2, 512])
    w_branch = din("w_branch", [2, 3, 512, D])
    w_out = din("w_out", [2, D, D])
    ffn_w1 = din("ffn_w1", [1, D, FFN])
    ffn_w3 = din("ffn_w3", [1, D, FFN])
    ffn_w2 = din("ffn_w2", [1, FFN, D])
    moe_router_t = din("moe_router_t", [NE, D])
    moe_router_p = din("moe_router_p", [128, 8, NE])
    moe_router_b = din("moe_router_b", [1, NE])
    w1q = [[din("w1q_%d_%d" % (fb, h), [NE * 128, 2048]) for h in range(2)] for fb in range(7)]
    w3q = [[din("w3q_%d_%d" % (fb, h), [NE * 128, 2048]) for h in range(2)] for fb in range(7)]
    w2q = [[din("w2q_%d_%d" % (fb, h), [NE * 128, 2048]) for h in range(2)] for fb in range(7)]
    final_norm = din("final_norm", [D])
    k_ident = din("k_ident", [128, 128])
    k_acos = din("k_acos", [128, NTILE, 64])
    k_asin = din("k_asin", [128, NTILE, 64])
    k_rt4 = din("k_rt4", [128, NTILE, 4, 32])
    k_rtab = din("k_rtab", [128, 6, 128])
    k_pcol = din("k_pcol", [128, 2])
    k_pedge = din("k_pedge", [128, 4, 2, 8])
    k_tri = din("k_tri", [128, 128])
    k_iot = din("k_iot", [128, 25])

    out = nc.dram_tensor("out", [SEQ, D], F32, kind="ExternalOutput").ap()

    modd = dscr("modd", [2, 2, 6, 128, D])
    hT_d = dscr("hT_d", [128, 8, NT], BF16)
    yT_d = dscr("yT_d", [3, 128, 4, NT], BF16)
    xa_d = dscr("xa_d", [NT, D])
    xb_d = dscr("xb_d", [NT, D])
    xc_d = dscr("xc_d", [NT, D])
    xd_d = dscr("xd_d", [NT, D])
    G_d = dscr("G_d", [24 * 512, D], BF16)
    Y_d = dscr("Y_d", [24 * 512, D])

    ps = [nc.alloc_psum_tensor("ps%d" % i, [128, 512], F32) for i in range(8)]
    PS = ["ps%d" % i for i in range(8)]

    GROUPS = [(g * 512, 512) for g in range(8)] + [(SEQ, CTX)]

    ident = A.alloc("ident", [128, 128], BF16)
    identf = A.alloc("identf", [128, 128], F32)
    ones_bf = A.alloc("ones", [128, 128], BF16)
    P.dma("sync", identf[:], k_ident, writes=["identf"])
    P.op("vector", lambda e: e.tensor_copy(ident[:], identf[:]), reads=["identf"], writes=["ident"])
    P.op("vector", lambda e: e.memset(ones_bf[:], 1.0), writes=["ones"])
    P.barrier()
    A_PERSIST = A.mark()

    def done(tag):
        return stop_after is not None and stop_after == tag

    def finish():
        P.mute = False
        P.barrier()
        P.emit()
        return nc

    def make_normT(pfx, Amod, Bmod, psbank):
        xt = [A.alloc(pfx + "xt%d" % i, [128, D], F32) for i in range(2)]
        junks = [A.alloc(pfx + "junk%d" % i, [128, D], BF16) for i in range(2)]
        tmps = [A.alloc(pfx + "tmp%d" % i, [128, D], F32) for i in range(2)]
        hb = [A.alloc(pfx + "hb%d" % i, [128, D], BF16) for i in range(4)]
        sts = [A.alloc(pfx + "st%d" % i, [128, 4], F32) for i in range(2)]
        cnt = [0]

        def runA(src_d, t0, ntl, which):
            hs = []
            for j in range(ntl):
                i = cnt[0]
                cnt[0] += 1
                s = i % 2
                x_t, h_b = xt[s], hb[i % 4]
                junk, tmp, st = junks[s], tmps[s], sts[s]
                xtok, htok = pfx + "xt%d" % s, pfx + "hb%d" % (i % 4)
                sfx = "_%d" % s
                r0 = t0 + j * 128
                P.dma("sync", x_t[:], src_d[r0:r0 + 128, :], writes=[xtok])
                P.op("scalar", lambda e, x_t=x_t, junk=junk, st=st: e.activation(junk[:], x_t[:], AF.Square, accum_out=st[:, 0:1]),
                     reads=[xtok], writes=[pfx + "junk" + sfx, pfx + "st" + sfx])
                P.op("scalar", lambda e, st=st: e.activation(st[:, 1:2], st[:, 0:1], AF.Sqrt, bias=NORM_EPS, scale=1.0 / D),
                     reads=[pfx + "st" + sfx], writes=[pfx + "st1" + sfx])
                P.op("vector", lambda e, st=st: e.reciprocal(st[:, 2:3], st[:, 1:2]),
                     reads=[pfx + "st1" + sfx], writes=[pfx + "st2" + sfx])
                am, bm = Amod[which], Bmod[which]
                P.op("vector", lambda e, x_t=x_t, am=am, tmp=tmp, st=st: e.scalar_tensor_tensor(tmp[:], x_t[:], st[:, 2:3], am[0][:], ALU.mult, ALU.mult),
                     reads=[xtok, pfx + "st2" + sfx, am[1]], writes=[pfx + "tmp" + sfx])
                P.op("gpsimd", lambda e, h_b=h_b, bm=bm, tmp=tmp: e.tensor_tensor(h_b[:], tmp[:], bm[0][:], ALU.add),
                     reads=[pfx + "tmp" + sfx, bm[1]], writes=[htok])
                hs.append((h_b, htok))
            return hs

        def runB(hs, dst, dst_tok):
            pb = ps[psbank][:].bitcast(BF16)
            for j, (h_b, htok) in enumerate(hs):
                for k in range(8):
                    P.op("tensor", lambda e, k=k, h_b=h_b: e.transpose(pb[:, k * 128:(k + 1) * 128], h_b[:, k * 128:(k + 1) * 128], ident[:]),
                         reads=[htok, "ident"], writes=[PS[psbank]])
                P.op("scalar", lambda e, j=j: e.copy(dst[:, :, j * 128:(j + 1) * 128], pb[:, 0:1024].rearrange("p (k t) -> p k t", k=8)),
                     reads=[PS[psbank]], writes=[dst_tok])

        def run(src_d, t0, ntl, dst, dst_tok, which):
            prev = None
            for j in range(ntl):
                cur = (runA(src_d, t0 + j * 128, 1, which), j)
                if prev is not None:
                    runB(prev[0], dst[:, :, prev[1] * 128:(prev[1] + 1) * 128], dst_tok)
                prev = cur
            runB(prev[0], dst[:, :, prev[1] * 128:(prev[1] + 1) * 128], dst_tok)

        return runA, runB, run

    def load_mod(L, idxs):
        res = {}
        for idx in idxs:
            pair = []
            for w in range(2):
                t = A.alloc("mod%d_%d" % (idx, w), [128, D], F32)
                tok = "mod%d_%d" % (idx, w)
                P.dma("sync", t[:], modd[L, w, idx], reads=[("modd", L, w, idx)], writes=[tok])
                pair.append((t, tok))
            res[idx] = pair
        return res

    P.mute = "mod" in skip
    A.reset(A_PERSIST)
    cs = A.alloc("cs", [128, 16], F32)
    ss = A.alloc("ss", [128, 16], F32)
    sbl = A.alloc("sbl", [128, 16, 128], BF16)
    P.dma("sync", cs[:], cT, writes=["cs"])
    P.op("scalar", lambda e: e.activation(ss[:], cs[:], AF.Silu), reads=["cs"], writes=["ss"])
    P.op("vector", lambda e: e.tensor_copy(sbl[:], ss[:].unsqueeze(2).to_broadcast([128, 16, 128])), reads=["ss"], writes=["sbl"])
    wad = [A.alloc("wad%d" % i, [128, 8, 512], BF16) for i in range(2)]
    bt = [A.alloc("bt%d" % i, [128, 512], F32) for i in range(2)]
    gn = [A.alloc("gnm%d" % i, [128, D], F32) for i in range(2)]
    mo = [A.alloc("mo%d" % i, [128, 512], F32) for i in range(4)]
    it = 0
    for L in range(2):
        P.dma("sync", gn[0][:], norm_mix[L].partition_broadcast(128), writes=["gn0"])
        P.dma("sync", gn[1][:], norm_ffn[L].partition_broadcast(128), writes=["gn1"])
        for cb in range(12):
            s = it % 2
            it += 1
            idx = cb // 2
            c0 = (cb % 2) * 512
            P.dma("gpsimd", wad[s][:], w_ada[L, :, cb * 512:(cb + 1) * 512].rearrange("(k p) f -> p k f", p=128), writes=["wad%d" % s])
            P.dma("sync", bt[s][:], b_ada[L, cb * 512:(cb + 1) * 512].partition_broadcast(128), writes=["bt%d" % s])
            for w in range(2):
                bank = 2 * s + w
                for k in range(8):
                    P.op("tensor", lambda e, k=k, w=w, s=s, bank=bank: e.matmul(ps[bank][:], sbl[:, w * 8 + k, :], wad[s][:, k, :], start=(k == 0), stop=(k == 7)),
                         reads=["sbl", "wad%d" % s], writes=[PS[bank]])
                m = mo[bank]
                mtok = "mo%d" % bank
                P.op("vector", lambda e, m=m, bank=bank, s=s: e.tensor_tensor(m[:], ps[bank][:], bt[s][:], ALU.add),
                     reads=[PS[bank], "bt%d" % s], writes=[mtok])
                if idx in (1, 4):
                    g = gn[0] if idx == 1 else gn[1]
                    gtok = "gn0" if idx == 1 else "gn1"
                    P.op("vector", lambda e, m=m, g=g, c0=c0: e.scalar_tensor_tensor(m[:], m[:], 1.0, g[:, c0:c0 + 512], ALU.add, ALU.mult),
                         reads=[mtok, gtok], writes=[mtok])
                P.dma("sync", modd[L, w, idx, :, c0:c0 + 512], m[:], reads=[mtok], writes=[("modd", L, w, idx, c0)])
    P.barrier()
    if done("mod"):
        return finish()

    def layer(L, x_src, x_mix, x_out, need_ctx, moe):
        ngrp = 9
        P.mute = "p1" in skip
        A.reset(A_PERSIST)
        md = load_mod(L, [0, 1])
        _, _, normT = make_normT("p1", md[1], md[0], 0)
        hTt = [A.alloc("hTt%d" % i, [128, 8, 512], BF16) for i in range(2)]
        for g, (t0, n) in enumerate(GROUPS):
            s = g % 2
            normT(x_src, t0, n // 128, hTt[s], "hTt%d" % s, 0 if g < 8 else 1)
            P.dma("sync", hT_d[:, :, t0:t0 + n], hTt[s][:, :, 0:n], reads=["hTt%d" % s], writes=[("hT", g)])
        P.barrier()
        if done("p1_%d" % L):
            return True

        P.mute = "p2" in skip
        A.reset(A_PERSIST)
        Wa = A.alloc("Wa", [128, 8, 1024], BF16)
        for cbk in range(2):
            P.dma("gpsimd", Wa[:, :, cbk * 512:(cbk + 1) * 512], w_in[L, :, 1536 + cbk * 512:1536 + (cbk + 1) * 512].rearrange("(k p) f -> p k f", p=128), writes=[("Wa", cbk)])
        QT = A.alloc("QT", [128, 4, NT], BF16)
        KT = A.alloc("KT", [128, 2, NT], BF16)
        Vres = A.alloc("Vres", [128, NTILE, 256], BF16)
        acos = A.alloc("acos", [128, NTILE, 64], F32)
        asin = A.alloc("asin", [128, NTILE, 64], F32)
        P.dma("sync", acos[:], k_acos, writes=["acos"])
        P.dma("sync", asin[:], k_asin, writes=["asin"])
        gq = A.alloc("gq", [128, 128], F32)
        gk = A.alloc("gk", [128, 128], F32)
        P.dma("sync", gq[:], attn_qn[L].partition_broadcast(128), writes=["gq"])
        P.dma("sync", gk[:], attn_kn[L].partition_broadcast(128), writes=["gk"])
        m_prep = A.mark()
        hg = [A.alloc("hg%d" % i, [128, 8, 512], BF16) for i in range(2)]
        sqs = [A.alloc("sq%d" % i, [128, 768], F32) for i in range(2)]
        st6s = [A.alloc("st6_%d" % i, [128, 6], F32) for i in range(2)]
        st6bs = [A.alloc("st6b%d" % i, [128, 6], F32) for i in range(2)]
        rs6s = [A.alloc("rs6_%d" % i, [128, 6], F32) for i in range(2)]
        qns = [A.alloc("qn%d" % i, [128, 6, 128], F32) for i in range(2)]
        tas = [A.alloc("ta%d" % i, [128, 6, 64], F32) for i in range(2)]
        tbs = [A.alloc("tb%d" % i, [128, 6, 64], F32) for i in range(2)]
        tcs = [A.alloc("tc%d" % i, [128, 6, 64], F32) for i in range(2)]
        tds = [A.alloc("td%d" % i, [128, 6, 64], F32) for i in range(2)]
        qrs = [A.alloc("qr%d" % i, [128, 6, 128], BF16) for i in range(2)]
        prev_gen = [None]
        for g, (t0, n) in enumerate(GROUPS):
            s = g % 2
            P.dma("sync", hg[s][:, :, 0:n], hT_d[:, :, t0:t0 + n], reads=[("hT", g)], writes=["hg%d" % s])
            for j in range(n // 128):
                def tile_body(g=g, s=s, j=j, ti=(t0 // 128) + j):
                    u = ti % 2
                    sfx = "_%d" % u
                    sq, st6, st6b, rs6, qn = sqs[u], st6s[u], st6bs[u], rs6s[u], qns[u]
                    ta, tb, tc_, td, qr = tas[u], tbs[u], tcs[u], tds[u], qrs[u]
                    bk0, bk1, bk2 = (0, 1, 2) if u == 0 else (3, 4, 5)
                    for k in range(8):
                        P.op("tensor", lambda e, k=k, s=s, j=j: e.matmul(ps[bk0][:], hg[s][:, k, j * 128:(j + 1) * 128], Wa[:, k, 0:512], start=(k == 0), stop=(k == 7)),
                             reads=["hg%d" % s, ("Wa", 0)], writes=[PS[bk0]])
                    for k in range(8):
                        P.op("tensor", lambda e, k=k, s=s, j=j: e.matmul(ps[bk1][:], hg[s][:, k, j * 128:(j + 1) * 128], Wa[:, k, 512:1024], start=(k == 0), stop=(k == 7)),
                             reads=["hg%d" % s, ("Wa", 1)], writes=[PS[bk1]])
                    P.op("scalar", lambda e: e.activation(sq[:, 0:512], ps[bk0][:], AF.Square), reads=[PS[bk0]], writes=["sq" + sfx])
                    P.op("scalar", lambda e: e.activation(sq[:, 512:768], ps[bk1][:, 0:256], AF.Square), reads=[PS[bk1]], writes=["sqb" + sfx])
                    P.op("vector", lambda e: e.reduce_sum(st6[:], sq[:].rearrange("p (h d) -> p h d", d=128), axis=AX.X),
                         reads=["sq" + sfx, "sqb" + sfx], writes=["st6" + sfx])
                    P.op("scalar", lambda e: e.activation(st6b[:], st6[:], AF.Sqrt, bias=NORM_EPS, scale=1.0 / 128), reads=["st6" + sfx], writes=["st6b" + sfx])
                    P.op("vector", lambda e: e.reciprocal(rs6[:], st6b[:]), reads=["st6b" + sfx], writes=["rs6" + sfx])
                    P.op("vector", lambda e: e.tensor_tensor(qn[:, 0:4, :], ps[bk0][:].rearrange("p (h d) -> p h d", d=128),
                                                             rs6[:, 0:4].unsqueeze(2).to_broadcast([128, 4, 128]), ALU.mult),
                         reads=[PS[bk0], "rs6" + sfx], writes=["qn_q" + sfx])
                    P.op("vector", lambda e: e.tensor_tensor(qn[:, 4:6, :], ps[bk1][:, 0:256].rearrange("p (h d) -> p h d", d=128),
                                                             rs6[:, 4:6].unsqueeze(2).to_broadcast([128, 2, 128]), ALU.mult),
                         reads=[PS[bk1], "rs6" + sfx], writes=["qn_k" + sfx])
                    P.op("scalar", lambda e, ti=ti: e.copy(Vres[:, ti, :], ps[bk1][:, 256:512]), reads=[PS[bk1]], writes=[("Vres", ti)])
                    P.op("gpsimd", lambda e: e.tensor_tensor(qn[:, 0:4, :], qn[:, 0:4, :], gq[:].unsqueeze(1).to_broadcast([128, 4, 128]), ALU.mult),
                         reads=["qn_q" + sfx, "gq"], writes=["qn_q" + sfx])
                    P.op("gpsimd", lambda e: e.tensor_tensor(qn[:, 4:6, :], qn[:, 4:6, :], gk[:].unsqueeze(1).to_broadcast([128, 2, 128]), ALU.mult),
                         reads=["qn_k" + sfx, "gk"], writes=["qn_k" + sfx])
                    yield
                    x0 = qn[:, :, 0::2]
                    x1 = qn[:, :, 1::2]
                    cb_ = acos[:, ti, :].unsqueeze(1).to_broadcast([128, 6, 64])
                    sb_ = asin[:, ti, :].unsqueeze(1).to_broadcast([128, 6, 64])
                    P.op("vector", lambda e, x0=x0, cb_=cb_: e.tensor_tensor(ta[:], x0, cb_, ALU.mult), reads=["qn_q" + sfx, "qn_k" + sfx, "acos"], writes=["ta" + sfx])
                    P.op("gpsimd", lambda e, x1=x1, sb_=sb_: e.tensor_tensor(tb[:], x1, sb_, ALU.mult), reads=["qn_q" + sfx, "qn_k" + sfx, "asin"], writes=["tb" + sfx])
                    P.op("vector", lambda e, x0=x0, sb_=sb_: e.tensor_tensor(tc_[:], x0, sb_, ALU.mult), reads=["qn_q" + sfx, "qn_k" + sfx, "asin"], writes=["tc" + sfx])
                    P.op("gpsimd", lambda e, x1=x1, cb_=cb_: e.tensor_tensor(td[:], x1, cb_, ALU.mult), reads=["qn_q" + sfx, "qn_k" + sfx, "acos"], writes=["td" + sfx])
                    P.op("vector", lambda e: e.tensor_tensor(qr[:, :, 0::2], ta[:], tb[:], ALU.subtract), reads=["ta" + sfx, "tb" + sfx], writes=["qr0" + sfx])
                    P.op("gpsimd", lambda e: e.tensor_tensor(qr[:, :, 1::2], tc_[:], td[:], ALU.add), reads=["tc" + sfx, "td" + sfx], writes=["qr1" + sfx])
                    pbA = ps[bk2][:].bitcast(BF16)
                    for h in range(6):
                        P.op("tensor", lambda e, h=h: e.transpose(pbA[:, h * 128:(h + 1) * 128], qr[:, h, :], ident[:]),
                             reads=["qr0" + sfx, "qr1" + sfx, "ident"], writes=[PS[bk2]])
                    P.op("scalar", lambda e, ti=ti: e.copy(QT[:, :, ti * 128:(ti + 1) * 128], pbA[:, 0:512].rearrange("p (h t) -> p h t", h=4)),
                         reads=[PS[bk2]], writes=[("QT", ti)])
                    P.op("scalar", lambda e, ti=ti: e.copy(KT[:, :, ti * 128:(ti + 1) * 128], pbA[:, 512:768].rearrange("p (h t) -> p h t", h=2)),
                         reads=[PS[bk2]], writes=[("KT", ti)])
                gen_ = tile_body()
                next(gen_)
                if prev_gen[0] is not None:
                    for _ in prev_gen[0]:
                        pass
                prev_gen[0] = gen_
        for _ in prev_gen[0]:
            pass
        P.barrier()
        if done("p2a_%d" % L):
            return True
        A.reset(m_prep)
        pT = [A.alloc("pT%d" % i, [128, 512], BF16) for i in range(3)]
        rden = A.alloc("rden", [128, 512], F32)
        yo = [A.alloc("yo%d" % i, [128, 512], BF16) for i in range(2)]
        scale = float(128 ** -0.5)
        qgroups = list(range(8)) + ([8] if need_ctx else [])
        blocks = []
        oc = 0
        for g in qgroups:
            t0, n = GROUPS[g]
            keys = list(range(NTILE)) if g < 8 else [32, 33]
            for h in range(4):
                for ji, j in enumerate(keys):
                    blocks.append((g, t0, n, h, ji, j, len(keys), oc))
                oc += 1
        LOOK = 2

        def emit_score(bi):
            g, t0, n, h, ji, j, nk, oc_ = blocks[bi]
            sbk = bi % 3
            kvh = h // 2
            P.op("tensor", lambda e: e.matmul(ps[sbk][:, 0:n], KT[:, kvh, j * 128:(j + 1) * 128], QT[:, h, t0:t0 + n], start=True, stop=True),
                 reads=[], writes=[PS[sbk]])
            P.op("scalar", lambda e: e.activation(pT[sbk][:, 0:n], ps[sbk][:, 0:n], AF.Exp, scale=scale),
                 reads=[PS[sbk]], writes=["pT%d" % sbk])

        def emit_pv(bi):
            g, t0, n, h, ji, j, nk, oc_ = blocks[bi]
            sbk = bi % 3
            kvh = h // 2
            ob = 4 + (oc_ % 2) * 2
            db = ob + 1
            pt = pT[sbk]
            P.op("tensor", lambda e: e.matmul(ps[ob][:, 0:n], Vres[:, j, kvh * 128:(kvh + 1) * 128], pt[:, 0:n], start=(ji == 0), stop=(ji == nk - 1)),
                 reads=["pT%d" % sbk], writes=[PS[ob]])
            P.op("tensor", lambda e: e.matmul(ps[db][:, 0:n], ones_bf[:], pt[:, 0:n], start=(ji == 0), stop=(ji == nk - 1)),
                 reads=["pT%d" % sbk], writes=[PS[db]])
            if ji == nk - 1:
                y_o = yo[oc_ % 2]
                ytok = "yo%d" % (oc_ % 2)
                P.op("vector", lambda e: e.reciprocal(rden[:, 0:n], ps[db][:, 0:n]), reads=[PS[db]], writes=["rden"])
                P.op("vector", lambda e: e.tensor_tensor(y_o[:, 0:n], ps[ob][:, 0:n], rden[:, 0:n], ALU.mult),
                     reads=[PS[ob], "rden"], writes=[ytok])
                P.dma("sync", yT_d[1, :, h, t0:t0 + n], y_o[:, 0:n], reads=[ytok], writes=[("yT1", g, h)])

        for bi in range(len(blocks) + LOOK):
            if bi < len(blocks):
                emit_score(bi)
            if bi - LOOK >= 0:
                emit_pv(bi - LOOK)
        P.barrier()
        if done("p2_%d" % L):
            return True
        P.mute = "p3" in skip
        A.reset(A_PERSIST)
        Wr = A.alloc("Wr", [128, 8, 1536], BF16)
        m_wr = A.mark()
        for cbk in range(3):
            P.dma("gpsimd", Wr[:, :, cbk * 512:(cbk + 1) * 512], w_in[L, :, cbk * 512:(cbk + 1) * 512].rearrange("(k p) f -> p k f", p=128), writes=[("Wr", cbk)])
        QTr = A.alloc("QTr", [128, 2, NT], BF16)
        KTr = A.alloc("KTr", [128, 2, NT], BF16)
        Kres = A.alloc("Kres", [128, NTILE, 256], BF16)
        Vr = A.alloc("Vr", [128, NTILE, 512], BF16)
        SG = A.alloc("SG", [128, NTILE, 512], BF16)
        rtab = A.alloc("rtab", [128, 6, 128], F32)
        pcol = A.alloc("pcol", [128, 2], F32)
        rd8 = A.alloc("rd8", [128, 8], F32)
        lg8 = A.alloc("lg8", [128, 8], F32)
        lgp = A.alloc("lgp", [128, 4], F32)
        gch = A.alloc("gch", [128, 4], F32)
        Dcomb = A.alloc("Dcomb", [128, 4, 128], F32)
        XI = A.alloc("XI", [128, 4, 128], F32)
        ZZ = A.alloc("ZZ", [128, 2, 256], F32)
        XIm = A.alloc("XIm", [128, 6, 2, 128], F32)
        mk = A.alloc("mk", [128, 2], F32)
        P.op("vector", lambda e: e.memset(mk[:], 0.0), writes=["mk"])
        P.op("vector", lambda e: e.memset(mk[0:64, 0:1], 1.0), reads=["mk"], writes=["mk"])
        P.op("vector", lambda e: e.memset(mk[64:128, 1:2], 1.0), reads=["mk"], writes=["mk"])
        gnw = A.alloc("gnw", [128, 512], F32)
        t1 = A.alloc("t1", [128, 128], F32)
        t2 = A.alloc("t2", [128, 128], F32)
        P.dma("sync", rtab[:], k_rtab, writes=["rtab"])
        P.dma("sync", pcol[:], k_pcol, writes=["pcol"])
        P.dma("sync", rd8[:], ret_decay[L].partition_broadcast(128), writes=["rd8"])
        P.dma("sync", gnw[:], ret_gn[L].partition_broadcast(128), writes=["gnw"])
        P.op("scalar", lambda e: e.activation(lg8[:], rd8[:], AF.Exp), reads=["rd8"], writes=["lg8a"])
        P.op("vector", lambda e: e.tensor_scalar(lg8[:], lg8[:], -1.0, None, ALU.mult), reads=["lg8a"], writes=["lg8"])
        for j in range(2):
            for dr in range(2):
                P.op("vector", lambda e, j=j, dr=dr: e.tensor_copy(lgp[0:64, dr * 2 + j:dr * 2 + j + 1], lg8[0:64, dr * 4 + 2 * j:dr * 4 + 2 * j + 1]), reads=["lg8"], writes=[("lgp", j, dr, 0)])
                P.op("vector", lambda e, j=j, dr=dr: e.tensor_copy(lgp[64:128, dr * 2 + j:dr * 2 + j + 1], lg8[64:128, dr * 4 + 2 * j + 1:dr * 4 + 2 * j + 2]), reads=["lg8"], writes=[("lgp", j, dr, 1)])
        LGP = [("lgp", j, dr, hh) for j in range(2) for dr in range(2) for hh in range(2)]
        P.op("scalar", lambda e: e.activation(gch[:], lgp[:], AF.Exp, scale=128.0), reads=LGP, writes=["gch"])
        for h in range(4):
            P.op("scalar", lambda e, h=h: e.activation(t1[:], rtab[:, 0, :], AF.Exp, scale=lg8[:, h:h + 1]), reads=["rtab", "lg8"], writes=["t1"])
            P.op("vector", lambda e: e.tensor_tensor(t1[:], t1[:], rtab[:, 1, :], ALU.mult), reads=["t1", "rtab"], writes=["t1"])
            P.op("scalar", lambda e, h=h: e.activation(t2[:], rtab[:, 2, :], AF.Exp, scale=lg8[:, 4 + h:5 + h]), reads=["rtab", "lg8"], writes=["t2"])
            P.op("vector", lambda e: e.tensor_tensor(t2[:], t2[:], rtab[:, 3, :], ALU.mult), reads=["t2", "rtab"], writes=["t2"])
            P.op("vector", lambda e, h=h: e.tensor_tensor(Dcomb[:, h, :], t1[:], t2[:], ALU.add), reads=["t1", "t2"], writes=[("Dcomb", h)])
            for dr in range(2):
                P.op("scalar", lambda e, h=h, dr=dr: e.activation(ZZ[:, dr, h * 64:(h + 1) * 64], lg8[:, dr * 4 + h:dr * 4 + h + 1].to_broadcast([128, 64]), AF.Exp, scale=pcol[:, dr:dr + 1]),
                     reads=["lg8", "pcol"], writes=[("ZZ", dr, h)])
        for j in range(2):
            for dr in range(2):
                P.op("scalar", lambda e, j=j, dr=dr: e.activation(XI[:, dr * 2 + j, :], rtab[:, 4 + dr, :], AF.Exp, scale=lgp[:, dr * 2 + j:dr * 2 + j + 1]),
                     reads=["rtab"] + LGP, writes=[("XI", dr, j)])
        XIT = [("XI", dr, j) for dr in range(2) for j in range(2)]
        for hh in range(2):
            for dr in range(2):
                P.op("vector", lambda e, hh=hh, dr=dr: e.tensor_scalar(XIm[:, 2 * dr + hh], XI[:, dr * 2:dr * 2 + 2, :], mk[:, hh:hh + 1], None, ALU.mult), reads=XIT + ["mk"], writes=[("XIm", 2 * dr + hh)])
            P.op("vector", lambda e, hh=hh: e.tensor_copy(XIm[:, 4 + hh].rearrange("p j l -> p (j l)"), mk[:, hh:hh + 1].to_broadcast([128, 256])), reads=["mk"], writes=[("XIm", 4 + hh)])
        if done("p3a_%d" % L):
            return True
        m_rprep = A.mark()
        hgrs = [A.alloc("hgr%d" % i, [128, 8, 512], BF16) for i in range(2)]
        rt4 = [A.alloc("rt4_%d" % i, [128, 4, 32], F32) for i in range(2)]
        ras = [A.alloc("ra%d" % i, [128, 8, 32], F32) for i in range(2)]
        rbs = [A.alloc("rb%d" % i, [128, 8, 32], F32) for i in range(2)]
        rcs = [A.alloc("rc%d" % i, [128, 8, 32], F32) for i in range(2)]
        rdds = [A.alloc("rdd%d" % i, [128, 8, 32], F32) for i in range(2)]
        qrrs = [A.alloc("qrr%d" % i, [128, 256], BF16) for i in range(2)]
        prev_r = [None]

        def r_load(g_):
            t0_, n_ = GROUPS[g_]
            P.dma("sync", hgrs[g_ % 2][:, :, 0:n_], hT_d[:, :, t0_:t0_ + n_], reads=[("hT", g_)], writes=["hgr%d" % (g_ % 2)])

        r_load(0)
        for g, (t0, n) in enumerate(GROUPS):
            if g + 1 < len(GROUPS):
                r_load(g + 1)
            for j in range(n // 128):
                def r_tile(g=g, j=j, ti=(t0 // 128) + j):
                    hgr = hgrs[g % 2]
                    hgtok = "hgr%d" % (g % 2)
                    u = ti % 2
                    s = u
                    sfx = "_%d" % u
                    ra, rb, rc_, rdd, qrr = ras[u], rbs[u], rcs[u], rdds[u], qrrs[u]
                    b0 = 0 if u == 0 else 4
                    P.dma("sync", rt4[s][:], k_rt4[:, ti], writes=["rt4_%d" % s])
                    for blk in range(3):
                        for k in range(8):
                            P.op("tensor", lambda e, k=k, j=j, blk=blk: e.matmul(ps[b0 + blk][:], hgr[:, k, j * 128:(j + 1) * 128], Wr[:, k, blk * 512:(blk + 1) * 512], start=(k == 0), stop=(k == 7)),
                                 reads=[hgtok, ("Wr", blk)], writes=[PS[b0 + blk]])
                    P.op("scalar", lambda e, ti=ti: e.copy(Vr[:, ti, :], ps[b0 + 1][:]), reads=[PS[b0 + 1]], writes=[("Vr", ti)])
                    P.op("scalar", lambda e, ti=ti: e.activation(SG[:, ti, :], ps[b0 + 2][:], AF.Silu), reads=[PS[b0 + 2]], writes=[("SG", ti)])
                    pv = ps[b0][:].rearrange("p (h d) -> p h d", d=64)
                    for hs, (ci, si) in ((slice(0, 4), (0, 1)), (slice(4, 8), (2, 3))):
                        x0 = pv[:, hs, 0::2]
                        x1 = pv[:, hs, 1::2]
                        cb_ = rt4[s][:, ci, :].unsqueeze(1).to_broadcast([128, 4, 32])
                        sb_ = rt4[s][:, si, :].unsqueeze(1).to_broadcast([128, 4, 32])
                        tk = "q" if ci == 0 else "k"
                        P.op("vector", lambda e, x0=x0, cb_=cb_, hs=hs: e.tensor_tensor(ra[:, hs, :], x0, cb_, ALU.mult), reads=[PS[b0], "rt4_%d" % s], writes=["ra" + tk + sfx])
                        P.op("vector", lambda e, x1=x1, sb_=sb_, hs=hs: e.tensor_tensor(rb[:, hs, :], x1, sb_, ALU.mult), reads=[PS[b0], "rt4_%d" % s], writes=["rb" + tk + sfx])
                        P.op("vector", lambda e, x0=x0, sb_=sb_, hs=hs: e.tensor_tensor(rc_[:, hs, :], x0, sb_, ALU.mult), reads=[PS[b0], "rt4_%d" % s], writes=["rc" + tk + sfx])
                        P.op("vector", lambda e, x1=x1, cb_=cb_, hs=hs: e.tensor_tensor(rdd[:, hs, :], x1, cb_, ALU.mult), reads=[PS[b0], "rt4_%d" % s], writes=["rd" + tk + sfx])
                    yield
                    qv = qrr[:].rearrange("p (h d) -> p h d", d=64)
                    kv_ = Kres[:, ti, :].rearrange("p (h d) -> p h d", d=64)
                    P.op("gpsimd", lambda e, qv=qv: e.tensor_tensor(qv[:, :, 0::2], ra[:, 0:4, :], rb[:, 0:4, :], ALU.subtract), reads=["raq" + sfx, "rbq" + sfx], writes=["qrr0" + sfx])
                    P.op("gpsimd", lambda e, qv=qv: e.tensor_tensor(qv[:, :, 1::2], rc_[:, 0:4, :], rdd[:, 0:4, :], ALU.add), reads=["rcq" + sfx, "rdq" + sfx], writes=["qrr1" + sfx])
                    P.op("gpsimd", lambda e, kv_=kv_: e.tensor_tensor(kv_[:, :, 0::2], ra[:, 4:8, :], rb[:, 4:8, :], ALU.subtract), reads=["rak" + sfx, "rbk" + sfx], writes=[("Kres0", ti)])
                    P.op("gpsimd", lambda e, kv_=kv_: e.tensor_tensor(kv_[:, :, 1::2], rc_[:, 4:8, :], rdd[:, 4:8, :], ALU.add), reads=["rck" + sfx, "rdk" + sfx], writes=[("Kres1", ti)])
                    pbB = ps[b0 + 3][:].bitcast(BF16)
                    for jj in range(2):
                        P.op("tensor", lambda e, jj=jj: e.transpose(pbB[:, jj * 128:(jj + 1) * 128], qrr[:, jj * 128:(jj + 1) * 128], ident[:]), reads=["qrr0" + sfx, "qrr1" + sfx, "ident"], writes=[PS[b0 + 3]])
                    for jj in range(2):
                        P.op("tensor", lambda e, jj=jj, ti=ti: e.transpose(pbB[:, 256 + jj * 128:256 + (jj + 1) * 128], Kres[:, ti, jj * 128:(jj + 1) * 128], ident[:]), reads=[("Kres0", ti), ("Kres1", ti), "ident"], writes=[PS[b0 + 3]])
                    P.op("scalar", lambda e, ti=ti: e.copy(QTr[:, :, ti * 128:(ti + 1) * 128], pbB[:, 0:256].rearrange("p (h t) -> p h t", h=2)), reads=[PS[b0 + 3]], writes=[("QTr", ti)])
                    P.op("scalar", lambda e, ti=ti: e.copy(KTr[:, :, ti * 128:(ti + 1) * 128], pbB[:, 256:512].rearrange("p (h t) -> p h t", h=2)), reads=[PS[b0 + 3]], writes=[("KTr", ti)])
                gen_ = r_tile()
                next(gen_)
                if prev_r[0] is not None:
                    for _ in prev_r[0]:
                        pass
                prev_r[0] = gen_
        for _ in prev_r[0]:
            pass
        P.barrier()
        if done("p3b_%d" % L):
            return True
        A.reset(m_rprep)
        Sb_all = nc.alloc_sbuf_tensor_at("Sb_all_%d" % L, [128, NTILE, 2, 128], BF16, offset=A_PERSIST)
        Sf = A.alloc("Sf", [128, 2, 128], F32)
        Sb = A.alloc("Sb", [128, 2, 128], F32)
        Sfb = [A.alloc("Sfb%d" % i, [128, 2, 128], BF16) for i in range(2)]
        kz = [A.alloc("kz%d" % i, [128, 256], BF16) for i in range(2)]
        PTs = [A.alloc("PT%d" % i, [128, 512], BF16) for i in range(2)]
        qms = [[A.alloc("qm%d_%d" % (i, q), [128, 2, 128], BF16) for i in range(6)] for q in range(2)]
        sqrs = [A.alloc("sqr%d" % i, [128, 512], F32) for i in range(2)]
        onrs = [A.alloc("onr%d" % i, [128, 512], F32) for i in range(2)]
        osbs = [A.alloc("osb%d" % i, [128, 512], F32) for i in range(2)]
        gss = [A.alloc("gs%d" % i, [128, 24], F32) for i in range(2)]
        yrbs = [A.alloc("yrb%d" % i, [128, 512], BF16) for i in range(2)]
        yrT = [A.alloc("yrT%d" % i, [128, 4, 512], BF16) for i in range(2)]
        P.op("vector", lambda e: e.memset(Sf[:], 0.0), writes=["Sf"])
        P.op("vector", lambda e: e.memset(Sb[:], 0.0), writes=["Sb"])

        def state_update(S, Stok, dr, c, bank):
            kzt = kz[dr]
            P.op("gpsimd", lambda e: e.tensor_tensor(kzt[:], Kres[:, c, :], ZZ[:, dr, :], ALU.mult), reads=[], writes=["kz%d" % dr])
            for h in range(4):
                P.op("tensor", lambda e, h=h: e.matmul(ps[bank][:, h * 128:(h + 1) * 128], kzt[:, (h // 2) * 128:(h // 2 + 1) * 128], Vr[:, c, h * 128:(h + 1) * 128], start=True, stop=True),
                     reads=["kz%d" % dr], writes=[PS[bank]])
            for j in range(2):
                for hh in range(2):
                    r0 = hh * 64
                    h = 2 * j + hh
                    P.op("vector", lambda e, j=j, r0=r0, h=h: e.scalar_tensor_tensor(S[r0:r0 + 64, j, :], S[r0:r0 + 64, j, :], gch[r0:r0 + 64, dr * 2 + j:dr * 2 + j + 1],
                                                                                   ps[bank][r0:r0 + 64, h * 128:(h + 1) * 128], ALU.mult, ALU.add),
                         reads=[PS[bank], Stok], writes=[Stok])

        order_b = [33, 32] + list(range(31, -1, -1))
        order_f = [32, 33] + list(range(32))
        for c in order_b:
            P.op("vector", lambda e, c=c: e.tensor_copy(Sb_all[:, c], Sb[:]), reads=["Sb"], writes=[("Sb_all", c)])
            state_update(Sb, "Sb", 1, c, 4)
        if done("p3c_%d" % L):
            return True
        yi = 0
        yi_box = [0]
        prev_fw = [None]
        for ci, c in enumerate(order_f):
            def fwd_chunk(ci=ci, c=c):
                u = ci % 2
                sfx = "_%d" % u
                PT, qm, sqr, onr, osb, gs, yrb = PTs[u], qms[u], sqrs[u], onrs[u], osbs[u], gss[u], yrbs[u]
                bR, bO, bT = (0, 1, 2) if u == 0 else (4, 5, 6)
                sfb = Sfb[ci % 2]
                sftok = "Sfb%d" % (ci % 2)
                P.op("vector", lambda e, sfb=sfb: e.tensor_copy(sfb[:], Sf[:]), reads=["Sf"], writes=[sftok])
                emit_out = (c < 32) or need_ctx
                if emit_out:
                    for idx in range(6):
                        eng = "vector" if idx % 2 == 0 else "gpsimd"
                        P.op(eng, lambda e, idx=idx, c=c: e.tensor_tensor(qm[idx][:], QTr[:, :, c * 128:(c + 1) * 128], XIm[:, idx], ALU.mult), reads=[], writes=[("qm", idx, u)])
                    for h in range(4):
                        j, hh = h // 2, h % 2
                        P.op("tensor", lambda e, h=h, j=j, hh=hh, c=c: e.matmul(ps[bR][:, h * 128:(h + 1) * 128], KTr[:, j, c * 128:(c + 1) * 128], qm[4 + hh][:, j, :], start=True, stop=True),
                             reads=[("qm", 4 + hh, u)], writes=[PS[bR]])
                    P.op("vector", lambda e: e.tensor_tensor(PT[:], ps[bR][:], Dcomb[:].rearrange("p h l -> p (h l)"), ALU.mult), reads=[PS[bR]], writes=["PT" + sfx])
                    for h in range(4):
                        j, hh = h // 2, h % 2
                        P.op("tensor", lambda e, h=h, c=c: e.matmul(ps[bO][:, h * 128:(h + 1) * 128], PT[:, h * 128:(h + 1) * 128], Vr[:, c, h * 128:(h + 1) * 128], start=True, stop=False),
                             reads=["PT" + sfx], writes=[PS[bO]])
                        P.op("tensor", lambda e, h=h, j=j, hh=hh, sfb=sfb: e.matmul(ps[bO][:, h * 128:(h + 1) * 128], qm[hh][:, j, :], sfb[:, j, :], start=False, stop=False),
                             reads=[("qm", hh, u), sftok], writes=[PS[bO]])
                        P.op("tensor", lambda e, h=h, j=j, hh=hh, c=c: e.matmul(ps[bO][:, h * 128:(h + 1) * 128], qm[2 + hh][:, j, :], Sb_all[:, c, j, :], start=False, stop=True),
                             reads=[("qm", 2 + hh, u), ("Sb_all", c)], writes=[PS[bO]])
                    state_update(Sf, "Sf", 0, c, 3 if u == 0 else 7)
                    yield
                    yi = yi_box[0]
                    ov = ps[bO][:].rearrange("p (h e) -> p h e", e=128)
                    P.op("scalar", lambda e: e.activation(sqr[:], ps[bO][:], AF.Square), reads=[PS[bO]], writes=["sqr" + sfx])
                    P.op("scalar", lambda e: e.copy(osb[:], ps[bO][:]), reads=[PS[bO]], writes=["osb" + sfx])
                    ov = osb[:].rearrange("p (h e) -> p h e", e=128)
                    P.op("vector", lambda e, ov=ov: e.reduce_sum(gs[:, 0:4], ov, axis=AX.X), reads=["osb" + sfx], writes=["gs0" + sfx])
                    P.op("vector", lambda e: e.reduce_sum(gs[:, 4:8], sqr[:].rearrange("p (h e) -> p h e", e=128), axis=AX.X), reads=["sqr" + sfx], writes=["gs1" + sfx])
                    P.op("vector", lambda e: e.tensor_scalar(gs[:, 8:12], gs[:, 0:4], 1.0 / 128, None, ALU.mult), reads=["gs0" + sfx], writes=["gs2" + sfx])
                    P.op("vector", lambda e: e.tensor_tensor(gs[:, 12:16], gs[:, 8:12], gs[:, 8:12], ALU.mult), reads=["gs2" + sfx], writes=["gs3" + sfx])
                    P.op("vector", lambda e: e.scalar_tensor_tensor(gs[:, 16:20], gs[:, 4:8], 1.0 / 128, gs[:, 12:16], ALU.mult, ALU.subtract), reads=["gs1" + sfx, "gs3" + sfx], writes=["gs4" + sfx])
                    P.op("scalar", lambda e: e.activation(gs[:, 20:24], gs[:, 16:20], AF.Sqrt, bias=GN_EPS, scale=1.0), reads=["gs4" + sfx], writes=["gs5" + sfx])
                    P.op("vector", lambda e: e.reciprocal(gs[:, 20:24], gs[:, 20:24]), reads=["gs5" + sfx], writes=["gs5" + sfx])
                    onv = onr[:].rearrange("p (h e) -> p h e", e=128)
                    P.op("vector", lambda e, ov=ov, onv=onv: e.tensor_tensor(onv, ov, gs[:, 8:12].unsqueeze(2).to_broadcast([128, 4, 128]), ALU.subtract), reads=["osb" + sfx, "gs2" + sfx], writes=["onr" + sfx])
                    P.op("gpsimd", lambda e, onv=onv: e.tensor_tensor(onv, onv, gs[:, 20:24].unsqueeze(2).to_broadcast([128, 4, 128]), ALU.mult), reads=["onr" + sfx, "gs5" + sfx], writes=["onr" + sfx])
                    P.op("gpsimd", lambda e: e.tensor_tensor(onr[:], onr[:], gnw[:], ALU.mult), reads=["onr" + sfx], writes=["onr" + sfx])
                    P.op("vector", lambda e, c=c: e.tensor_tensor(yrb[:], onr[:], SG[:, c, :], ALU.mult), reads=["onr" + sfx], writes=["yrb" + sfx])
                    pbC = ps[bT][:].bitcast(BF16)
                    for h in range(4):
                        P.op("tensor", lambda e, h=h: e.transpose(pbC[:, h * 128:(h + 1) * 128], yrb[:, h * 128:(h + 1) * 128], ident[:]), reads=["yrb" + sfx], writes=[PS[bT]])
                    g = c // 4 if c < 32 else 8
                    t0, n = GROUPS[g]
                    col = (c * 128 - t0)
                    yt = yrT[yi % 2]
                    ytok = "yrT%d" % (yi % 2)
                    P.op("scalar", lambda e, yt=yt, col=col: e.copy(yt[:, :, col:col + 128], pbC[:, 0:512].rearrange("p (h t) -> p h t", h=4)), reads=[PS[bT]], writes=[(ytok, col)])
                    if col + 128 == n:
                        P.dma("sync", yT_d[0, :, :, t0:t0 + n], yt[:, :, 0:n], reads=[(ytok, cc) for cc in range(0, n, 128)], writes=[("yT0", g)])
                        yi_box[0] += 1
                else:
                    state_update(Sf, "Sf", 0, c, 3 if u == 0 else 7)
                    yield
            gen_ = fwd_chunk()
            next(gen_)
            if prev_fw[0] is not None:
                for _ in prev_fw[0]:
                    pass
            prev_fw[0] = gen_
        for _ in prev_fw[0]:
            pass
        P.barrier()
        if done("p3_%d" % L):
            return True

        P.mute = "p4" in skip
        A.reset(A_PERSIST)
        LP = 16 + SEQ + 16 + CTX + 16
        OFFL, OFFC = 16, 16 + SEQ + 16
        Wp = A.alloc("Wp", [128, 8, 512], BF16)
        P.dma("gpsimd", Wp[:], w_in[L, :, 2560:3072].rearrange("(k p) f -> p k f", p=128), writes=["Wp"])
        pw = A.alloc("pw", [128, 4, 128], BF16)
        P.dma("gpsimd", pw[:], pool_w[L].rearrange("g c d -> c g d"), writes=["pw"])
        psc = A.alloc("psc", [128, 4], F32)
        for gi in range(4):
            P.dma("sync", psc[:, gi:gi + 1], pool_scale[L, gi * 128:(gi + 1) * 128].rearrange("(p o) -> p o", o=1), writes=[("psc", gi)])
        pedge = A.alloc("pedge", [128, 4, 2, 8], F32)
        P.dma("sync", pedge[:], k_pedge, writes=["pedge"])
        U = A.alloc("U", [128, 4, LP], F32)
        B1 = A.alloc("B1", [128, LP], F32)
        B2 = A.alloc("B2", [128, LP], F32)
        dT = A.alloc("dT", [128, 4, NT], BF16)
        hgp = [A.alloc("hgp%d" % i, [128, 8, 512], BF16) for i in range(2)]
        yp = [A.alloc("yp%d" % i, [128, 4, 512], BF16) for i in range(2)]
        P.op("gpsimd", lambda e: e.memset(U[:], 0.0), writes=["U"])
        P.op("vector", lambda e: e.memset(B1[:], 0.0), writes=["B1"])
        P.op("vector", lambda e: e.memset(B2[:], 0.0), writes=["B2"])
        for g, (t0, n) in enumerate(GROUPS):
            s = g % 2
            P.dma("sync", hgp[s][:, :, 0:n], hT_d[:, :, t0:t0 + n], reads=[("hT", g)], writes=["hgp%d" % s])
            c0 = OFFL + t0 if g < 8 else OFFC
            for gi in range(4):
                for k in range(8):
                    P.op("tensor", lambda e, k=k, gi=gi, s=s, n=n: e.matmul(ps[gi][:, 0:n], Wp[:, k, gi * 128:(gi + 1) * 128], hgp[s][:, k, 0:n], start=(k == 0), stop=(k == 7)),
                         reads=["hgp%d" % s, "Wp"], writes=[PS[gi]])
                P.op("scalar", lambda e, gi=gi, c0=c0, n=n: e.copy(U[:, gi, c0:c0 + n], ps[gi][:, 0:n]), reads=[PS[gi], "U"], writes=[("U", gi, g)])
        P.barrier()
        for gi, w in enumerate((2, 4, 8, 16)):
            hw = w // 2
            eng = "vector" if gi % 2 == 0 else "gpsimd"
            Ug = U[:, gi, :]
            P.op(eng, lambda e, Ug=Ug: e.tensor_tensor(B1[:, 1:LP], Ug[:, 1:LP], Ug[:, 0:LP - 1], ALU.add), writes=["B1"])
            cur, ctok = B1, "B1"
            if w >= 4:
                P.op(eng, lambda e: e.tensor_tensor(B2[:, 1:LP - 1], B1[:, 0:LP - 2], B1[:, 2:LP], ALU.add), reads=["B1"], writes=["B2"])
                cur, ctok = B2, "B2"
            if w >= 8:
                P.op(eng, lambda e: e.tensor_tensor(B1[:, 2:LP - 2], B2[:, 0:LP - 4], B2[:, 4:LP], ALU.add), reads=["B2"], writes=["B1"])
                cur, ctok = B1, "B1"
            if w >= 16:
                P.op(eng, lambda e: e.tensor_tensor(B2[:, 4:LP - 4], B1[:, 0:LP - 8], B1[:, 8:LP], ALU.add), reads=["B1"], writes=["B2"])
                cur, ctok = B2, "B2"
            for (o0, nn, d0) in ((OFFL, SEQ, 0), (OFFC, CTX, SEQ)):
                P.op(eng, lambda e, cur=cur, o0=o0, gi=gi, hw=hw: e.tensor_tensor(cur[:, o0:o0 + hw], cur[:, o0:o0 + hw], pedge[:, gi, 0, 0:hw], ALU.mult), reads=[ctok, "pedge"], writes=[ctok])
                P.op(eng, lambda e, cur=cur, o0=o0, nn=nn, gi=gi, hw=hw: e.tensor_tensor(cur[:, o0 + nn - hw:o0 + nn], cur[:, o0 + nn - hw:o0 + nn], pedge[:, gi, 1, 0:hw], ALU.mult), reads=[ctok, "pedge"], writes=[ctok])
                P.op("vector", lambda e, cur=cur, o0=o0, nn=nn, d0=d0, gi=gi, w=w, Ug=Ug: e.scalar_tensor_tensor(dT[:, gi, d0:d0 + nn], cur[:, o0:o0 + nn], 1.0 / w, Ug[:, o0:o0 + nn], ALU.mult, ALU.subtract),
                     reads=[ctok], writes=[("dT", gi, d0)])
        P.barrier()
        for g, (t0, n) in enumerate(GROUPS):
            s = g % 2
            for gi in range(4):
                P.op("tensor", lambda e, gi=gi, t0=t0, n=n: e.matmul(ps[gi][:, 0:n], pw[:, gi, :], dT[:, gi, t0:t0 + n], start=True, stop=True), reads=[], writes=[PS[gi]])
                P.op("scalar", lambda e, gi=gi, s=s, n=n: e.activation(yp[s][:, gi, 0:n], ps[gi][:, 0:n], AF.Copy, scale=psc[:, gi:gi + 1]), reads=[PS[gi]], writes=[("yp", s, gi)])
            P.dma("sync", yT_d[2, :, :, t0:t0 + n], yp[s][:, :, 0:n], reads=[("yp", s, gi) for gi in range(4)], writes=[("yT2", g)])
        P.barrier()
        if done("p4_%d" % L):
            return True

        P.mute = "p5" in skip
        A.reset(A_PERSIST)
        Wg = A.alloc("Wg", [128, 8, 3072], BF16)
        Wb = A.alloc("Wb", [128, 12, 1024], BF16)
        for hlf in range(2):
            for cbk in (hlf, 2 + hlf, 4 + hlf):
                P.dma("gpsimd", Wg[:, :, cbk * 512:(cbk + 1) * 512], w_in[L, :, 3072 + cbk * 512:3072 + (cbk + 1) * 512].rearrange("(k p) f -> p k f", p=128), writes=[("Wg", cbk)])
            for b in range(3):
                P.dma("gpsimd", Wb[:, b * 4:(b + 1) * 4, hlf * 512:(hlf + 1) * 512], w_branch[L, b, :, hlf * 512:(hlf + 1) * 512].rearrange("(k p) f -> p k f", p=128), writes=[("Wb", b, hlf)])
        Wo = A.alloc("Wo", [128, 8, 1024], BF16)
        for cbk in range(2):
            P.dma("gpsimd", Wo[:, :, cbk * 512:(cbk + 1) * 512], w_out[L, :, cbk * 512:(cbk + 1) * 512].rearrange("(k p) f -> p k f", p=128), writes=[("Wo", cbk)])
        md = load_mod(L, [2])
        hgms = [A.alloc("hgm%d" % i, [128, 8, 512], BF16) for i in range(2)]
        ygs = [A.alloc("yg%d" % i, [128, 3, 4, 512], BF16) for i in range(2)]
        mixT = A.alloc("mixT", [128, 8, 512], BF16)
        sgm = [A.alloc("sgm%d" % i, [128, 512], F32) for i in range(3)]
        tm = [A.alloc("tm%d" % i, [128, 512], F32) for i in range(3)]
        xtm = [A.alloc("xtm%d" % i, [128, D], F32) for i in range(2)]
        xom = [A.alloc("xom%d" % i, [128, D], F32) for i in range(2)]
        tmo = A.alloc("tmo", [128, 512], F32)
        xi_ = 0
        g5 = [g for g in range(9) if not (g == 8 and not need_ctx)]

        def p5_load(gp):
            g_ = g5[gp]
            t0_, n_ = GROUPS[g_]
            u_ = gp % 2
            P.dma("sync", hgms[u_][:, :, 0:n_], hT_d[:, :, t0_:t0_ + n_], reads=[("hT", g_)], writes=["hgm%d" % u_])
            for b in range(3):
                P.dma("sync", ygs[u_][:, b, :, 0:n_], yT_d[b, :, :, t0_:t0_ + n_], writes=[("yg", b, u_)])

        p5_load(0)
        for gp, g in enumerate(g5):
            t0, n = GROUPS[g]
            which = 0 if g < 8 else 1
            up = gp % 2
            hgm, yg = hgms[up], ygs[up]
            if gp + 1 < len(g5):
                p5_load(gp + 1)
            for jd in range(8):
                for b in range(3):
                    for k in range(8):
                        P.op("tensor", lambda e, k=k, b=b, jd=jd, n=n, hgm=hgm: e.matmul(ps[b][:, 0:n], Wg[:, k, b * 1024 + jd * 128:b * 1024 + (jd + 1) * 128], hgm[:, k, 0:n], start=(k == 0), stop=(k == 7)),
                             reads=["hgm%d" % up, ("Wg", (b * 1024 + jd * 128) // 512)], writes=[PS[b]])
                    for k in range(4):
                        P.op("tensor", lambda e, k=k, b=b, jd=jd, n=n, yg=yg: e.matmul(ps[3 + b][:, 0:n], Wb[:, b * 4 + k, jd * 128:(jd + 1) * 128], yg[:, b, k, 0:n], start=(k == 0), stop=(k == 3)),
                             reads=[("yg", b, up), ("Wb", b, jd // 4)], writes=[PS[3 + b]])
                    P.op("scalar", lambda e, b=b, n=n: e.activation(sgm[b][:, 0:n], ps[b][:, 0:n], AF.Sigmoid), reads=[PS[b]], writes=["sgm%d" % b])
                    P.op("vector", lambda e, b=b, n=n: e.tensor_tensor(tm[b][:, 0:n], ps[3 + b][:, 0:n], sgm[b][:, 0:n], ALU.mult), reads=[PS[3 + b], "sgm%d" % b], writes=["tm%d" % b])
                P.op("gpsimd", lambda e, n=n: e.tensor_tensor(tm[0][:, 0:n], tm[0][:, 0:n], tm[1][:, 0:n], ALU.add), reads=["tm0", "tm1"], writes=["tm0"])
                P.op("gpsimd", lambda e, jd=jd, n=n: e.tensor_tensor(mixT[:, jd, 0:n], tm[0][:, 0:n], tm[2][:, 0:n], ALU.add), reads=["tm0", "tm2"], writes=[("mixT", jd)])
            for j in range(n // 128):
                s = xi_ % 2
                xi_ += 1
                r0 = t0 + j * 128
                P.dma("sync", xtm[s][:], x_src[r0:r0 + 128, :], writes=["xtm%d" % s])
                for half in range(2):
                    for k in range(8):
                        P.op("tensor", lambda e, k=k, j=j, half=half: e.matmul(ps[6 + half][:], mixT[:, k, j * 128:(j + 1) * 128], Wo[:, k, half * 512:(half + 1) * 512], start=(k == 0), stop=(k == 7)),
                             reads=[("mixT", k), ("Wo", half)], writes=[PS[6 + half]])
                    g1 = md[2][which]
                    P.op("vector", lambda e, half=half, g1=g1: e.tensor_tensor(tmo[:], ps[6 + half][:], g1[0][:, half * 512:(half + 1) * 512], ALU.mult), reads=[PS[6 + half], g1[1]], writes=["tmo"])
                    P.op("gpsimd", lambda e, half=half, s=s: e.tensor_tensor(xom[s][:, half * 512:(half + 1) * 512], tmo[:], xtm[s][:, half * 512:(half + 1) * 512], ALU.add), reads=["tmo", "xtm%d" % s], writes=[("xom", s, half)])
                P.dma("gpsimd", x_mix[r0:r0 + 128, :], xom[s][:], reads=[("xom", s, 0), ("xom", s, 1)], writes=[("xmix", r0)])
        P.barrier()
        if done("p5_%d" % L):
            return True

        P.mute = "p6" in skip
        A.reset(A_PERSIST)
        if moe:
            NTL = 24
            NR = NTL * 512
            I32 = mybir.dt.int32
            md = load_mod(L, [3, 4, 5])
            A2m, B2m, G2m = md[4][0], md[3][0], md[5][0]
            idx_all = A.alloc("idx_all", [128, 64], I32)
            sw_all = A.alloc("sw_all", [128, 64], F32)
            widx = A.alloc("widx", [128, NTL], I32)
            m_meta = A.mark()
            abf_all = A.alloc("abf_all", [128, 32, D], BF16)
            mk_all = A.alloc("mk_all", [128, 2, 32, NE], F32)
            pos_all = A.alloc("pos_all", [128, 32, NE], F32)
            rwf = A.alloc("rwf", [128, 8, NE], F32)
            aTfs = [A.alloc("aTf%d" % i, [128, 8, 128], F32) for i in range(2)]
            rbb = A.alloc("rbb", [128, NE], F32)
            trif = A.alloc("trif", [128, 128], F32)
            tri = A.alloc("tri", [128, 128], BF16)
            iot = A.alloc("iot", [128, NTL + 1], F32)
            base = A.alloc("base", [128, NE], F32)
            zt = A.alloc("zt", [128, 8192], BF16)
            xtr = [A.alloc("xtr%d" % i, [128, D], F32) for i in range(2)]
            afs = [A.alloc("af%d" % i, [128, D], F32) for i in range(2)]
            prs = [[A.alloc("pr%d_%d" % (i, q), [128, D], F32) for i in range(2)] for q in range(2)]
            jks = [A.alloc("jk%d" % i, [128, D], BF16) for i in range(2)]
            lgts = [A.alloc("lgt%d" % i, [128, NE], F32) for i in range(2)]
            rss = [A.alloc("rs_%d" % i, [128, 96], F32) for i in range(2)]
            mbfs = [A.alloc("mbf%d" % i, [128, NE], BF16) for i in range(2)]
            rs_ = rss[0]
            P.dma("sync", rwf[:], moe_router_p, writes=["rwf"])
            P.dma("sync", rbb[:], moe_router_b[0].partition_broadcast(128), writes=["rbb"])
            P.dma("sync", trif[:], k_tri, writes=["trif"])
            P.dma("sync", iot[:], k_iot, writes=["iot"])
            P.op("vector", lambda e: e.tensor_copy(tri[:], trif[:]), reads=["trif"], writes=["tri"])
            P.op("vector", lambda e: e.memset(base[:], 0.0), writes=["base"])
            P.op("gpsimd", lambda e: e.memset(zt[:], 0.0), writes=["zt"])
            Gv = G_d.rearrange("(p r) d -> p (r d)", p=128)
            NZ = NR * D // 128 // 8192
            for z in range(NZ):
                P.dma("scalar", Gv[:, z * 8192:(z + 1) * 8192], zt[:], reads=["zt"], writes=[("Gz", z)])
            GZ = [("Gz", z) for z in range(NZ)]
            prev_rt = [None]
            for ti in range(SEQ // 128):
                def route_tile(ti=ti):
                    u = ti % 2
                    sfx = "_%d" % u
                    af, jk, lgt, rs_, mbf = afs[u], jks[u], lgts[u], rss[u], mbfs[u]
                    pr = prs[u]
                    bkA, bkB, bkT = (0, 1, 2) if u == 0 else (4, 5, 6)
                    aTf = aTfs[u]
                    s = ti % 2
                    r0 = ti * 128
                    xt_ = xtr[s]
                    P.dma("sync", xt_[:], x_mix[r0:r0 + 128, :], writes=["xtr%d" % s])
                    P.op("scalar", lambda e, xt_=xt_: e.activation(jk[:], xt_[:], AF.Square, accum_out=rs_[:, 0:1]), reads=["xtr%d" % s], writes=["jk" + sfx, "rs0" + sfx])
                    P.op("scalar", lambda e: e.activation(rs_[:, 1:2], rs_[:, 0:1], AF.Sqrt, bias=NORM_EPS, scale=1.0 / D), reads=["rs0" + sfx], writes=["rs1" + sfx])
                    P.op("vector", lambda e: e.reciprocal(rs_[:, 2:3], rs_[:, 1:2]), reads=["rs1" + sfx], writes=["rs2" + sfx])
                    P.op("vector", lambda e, xt_=xt_: e.scalar_tensor_tensor(af[:], xt_[:], rs_[:, 2:3], A2m[0][:], ALU.mult, ALU.mult), reads=["xtr%d" % s, "rs2" + sfx, A2m[1]], writes=["af" + sfx])
                    P.op("gpsimd", lambda e: e.tensor_tensor(af[:], af[:], B2m[0][:], ALU.add), reads=["af" + sfx, B2m[1]], writes=["af" + sfx])
                    P.op("scalar", lambda e, ti=ti: e.copy(abf_all[:, ti, :], af[:]), reads=["af" + sfx], writes=[("abf", ti)])
                    for k in range(8):
                        P.op("tensor", lambda e, k=k: e.transpose(ps[bkT + k // 4][:, (k % 4) * 128:(k % 4 + 1) * 128], af[:, k * 128:(k + 1) * 128], identf[:]),
                             reads=["af" + sfx, "identf"], writes=[PS[bkT + k // 4]])
                    for hh in range(2):
                        P.op("scalar", lambda e, hh=hh: e.copy(aTf[:, hh * 4:(hh + 1) * 4, :], ps[bkT + hh][:].rearrange("p (k t) -> p k t", k=4)), reads=[PS[bkT + hh]], writes=[("aTf", hh, u)])
                    for k in range(8):
                        P.op("tensor", lambda e, k=k: e.matmul(ps[bkA][:, 16:24], aTf[:, k, :], rwf[:, k, :], start=(k == 0), stop=(k == 7)),
                             reads=[("aTf", 0, u), ("aTf", 1, u), "rwf"], writes=[PS[bkA]])
                    P.op("vector", lambda e: e.tensor_tensor(lgt[:], ps[bkA][:, 16:24], rbb[:], ALU.add), reads=[PS[bkA], "rbb"], writes=[("lgt", ex, u) for ex in range(NE)])
                    yield
                    LT = [("lgt", ex, u) for ex in range(NE)]
                    mk1 = mk_all[:, 0, ti, :]
                    mk2 = mk_all[:, 1, ti, :]
                    P.op("vector", lambda e: e.reduce_max(rs_[:, 8:9], lgt[:], axis=AX.X), reads=LT, writes=["m1" + sfx])
                    P.op("vector", lambda e, mk1=mk1: e.tensor_scalar(mk1, lgt[:], rs_[:, 8:9], None, ALU.is_ge), reads=LT + ["m1" + sfx], writes=[("mk1", ti)])
                    P.op("vector", lambda e, mk1=mk1: e.scalar_tensor_tensor(rs_[:, 24:32], mk1, -1e30, lgt[:], ALU.mult, ALU.add), reads=LT + [("mk1", ti)], writes=["l2" + sfx])
                    P.op("vector", lambda e: e.reduce_max(rs_[:, 9:10], rs_[:, 24:32], axis=AX.X), reads=["l2" + sfx], writes=["m2" + sfx])
                    P.op("vector", lambda e, mk2=mk2: e.tensor_scalar(mk2, rs_[:, 24:32], rs_[:, 9:10], None, ALU.is_ge), reads=["l2" + sfx, "m2" + sfx], writes=[("mk2", ti)])
                    P.op("vector", lambda e: e.tensor_tensor(rs_[:, 10:11], rs_[:, 9:10], rs_[:, 8:9], ALU.subtract), reads=["m1" + sfx, "m2" + sfx], writes=["dl" + sfx])
                    P.op("scalar", lambda e, ti=ti: e.activation(sw_all[:, 32 + ti:33 + ti], rs_[:, 10:11], AF.Sigmoid), reads=["dl" + sfx], writes=[("sw2", ti)])
                    P.op("vector", lambda e, ti=ti: e.tensor_scalar(sw_all[:, ti:ti + 1], sw_all[:, 32 + ti:33 + ti], -1.0, 1.0, ALU.mult, ALU.add), reads=[("sw2", ti)], writes=[("sw1", ti)])
                    P.op("vector", lambda e, mk1=mk1, mk2=mk2: e.tensor_tensor(rs_[:, 40:48], mk1, mk2, ALU.add), reads=[("mk1", ti), ("mk2", ti)], writes=["mall" + sfx])
                    P.op("vector", lambda e: e.tensor_copy(mbf[:], rs_[:, 40:48]), reads=["mall" + sfx], writes=["mbf" + sfx])
                    P.op("tensor", lambda e: e.matmul(ps[bkA][:, 0:NE], tri[:], mbf[:], start=True, stop=True), reads=["tri", "mbf" + sfx], writes=[PS[bkA]])
                    P.op("tensor", lambda e: e.matmul(ps[bkB][:, 0:NE], ones_bf[:], mbf[:], start=True, stop=True), reads=["mbf" + sfx], writes=[PS[bkB]])
                    P.op("vector", lambda e, ti=ti: e.tensor_tensor(pos_all[:, ti, :], ps[bkA][:, 0:NE], base[:], ALU.add), reads=[PS[bkA], "base"], writes=[("pos", ti)])
                    P.op("vector", lambda e: e.tensor_tensor(base[:], ps[bkB][:, 0:NE], base[:], ALU.add), reads=[PS[bkB], "base"], writes=["base"])
                gen_ = route_tile()
                next(gen_)
                if prev_rt[0] is not None:
                    for _ in prev_rt[0]:
                        pass
                prev_rt[0] = gen_
            for _ in prev_rt[0]:
                pass
            MKS = [("mk1", ti) for ti in range(32)] + [("mk2", ti) for ti in range(32)]
            POS = [("pos", ti) for ti in range(32)]
            nt_ = rs_[:, 16:24]
            stt = rs_[:, 48:56]
            P.op("vector", lambda e: e.tensor_scalar(nt_, base[:], 0.0, None, ALU.is_gt), reads=["base"], writes=["nt"])
            for m in range(1, 8):
                P.op("vector", lambda e, m=m: e.scalar_tensor_tensor(nt_, base[:], 512.0 * m, nt_, ALU.is_gt, ALU.add), reads=["base", "nt"], writes=["nt"])
            P.op("vector", lambda e: e.memset(stt, 0.0), writes=["stt"])
            for ex in range(1, NE):
                P.op("vector", lambda e, ex=ex: e.tensor_tensor(rs_[:, 48 + ex:49 + ex], rs_[:, 47 + ex:48 + ex], rs_[:, 15 + ex:16 + ex], ALU.add), reads=["stt", "nt"], writes=["stt"])
            P.op("vector", lambda e: e.tensor_tensor(rs_[:, 56:64], stt, nt_, ALU.add), reads=["stt", "nt"], writes=["endt"])
            P.op("vector", lambda e: e.tensor_scalar(rs_[:, 64:72], stt, 512.0, None, ALU.mult), reads=["stt"], writes=["rowb"])
            eidf = A.alloc("eidf", [128, NTL], F32)
            P.op("vector", lambda e: e.tensor_scalar(eidf[:], iot[:, 0:NTL], rs_[:, 56:57], None, ALU.is_ge), reads=["iot", "endt"], writes=["eidf"])
            for ex in range(1, NE - 1):
                P.op("vector", lambda e, ex=ex: e.scalar_tensor_tensor(eidf[:], iot[:, 0:NTL], rs_[:, 56 + ex:57 + ex], eidf[:], ALU.is_ge, ALU.add), reads=["iot", "endt", "eidf"], writes=["eidf"])
            P.op("vector", lambda e: e.tensor_scalar(eidf[:], eidf[:], 128.0, iot[:, NTL:NTL + 1], ALU.mult, ALU.add), reads=["eidf", "iot"], writes=["eidf"])
            P.op("vector", lambda e: e.tensor_copy(widx[:], eidf[:]), reads=["eidf"], writes=["widx"])
            rowi = A.alloc("rowi", [128, 32, NE], F32)
            t8 = A.alloc("t8", [128, 32, NE], F32)
            d32 = A.alloc("d32", [128, 64], F32)
            P.op("vector", lambda e: e.tensor_tensor(rowi[:], pos_all[:], rs_[:, 64:72].unsqueeze(1).to_broadcast([128, 32, NE]), ALU.add), reads=POS + ["rowb"], writes=["rowi"])
            for sl_ in range(2):
                P.op("vector", lambda e, sl_=sl_: e.tensor_tensor(t8[:], mk_all[:, sl_], rowi[:], ALU.mult), reads=MKS + ["rowi"], writes=["t8"])
                P.op("vector", lambda e, sl_=sl_: e.reduce_sum(d32[:, sl_ * 32:(sl_ + 1) * 32], t8[:], axis=AX.X), reads=["t8"], writes=[("d32", sl_)])
            P.op("vector", lambda e: e.tensor_copy(idx_all[:], d32[:]), reads=[("d32", 0), ("d32", 1)], writes=["idx_all"])
            for ti in range(32):
                for sl_ in range(2):
                    col = sl_ * 32 + ti
                    P.op("gpsimd", lambda e, col=col, ti=ti: e.indirect_dma_start(out=G_d[:, :], out_offset=bass.IndirectOffsetOnAxis(ap=idx_all[:, col:col + 1], axis=0),
                                                                               in_=abf_all[:, ti, :], in_offset=None),
                         reads=["idx_all", ("abf", ti)] + GZ, writes=[("Gs", col)], dma=True)
            P.barrier()
            if done("p6a_%d" % L):
                return True
            A.reset(m_meta)
            gt = [A.alloc("gt%d" % i, [128, 1024], BF16) for i in range(8)]
            aT = A.alloc("aTm", [128, 8, 512], BF16)
            hid = A.alloc("hidm", [128, 28, 512], BF16)
            wA = [A.alloc("wAm%d" % i, [128, 8, 512], BF16) for i in range(4)]
            wB = [A.alloc("wBm%d" % i, [128, 4, 1024], BF16) for i in range(3)]
            slm = [A.alloc("slm%d" % i, [128, 512], F32) for i in range(2)]
            yo = [A.alloc("yom%d" % i, [128, D], F32) for i in range(2)]
            wa_i = 0
            wb_i = 0
            gi_ = 0
            yo_i = 0
            pk = 0
            def g_load(i_):
                for j_ in range(4):
                    q_ = (i_ * 4 + j_) % 8
                    rr_ = i_ * 512 + j_ * 128
                    P.dma("sync", gt[q_][:], G_d[rr_:rr_ + 128, :], writes=["gt%d" % q_])

            g_load(0)
            for i in range(NTL):
                ixc = widx[:, i:i + 1]
                if i + 1 < NTL:
                    g_load(i + 1)
                for j in range(4):
                    g_ = gt[(i * 4 + j) % 8]
                    gtok = "gt%d" % ((i * 4 + j) % 8)
                    bank = 6 + (j % 2)
                    pbm = ps[bank][:].bitcast(BF16)
                    for k in range(8):
                        P.op("tensor", lambda e, k=k, g_=g_, pbm=pbm: e.transpose(pbm[:, k * 128:(k + 1) * 128], g_[:, k * 128:(k + 1) * 128], ident[:]), reads=[gtok, "ident"], writes=[PS[bank]])
                    P.op("scalar", lambda e, j=j, pbm=pbm: e.copy(aT[:, :, j * 128:(j + 1) * 128], pbm[:, 0:1024].rearrange("p (k t) -> p k t", k=8)), reads=[PS[bank]], writes=[("aTm", j)])
                AT = [("aTm", j) for j in range(4)]
                for fb in range(7):
                    sl_w = []
                    for wn, Wq in enumerate((w1q, w3q)):
                        s = wa_i % 4
                        wa_i += 1
                        for h in range(2):
                            P.op("gpsimd", lambda e, s=s, h=h, fb=fb, Wq=Wq, ixc=ixc: e.indirect_dma_start(out=wA[s][:, h * 4:(h + 1) * 4, :].rearrange("p k f -> p (k f)"), out_offset=None,
                                                                                                   in_=Wq[fb][h][:, :], in_offset=bass.IndirectOffsetOnAxis(ap=ixc, axis=0)),
                                 reads=[], writes=[("wAm", s, h)], dma=True)
                        sl_w.append(s)
                    for fc in range(4):
                        b1, b3 = (pk % 2) * 2, (pk % 2) * 2 + 1
                        sls = slm[pk % 2]
                        stok = "slm%d" % (pk % 2)
                        pk += 1
                        for (bank, s) in ((b1, sl_w[0]), (b3, sl_w[1])):
                            for k in range(8):
                                P.op("tensor", lambda e, k=k, bank=bank, s=s, fc=fc: e.matmul(ps[bank][:], wA[s][:, k, fc * 128:(fc + 1) * 128], aT[:, k, :], start=(k == 0), stop=(k == 7)),
                                     reads=AT + [("wAm", s, 0), ("wAm", s, 1)], writes=[PS[bank]])
                        P.op("scalar", lambda e, b1=b1, sls=sls: e.activation(sls[:], ps[b1][:], AF.Silu), reads=[PS[b1]], writes=[stok])
                        P.op("vector", lambda e, b3=b3, sls=sls, fb=fb, fc=fc: e.tensor_tensor(hid[:, fb * 4 + fc, :], ps[b3][:], sls[:], ALU.mult),
                             reads=[PS[b3], stok], writes=[("hidm", fb * 4 + fc)])
                for fb in range(7):
                    s = wb_i % 3
                    wb_i += 1
                    for h in range(2):
                        P.op("gpsimd", lambda e, s=s, h=h, fb=fb, ixc=ixc: e.indirect_dma_start(out=wB[s][:, h * 2:(h + 1) * 2, :].rearrange("p k f -> p (k f)"), out_offset=None,
                                                                                         in_=w2q[fb][h][:, :], in_offset=bass.IndirectOffsetOnAxis(ap=ixc, axis=0)),
                             reads=[], writes=[("wBm", s, h)], dma=True)
                    for j in range(4):
                        for half in range(2):
                            bank = j * 2 + half
                            for k in range(4):
                                P.op("tensor", lambda e, k=k, j=j, half=half, bank=bank, s=s, fb=fb: e.matmul(ps[bank][:], hid[:, fb * 4 + k, j * 128:(j + 1) * 128], wB[s][:, k, half * 512:(half + 1) * 512],
                                                                                               start=(fb == 0 and k == 0), stop=(fb == 6 and k == 3)),
                                     reads=[("hidm", fb * 4 + k), ("wBm", s, k // 2)], writes=[PS[bank]])
                for j in range(4):
                    y_ = yo[yo_i % 2]
                    ytok = "yom%d" % (yo_i % 2)
                    yo_i += 1
                    for half in range(2):
                        bank = j * 2 + half
                        if half == 0:
                            P.op("scalar", lambda e, y_=y_, bank=bank: e.copy(y_[:, 0:512], ps[bank][:]), reads=[PS[bank]], writes=[(ytok, 0)])
                        else:
                            P.op("vector", lambda e, y_=y_, bank=bank: e.tensor_copy(y_[:, 512:1024], ps[bank][:]), reads=[PS[bank]], writes=[(ytok, 1)])
                    rr = i * 512 + j * 128
                    P.dma("sync", Y_d[rr:rr + 128, :], y_[:], reads=[(ytok, 0), (ytok, 1)], writes=[("Y", rr)])
            P.barrier()
            if done("p6b_%d" % L):
                return True
            A.reset(m_meta)
            y1 = [A.alloc("y1_%d" % i, [128, D], F32) for i in range(2)]
            y2 = [A.alloc("y2_%d" % i, [128, D], F32) for i in range(2)]
            xtc = [A.alloc("xtc%d" % i, [128, D], F32) for i in range(2)]
            cmb = [A.alloc("cmb%d" % i, [128, D], F32) for i in range(2)]
            xoz = [A.alloc("xozm%d" % i, [128, D], F32) for i in range(2)]
            jzz = [A.alloc("jzz%d" % i, [128, D], BF16) for i in range(2)]
            szz = [A.alloc("szz%d" % i, [128, 4], F32) for i in range(2)]
            fngm = A.alloc("fngm", [128, D], F32)
            P.dma("sync", fngm[:], final_norm.partition_broadcast(128), writes=["fngm"])
            prev_c = [None]
            for ti in range(SEQ // 128):
                def comb_tile(ti=ti):
                    s = ti % 2
                    r0 = ti * 128
                    for (yt_, nm, col) in ((y1[s], "y1_%d" % s, ti), (y2[s], "y2_%d" % s, 32 + ti)):
                        P.op("gpsimd", lambda e, yt_=yt_, col=col: e.indirect_dma_start(out=yt_[:, :], out_offset=None, in_=Y_d[:, :],
                                                                                     in_offset=bass.IndirectOffsetOnAxis(ap=idx_all[:, col:col + 1], axis=0)),
                             reads=[], writes=[nm], dma=True)
                    P.dma("sync", xtc[s][:], x_mix[r0:r0 + 128, :], writes=["xtc%d" % s])
                    cm = cmb[s]
                    P.op("vector", lambda e, cm=cm, s=s, ti=ti: e.tensor_scalar(cm[:], y1[s][:], sw_all[:, ti:ti + 1], None, ALU.mult), reads=["y1_%d" % s], writes=["cmb%d" % s])
                    P.op("vector", lambda e, cm=cm, s=s, ti=ti: e.scalar_tensor_tensor(cm[:], y2[s][:], sw_all[:, 32 + ti:33 + ti], cm[:], ALU.mult, ALU.add), reads=["y2_%d" % s, "cmb%d" % s], writes=["cmb%d" % s])
                    P.op("gpsimd", lambda e, cm=cm: e.tensor_tensor(cm[:], cm[:], G2m[0][:], ALU.mult), reads=["cmb%d" % s, G2m[1]], writes=["cmb%d" % s])
                    P.op("gpsimd", lambda e, cm=cm, s=s: e.tensor_tensor(cm[:], cm[:], xtc[s][:], ALU.add), reads=["cmb%d" % s, "xtc%d" % s], writes=["cmb%d" % s])
                    yield
                    szs = szz[s]
                    P.op("scalar", lambda e, cm=cm, s=s, szs=szs: e.activation(jzz[s][:], cm[:], AF.Square, accum_out=szs[:, 0:1]), reads=["cmb%d" % s], writes=["jzz%d" % s, "szz0_%d" % s])
                    P.op("scalar", lambda e, szs=szs: e.activation(szs[:, 1:2], szs[:, 0:1], AF.Sqrt, bias=NORM_EPS, scale=1.0 / D), reads=["szz0_%d" % s], writes=["szz1_%d" % s])
                    P.op("vector", lambda e, szs=szs: e.reciprocal(szs[:, 2:3], szs[:, 1:2]), reads=["szz1_%d" % s], writes=["szz2_%d" % s])
                    P.op("vector", lambda e, cm=cm, s=s, szs=szs: e.scalar_tensor_tensor(xoz[s][:], cm[:], szs[:, 2:3], fngm[:], ALU.mult, ALU.mult), reads=["cmb%d" % s, "szz2_%d" % s, "fngm"], writes=["xozm%d" % s])
                    P.dma("sync", out[r0:r0 + 128, :], xoz[s][:], reads=["xozm%d" % s], writes=[("out", r0)])
                gen_ = comb_tile()
                next(gen_)
                if prev_c[0] is not None:
                    for _ in prev_c[0]:
                        pass
                prev_c[0] = gen_
            for _ in prev_c[0]:
                pass
            P.barrier()
            return False
        md = load_mod(L, [3, 4, 5])
        normFA, normFB, normF = make_normT("p6", md[4], md[3], 7)
        aT = A.alloc("aT", [128, 8, 512], BF16)
        hid = A.alloc("hid", [128, 28, 512], BF16)
        wA = [A.alloc("wA%d" % i, [128, 8, 512], BF16) for i in range(4)]
        W2res = A.alloc("W2res", [128, 28, 1024], BF16)
        for fb in range(7):
            for cbk in range(2):
                P.dma("gpsimd", W2res[:, fb * 4:(fb + 1) * 4, cbk * 512:(cbk + 1) * 512], ffn_w2[0][fb * 512:(fb + 1) * 512, cbk * 512:(cbk + 1) * 512].rearrange("(k p) f -> p k f", p=128), writes=[("W2res", fb, cbk)])
        sl = [A.alloc("sl%d" % i, [128, 512], F32) for i in range(2)]
        xtf = [A.alloc("xtf%d" % i, [128, D], F32) for i in range(2)]
        xof = [A.alloc("xof%d" % i, [128, D], F32) for i in range(2)]
        tmf = A.alloc("tmf", [128, 512], F32)
        if moe:
            acc = A.alloc("acc", [128, 4, D], F32)
            rwb = A.alloc("rwb", [128, NE, D], F32)
            rbb = A.alloc("rbb", [128, NE], F32)
            af = A.alloc("af", [128, D], F32)
            pr = A.alloc("pr", [128, D], F32)
            lgt = A.alloc("lgt", [128, 4, NE], F32)
            wgt = A.alloc("wgt", [128, 4, NE], F32)
            rs_ = A.alloc("rs_", [128, 40], F32)
            for ex in range(NE):
                P.dma("sync", rwb[:, ex, :], moe_router_t[ex].partition_broadcast(128), writes=[("rwb", ex)])
            P.dma("sync", rbb[:], moe_router_b[0].partition_broadcast(128), writes=["rbb"])
        wa_i = [0]
        wb_i = [0]
        xf_i = 0
        glist = [g for g in range(9) if not (g == 8 and (moe or not need_ctx))]
        pend = [None]
        for gpos, g in enumerate(glist):
            t0, n = GROUPS[g]
            which = 0 if g < 8 else 1
            nt_ = n // 128
            if pend[0] is None:
                normFB(normFA(x_mix, t0, nt_, which), aT, "aT")
            else:
                normFB(pend[0], aT, "aT")
                pend[0] = None
            if moe:
                for j in range(nt_):
                    r0 = t0 + j * 128
                    P.dma("sync", xtf[0][:], x_mix[r0:r0 + 128, :], writes=["xtf0"])
                    P.op("scalar", lambda e: e.activation(pr[:], xtf[0][:], AF.Square, accum_out=rs_[:, 0:1]), reads=["xtf0"], writes=["pr", "rs0"])
                    P.op("scalar", lambda e: e.activation(rs_[:, 1:2], rs_[:, 0:1], AF.Sqrt, bias=NORM_EPS, scale=1.0 / D), reads=["rs0"], writes=["rs1"])
                    P.op("vector", lambda e: e.reciprocal(rs_[:, 2:3], rs_[:, 1:2]), reads=["rs1"], writes=["rs2"])
                    am, bm = md[4][which], md[3][which]
                    P.op("vector", lambda e, am=am: e.scalar_tensor_tensor(af[:], xtf[0][:], rs_[:, 2:3], am[0][:], ALU.mult, ALU.mult), reads=["xtf0", "rs2", am[1]], writes=["af"])
                    P.op("gpsimd", lambda e, bm=bm: e.tensor_tensor(af[:], af[:], bm[0][:], ALU.add), reads=["af", bm[1]], writes=["af"])
                    for ex in range(NE):
                        eng = "vector" if ex % 2 == 0 else "gpsimd"
                        P.op(eng, lambda e, ex=ex: e.tensor_tensor(pr[:], af[:], rwb[:, ex, :], ALU.mult), reads=["af", ("rwb", ex)], writes=["pr"])
                        P.op("vector", lambda e, ex=ex, j=j: e.reduce_sum(lgt[:, j, ex:ex + 1], pr[:], axis=AX.X), reads=["pr"], writes=[("lgt", j, ex)])
                    LT = [("lgt", j, ex) for ex in range(NE)]
                    lj = lgt[:, j, :]
                    P.op("vector", lambda e, lj=lj: e.tensor_tensor(lj, lj, rbb[:], ALU.add), reads=LT + ["rbb"], writes=LT)
                    P.op("vector", lambda e, lj=lj: e.reduce_max(rs_[:, 8:9], lj, axis=AX.X), reads=LT, writes=["m1"])
                    P.op("vector", lambda e, lj=lj: e.tensor_scalar(rs_[:, 16:24], lj, rs_[:, 8:9], None, ALU.is_ge), reads=LT + ["m1"], writes=["mk1"])
                    P.op("vector", lambda e, lj=lj: e.scalar_tensor_tensor(rs_[:, 24:32], rs_[:, 16:24], -1e30, lj, ALU.mult, ALU.add), reads=LT + ["mk1"], writes=["l2"])
                    P.op("vector", lambda e: e.reduce_max(rs_[:, 9:10], rs_[:, 24:32], axis=AX.X), reads=["l2"], writes=["m2"])
                    P.op("vector", lambda e: e.tensor_scalar(rs_[:, 32:40], rs_[:, 24:32], rs_[:, 9:10], None, ALU.is_ge), reads=["l2", "m2"], writes=["mk2"])
                    P.op("vector", lambda e: e.tensor_tensor(rs_[:, 10:11], rs_[:, 9:10], rs_[:, 8:9], ALU.subtract), reads=["m1", "m2"], writes=["dl"])
                    P.op("scalar", lambda e: e.activation(rs_[:, 11:12], rs_[:, 10:11], AF.Sigmoid), reads=["dl"], writes=["s2"])
                    P.op("vector", lambda e: e.tensor_scalar(rs_[:, 12:13], rs_[:, 11:12], -1.0, 1.0, ALU.mult, ALU.add), reads=["s2"], writes=["s1"])
                    P.op("vector", lambda e, j=j: e.tensor_scalar(wgt[:, j, :], rs_[:, 16:24], rs_[:, 12:13], None, ALU.mult), reads=["mk1", "s1"], writes=[("wgt", j)])
                    P.op("vector", lambda e, j=j: e.scalar_tensor_tensor(wgt[:, j, :], rs_[:, 32:40], rs_[:, 11:12], wgt[:, j, :], ALU.mult, ALU.add), reads=["mk2", "s2", ("wgt", j)], writes=[("wgt", j)])
            nexp = NE if moe else 1
            for ex in range(nexp):
                if moe:
                    W1, W3, W2 = moe_w1[0, ex], moe_w3[0, ex], moe_w2[0, ex]
                else:
                    W1, W3, W2 = ffn_w1[0], ffn_w3[0], ffn_w2[0]
                for fb in range(7):
                    sl_w = []
                    for Wsrc in (W1, W3):
                        s = wa_i[0] % 4
                        wa_i[0] += 1
                        P.dma("gpsimd", wA[s][:], Wsrc[:, fb * 512:(fb + 1) * 512].rearrange("(k p) f -> p k f", p=128), writes=["wA%d" % s])
                        sl_w.append(s)
                    for fc in range(4):
                        b1, b3 = (fc % 2) * 2, (fc % 2) * 2 + 1
                        for (bank, s) in ((b1, sl_w[0]), (b3, sl_w[1])):
                            for k in range(8):
                                P.op("tensor", lambda e, k=k, bank=bank, s=s, fc=fc, n=n: e.matmul(ps[bank][:, 0:n], wA[s][:, k, fc * 128:(fc + 1) * 128], aT[:, k, 0:n], start=(k == 0), stop=(k == 7)),
                                     reads=["aT", "wA%d" % s], writes=[PS[bank]])
                        sls = sl[fc % 2]
                        P.op("scalar", lambda e, b1=b1, sls=sls, n=n: e.activation(sls[:, 0:n], ps[b1][:, 0:n], AF.Silu), reads=[PS[b1]], writes=["sl%d" % (fc % 2)])
                        P.op("vector", lambda e, b3=b3, sls=sls, fb=fb, fc=fc, n=n: e.tensor_tensor(hid[:, fb * 4 + fc, 0:n], ps[b3][:, 0:n], sls[:, 0:n], ALU.mult),
                             reads=[PS[b3], "sl%d" % (fc % 2)], writes=[("hid", fb * 4 + fc)])
                if (not moe) and gpos + 1 < len(glist):
                    g2 = glist[gpos + 1]
                    pend[0] = normFA(x_mix, GROUPS[g2][0], GROUPS[g2][1] // 128, 0 if g2 < 8 else 1)
                for fb in range(7):
                    for j in range(nt_):
                        for half in range(2):
                            bank = j * 2 + half
                            for k in range(4):
                                P.op("tensor", lambda e, k=k, j=j, half=half, bank=bank, fb=fb: e.matmul(ps[bank][:], hid[:, fb * 4 + k, j * 128:(j + 1) * 128], W2res[:, fb * 4 + k, half * 512:(half + 1) * 512],
                                                                                          start=(fb == 0 and k == 0), stop=(fb == 6 and k == 3)),
                                     reads=[("hid", fb * 4 + k), ("W2res", fb, half)], writes=[PS[bank]])
                for j in range(nt_):
                    r0 = t0 + j * 128
                    if not moe or ex == nexp - 1:
                        sx = xf_i % 2
                        xf_i += 1
                        P.dma("sync", xtf[sx][:], x_mix[r0:r0 + 128, :], writes=["xtf%d" % sx])
                    for half in range(2):
                        bank = j * 2 + half
                        hs = slice(half * 512, (half + 1) * 512)
                        g2 = md[5][which]
                        if moe:
                            wc = wgt[:, j, ex:ex + 1]
                            if ex == 0:
                                P.op("vector", lambda e, j=j, hs=hs, bank=bank, wc=wc: e.tensor_scalar(acc[:, j, hs], ps[bank][:], wc, None, ALU.mult), reads=[PS[bank], ("wgt", j)], writes=[("acc", j, half)])
                            else:
                                P.op("vector", lambda e, j=j, hs=hs, bank=bank, wc=wc: e.scalar_tensor_tensor(acc[:, j, hs], ps[bank][:], wc, acc[:, j, hs], ALU.mult, ALU.add), reads=[PS[bank], ("wgt", j), ("acc", j, half)], writes=[("acc", j, half)])
                            if ex == nexp - 1:
                                P.op("gpsimd", lambda e, j=j, hs=hs, g2=g2: e.tensor_tensor(tmf[:], acc[:, j, hs], g2[0][:, hs], ALU.mult), reads=[("acc", j, half), g2[1]], writes=["tmf"])
                                P.op("gpsimd", lambda e, hs=hs, sx=sx: e.tensor_tensor(xof[sx][:, hs], tmf[:], xtf[sx][:, hs], ALU.add), reads=["tmf", "xtf%d" % sx], writes=[("xof", sx, half)])
                        else:
                            P.op("vector", lambda e, hs=hs, bank=bank, g2=g2: e.tensor_tensor(tmf[:], ps[bank][:], g2[0][:, hs], ALU.mult), reads=[PS[bank], g2[1]], writes=["tmf"])
                            P.op("gpsimd", lambda e, hs=hs, sx=sx: e.tensor_tensor(xof[sx][:, hs], tmf[:], xtf[sx][:, hs], ALU.add), reads=["tmf", "xtf%d" % sx], writes=[("xof", sx, half)])
                    if not moe or ex == nexp - 1:
                        P.dma("gpsimd", x_out[r0:r0 + 128, :], xof[sx][:], reads=[("xof", sx, 0), ("xof", sx, 1)], writes=[("xout", r0)])
        P.barrier()
        if done("p6_%d" % L):
            return True
        return False

    if layer(0, xin, xa_d, xb_d, True, False):
        return finish()
    if layer(1, xb_d, xc_d, xd_d, False, True):
        return finish()
    return finish()
    A.reset(A_PERSIST)
    fng = A.alloc("fng", [128, D], F32)
    P.dma("sync", fng[:], final_norm.partition_broadcast(128), writes=["fng"])
    xtz = [A.alloc("xtz%d" % i, [128, D], F32) for i in range(2)]
    xoz = [A.alloc("xoz%d" % i, [128, D], F32) for i in range(2)]
    jz = A.alloc("jz", [128, D], BF16)
    sz = A.alloc("sz", [128, 4], F32)
    for i in range(SEQ // 128):
        s = i % 2
        P.dma("sync", xtz[s][:], xd_d[i * 128:(i + 1) * 128, :], writes=["xtz%d" % s])
        P.op("scalar", lambda e, s=s: e.activation(jz[:], xtz[s][:], AF.Square, accum_out=sz[:, 0:1]), reads=["xtz%d" % s], writes=["jz", "sz0"])
        P.op("scalar", lambda e: e.activation(sz[:, 1:2], sz[:, 0:1], AF.Sqrt, bias=NORM_EPS, scale=1.0 / D), reads=["sz0"], writes=["sz1"])
        P.op("vector", lambda e: e.reciprocal(sz[:, 2:3], sz[:, 1:2]), reads=["sz1"], writes=["sz2"])
        P.op("vector", lambda e, s=s: e.scalar_tensor_tensor(xoz[s][:], xtz[s][:], sz[:, 2:3], fng[:], ALU.mult, ALU.mult), reads=["xtz%d" % s, "sz2", "fng"], writes=["xoz%d" % s])
        P.dma("sync", out[i * 128:(i + 1) * 128, :], xoz[s][:], reads=["xoz%d" % s], writes=[("out", i)])
    return finish()


_CACHE = {}


def _moe_layout(inputs):
    f = np.float32
    out = {}
    for nm, key in (("w1q", "moe_w1"), ("w3q", "moe_w3")):
        w = np.asarray(inputs[key], dtype=f)[0].reshape(NE, 2, 4, 128, 7, 512)
        w = w.transpose(4, 1, 0, 3, 2, 5)
        for fb in range(7):
            for h in range(2):
                out["%s_%d_%d" % (nm, fb, h)] = np.ascontiguousarray(w[fb, h]).reshape(NE * 128, 2048)
    w = np.asarray(inputs["moe_w2"], dtype=f)[0].reshape(NE, 7, 2, 2, 128, D)
    w = w.transpose(1, 2, 0, 4, 3, 5)
    for fb in range(7):
        for h in range(2):
            out["w2q_%d_%d" % (fb, h)] = np.ascontiguousarray(w[fb, h]).reshape(NE * 128, 2048)
    return out


def _core_inputs(inputs, b, consts, moe_l=None):
    f = np.float32
    m = {}
    m.update(moe_l if moe_l is not None else _moe_layout(inputs))
    m["xin"] = np.ascontiguousarray(np.concatenate([inputs["x"][b], inputs["ctx"][b]], axis=0), dtype=f)
    cc = np.concatenate([np.asarray(inputs["c"][b]).reshape(8, 128).T, np.asarray(inputs["c_ctx"]).reshape(8, 128).T], axis=1)
    m["cT"] = np.ascontiguousarray(cc, dtype=f)
    for k in ("w_ada", "b_ada", "norm_mix", "norm_ffn", "w_in", "ret_gn", "attn_qn", "attn_kn", "pool_w", "pool_scale",
              "w_branch", "w_out", "ffn_w1", "ffn_w3", "ffn_w2", "moe_router_b", "final_norm"):
        m[k] = np.ascontiguousarray(inputs[k], dtype=f)
    m["ret_decay"] = np.ascontiguousarray(np.asarray(inputs["ret_decay"]).reshape(2, 8), dtype=f)
    m["k_ident"] = consts["ident"]
    m["k_acos"] = consts["acos"]
    m["k_asin"] = consts["asin"]
    m["k_rt4"] = np.ascontiguousarray(np.stack([consts["rcos"], consts["rsin"], consts["rcosk"], consts["rsink"]], axis=2))
    m["moe_router_t"] = np.ascontiguousarray(np.asarray(inputs["moe_router"])[0].T, dtype=f)
    m["moe_router_p"] = np.ascontiguousarray(np.asarray(inputs["moe_router"], dtype=f)[0].reshape(8, 128, NE).transpose(1, 0, 2))
    m["k_rtab"] = consts["rtab"]
    m["k_pcol"] = consts["pcol"]
    m["k_pedge"] = consts["pedge"]
    m["k_tri"] = consts["tri"]
    m["k_iot"] = consts["iot"]
    return m


def kernel(**inputs):
    inputs = {k: np.asarray(v) for k, v in inputs.items()}
    consts = _host_consts()
    if "nc" not in _CACHE:
        _CACHE["nc"] = build()
    nc = _CACHE["nc"]
    moe_l = _moe_layout(inputs)
    in_maps = [_core_inputs(inputs, b, consts, moe_l) for b in range(8)]
    res = run_bass_kernel_spmd(nc, in_maps, core_ids=list(range(8)))
    outs = [np.asarray(r["out"], dtype=np.float32) for r in res.results]
    return np.stack(outs, axis=0)
```
